# Optimizing a Trainium2 kernel written in Bass

```python
import jax
import jax.numpy as jnp
from jax import lax
import numpy as np

D_MODEL = 1024
BATCH = 16
SEQ = 4096
DEPTH = 1

GRID_W = 64
CTX_LEN = 256
MIX_WIDTH = D_MODEL
HG_WIDTH = MIX_WIDTH // 2
HG_HEADS = 4
HG_KEY = HG_WIDTH // HG_HEADS
HG_VAL = HG_WIDTH // HG_HEADS
HG_CHUNK = 64
HG_SUB = 16
LOG_F_MIN = -4.0
ATT_WIDTH = MIX_WIDTH - HG_WIDTH
HEAD_DIM = 64
ATT_Q_HEADS = ATT_WIDTH // HEAD_DIM
ATT_KV_HEADS = 2
ATT_GROUP = ATT_Q_HEADS // ATT_KV_HEADS
WINDOW = 128
ATT_BLOCK = 128
ROPE_BASE = 10000.0
ROT_PAIRS = HEAD_DIM // 4
IN_COLS = 5 * HG_WIDTH + (ATT_Q_HEADS + 2 * ATT_KV_HEADS) * HEAD_DIM
N_EXPERTS = 64
TOP_K = 8
N_GROUPS = 8
TOPK_GROUPS = 4
EXPERT_FF = D_MODEL // 4
ROUTED_SCALE = 2.5
LN_EPS = 1e-5
NORM_EPS = 1e-6

kernel_name = 'hymba_hgrn2_swa_moe_dit_layer'


def layer_norm(x, w=None, b=None, eps=LN_EPS):
    x32 = x.astype(jnp.float32)
    mu = jnp.mean(x32, axis=-1, keepdims=True)
    xc = x32 - mu
    y = xc * lax.rsqrt(jnp.mean(xc * xc, axis=-1, keepdims=True) + eps)
    if w is not None:
        y = y * w.astype(jnp.float32) + b.astype(jnp.float32)
    return y.astype(x.dtype)


def modulate(x, shift, scale):
    return layer_norm(x, eps=NORM_EPS) * (1.0 + scale) + shift


def split_columns(z):
    sizes = [HG_WIDTH] * 5 + [ATT_Q_HEADS * HEAD_DIM, ATT_KV_HEADS * HEAD_DIM, ATT_KV_HEADS * HEAD_DIM]
    bounds = [int(v) for v in np.cumsum(sizes)[:-1]]
    return jnp.split(z, bounds, axis=-1)


def rope_2d_angles(n):
    rows = n // GRID_W
    row = jnp.repeat(jnp.arange(rows), GRID_W).astype(jnp.float32)
    col = jnp.tile(jnp.arange(GRID_W), rows).astype(jnp.float32)
    freqs = ROPE_BASE ** (-jnp.arange(ROT_PAIRS, dtype=jnp.float32) / ROT_PAIRS)
    return row[:, None] * freqs, col[:, None] * freqs


def rotate(x, ang):
    x1, x2 = jnp.split(x.astype(jnp.float32), 2, axis=-1)
    cos = jnp.cos(ang)[:, None, :]
    sin = jnp.sin(ang)[:, None, :]
    return jnp.concatenate([x1 * cos - x2 * sin, x1 * sin + x2 * cos], axis=-1)


def apply_rope_2d(x, ang_row, ang_col):
    half = HEAD_DIM // 2
    y = jnp.concatenate([rotate(x[..., :half], ang_row), rotate(x[..., half:], ang_col)], axis=-1)
    return y.astype(x.dtype)


def hgrn2_scan(q, k, v, logf, s0, with_output):
    bsz, nh, n, dk = q.shape
    dv = v.shape[-1]
    nc = n // HG_CHUNK
    ns = HG_CHUNK // HG_SUB

    def chunked(t):
        return jnp.moveaxis(t.reshape(bsz, nh, nc, HG_CHUNK, t.shape[-1]), 2, 0)

    pos = jnp.arange(HG_CHUNK)
    causal = pos[:, None] >= pos[None, :]
    key_ok = pos[None, :] < (jnp.arange(ns)[:, None] + 1) * HG_SUB

    def step(s, inp):
        qc, kc, vc, gc = inp
        bcum = jnp.cumsum(gc, axis=-2)
        b_last = bcum[:, :, -1:, :]
        s_new = jnp.exp(b_last[:, :, 0, :])[..., None] * s + jnp.einsum(
            'bhcd,bhce->bhde', kc * jnp.exp(b_last - bcum), vc)
        if not with_output:
            return s_new, None
        bsub = bcum.reshape(bsz, nh, ns, HG_SUB, dk)
        ref = jnp.concatenate([jnp.zeros_like(bsub[:, :, :1, 0]), bsub[:, :, :-1, -1]], axis=2)
        q_hat = qc.reshape(bsz, nh, ns, HG_SUB, dk) * jnp.exp(bsub - ref[:, :, :, None])
        e = ref[:, :, :, None, :] - bcum[:, :, None, :, :]
        e = jnp.where(key_ok[None, None, :, :, None], e, -jnp.inf)
        k_hat = kc[:, :, None] * jnp.exp(e)
        att = jnp.einsum('bhnid,bhnjd->bhnij', q_hat, k_hat).reshape(bsz, nh, HG_CHUNK, HG_CHUNK)
        att = jnp.where(causal, att, 0.0)
        o = jnp.einsum('bhij,bhje->bhie', att, vc) + jnp.einsum(
            'bhid,bhde->bhie', qc * jnp.exp(bcum), s)
        return s_new, o

    s_fin, o = lax.scan(step, s0, (chunked(q), chunked(k), chunked(v), chunked(logf)))
    if with_output:
        o = jnp.moveaxis(o, 0, 2).reshape(bsz, nh, n, dv)
    return o, s_fin


def hgrn2_group(z_q, z_ff, z_fb, z_i, lb_f, lb_b, s0_f, s0_b, with_output):
    bsz, n, _ = z_q.shape

    def heads(t):
        return t.astype(jnp.float32).reshape(bsz, n, HG_HEADS, -1).transpose(0, 2, 1, 3)

    def gates(zf, lb):
        lb32 = lb.astype(jnp.float32)
        f = lb32 + (1.0 - lb32) * jax.nn.sigmoid(zf.astype(jnp.float32))
        return heads(1.0 - f), heads(jnp.maximum(jnp.log(f), LOG_F_MIN))

    q = heads(z_q)
    v = heads(z_i)
    k_f, g_f = gates(z_ff, lb_f)
    k_b, g_b = gates(z_fb, lb_b)
    rev = lambda t: jnp.flip(t, axis=2)
    o_f, s_f = hgrn2_scan(q, k_f, v, g_f, s0_f, with_output)
    o_b, s_b = hgrn2_scan(rev(q), rev(k_b), rev(v), rev(g_b), s0_b, with_output)
    o = o_f + rev(o_b) if with_output else None
    return o, s_f, s_b


def hgrn2_readout(o, z_g, norm_w):
    bsz, nh, n, dv = o.shape
    o = o.transpose(0, 2, 1, 3)
    o = o * lax.rsqrt(jnp.mean(o * o, axis=-1, keepdims=True) + NORM_EPS)
    o = o.reshape(bsz, n, nh * dv) * norm_w.astype(jnp.float32)
    return (o * jax.nn.silu(z_g.astype(jnp.float32))).astype(z_g.dtype)


def attn_context(q, k_ctx, v_ctx, sink):
    bsz, l, _, _ = q.shape
    qg = q.reshape(bsz, l, ATT_KV_HEADS, ATT_GROUP, HEAD_DIM)
    s = jnp.einsum('blhgd,bmhd->bhglm', qg, k_ctx).astype(jnp.float32) * HEAD_DIM ** -0.5
    sink_col = jnp.broadcast_to(sink.astype(jnp.float32).reshape(ATT_KV_HEADS, ATT_GROUP)[None, :, :, None, None],
                                (bsz, ATT_KV_HEADS, ATT_GROUP, l, 1))
    p = jax.nn.softmax(jnp.concatenate([sink_col, s], axis=-1), axis=-1)[..., 1:].astype(v_ctx.dtype)
    o = jnp.einsum('bhglm,bmhd->blhgd', p, v_ctx)
    return o.reshape(bsz, l, ATT_Q_HEADS * HEAD_DIM)


def attn_latent_window(q, k, v, k_ctx, v_ctx, sink):
    bsz, n, _, _ = q.shape
    l = k_ctx.shape[1]
    nb = n // ATT_BLOCK
    pad = ((0, 0), (ATT_BLOCK, ATT_BLOCK), (0, 0), (0, 0))
    kp = jnp.pad(k, pad)
    vp = jnp.pad(v, pad)
    qi = jnp.arange(ATT_BLOCK)
    kj = jnp.arange(3 * ATT_BLOCK)
    scale = HEAD_DIM ** -0.5
    sink_col = jnp.broadcast_to(sink.astype(jnp.float32).reshape(ATT_KV_HEADS, ATT_GROUP)[None, :, :, None, None],
                                (bsz, ATT_KV_HEADS, ATT_GROUP, ATT_BLOCK, 1))

    def band(bidx):
        start = bidx * ATT_BLOCK
        qb = lax.dynamic_slice_in_dim(q, start, ATT_BLOCK, axis=1).reshape(
            bsz, ATT_BLOCK, ATT_KV_HEADS, ATT_GROUP, HEAD_DIM)
        kb = lax.dynamic_slice_in_dim(kp, start, 3 * ATT_BLOCK, axis=1)
        vb = lax.dynamic_slice_in_dim(vp, start, 3 * ATT_BLOCK, axis=1)
        qpos = start + qi
        kpos = start - ATT_BLOCK + kj
        ok = (jnp.abs(kpos[None, :] - qpos[:, None]) <= WINDOW) & (kpos >= 0)[None, :] & (kpos < n)[None, :]
        s_w = jnp.einsum('bqhgd,bkhd->bhgqk', qb, kb).astype(jnp.float32) * scale
        s_w = jnp.where(ok, s_w, -jnp.inf)
        s_c = jnp.einsum('bqhgd,bmhd->bhgqm', qb, k_ctx).astype(jnp.float32) * scale
        p = jax.nn.softmax(jnp.concatenate([sink_col, s_c, s_w], axis=-1), axis=-1).astype(v.dtype)
        o = jnp.einsum('bhgqm,bmhd->bqhgd', p[..., 1:1 + l], v_ctx) + jnp.einsum(
            'bhgqk,bkhd->bqhgd', p[..., 1 + l:], vb)
        return o.reshape(bsz, ATT_BLOCK, ATT_Q_HEADS * HEAD_DIM)

    out = lax.map(band, jnp.arange(nb))
    return jnp.moveaxis(out, 0, 1).reshape(bsz, n, ATT_Q_HEADS * HEAD_DIM)


def mixer_context(h, w_in, lb_f, lb_b, norm_w, sink, with_output):
    bsz, l, _ = h.shape
    zq, zff, zfb, zi, zg, aq, ak, av = split_columns(h @ w_in)
    k_ctx = ak.reshape(bsz, l, ATT_KV_HEADS, HEAD_DIM)
    v_ctx = av.reshape(bsz, l, ATT_KV_HEADS, HEAD_DIM)
    s0 = jnp.zeros((bsz, HG_HEADS, HG_KEY, HG_VAL), jnp.float32)
    o_h, s_f, s_b = hgrn2_group(zq, zff, zfb, zi, lb_f, lb_b, s0, s0, with_output)
    if not with_output:
        return None, k_ctx, v_ctx, s_f, s_b
    y_h = hgrn2_readout(o_h, zg, norm_w)
    y_a = attn_context(aq.reshape(bsz, l, ATT_Q_HEADS, HEAD_DIM), k_ctx, v_ctx, sink)
    return jnp.concatenate([y_h, y_a.astype(y_h.dtype)], axis=-1), k_ctx, v_ctx, s_f, s_b


def mixer_latent(h, w_in, lb_f, lb_b, norm_w, sink, k_ctx, v_ctx, s_f, s_b, ang_row, ang_col):
    bsz, n, _ = h.shape
    zq, zff, zfb, zi, zg, aq, ak, av = split_columns(h @ w_in)
    o_h, _, _ = hgrn2_group(zq, zff, zfb, zi, lb_f, lb_b, s_f, s_b, True)
    y_h = hgrn2_readout(o_h, zg, norm_w)
    q = apply_rope_2d(aq.reshape(bsz, n, ATT_Q_HEADS, HEAD_DIM), ang_row, ang_col)
    k = apply_rope_2d(ak.reshape(bsz, n, ATT_KV_HEADS, HEAD_DIM), ang_row, ang_col)
    v = av.reshape(bsz, n, ATT_KV_HEADS, HEAD_DIM)
    y_a = attn_latent_window(q, k, v, k_ctx, v_ctx, sink)
    return jnp.concatenate([y_h, y_a.astype(y_h.dtype)], axis=-1)


def swiglu(h, wg, wu, wd):
    return (jax.nn.silu(h @ wg) * (h @ wu)) @ wd


def moe(h, router_w, router_bias, w_gate, w_up, w_down, sw_gate, sw_up, sw_down):
    shape = h.shape
    ht = h.reshape(-1, shape[-1])
    t = ht.shape[0]
    scores = jax.nn.sigmoid(ht.astype(jnp.float32) @ router_w.astype(jnp.float32))
    sel = scores + router_bias.astype(jnp.float32)
    grp = sel.reshape(t, N_GROUPS, N_EXPERTS // N_GROUPS)
    grp_score = jnp.sum(lax.top_k(grp, 2)[0], axis=-1)
    _, gidx = lax.top_k(grp_score, TOPK_GROUPS)
    rows = jnp.arange(t)[:, None]
    gmask = jnp.zeros((t, N_GROUPS), bool).at[rows, gidx].set(True)
    emask = jnp.repeat(gmask, N_EXPERTS // N_GROUPS, axis=1)
    _, eidx = lax.top_k(jnp.where(emask, sel, -jnp.inf), TOP_K)
    w = jnp.take_along_axis(scores, eidx, axis=-1)
    w = w / jnp.sum(w, axis=-1, keepdims=True) * ROUTED_SCALE
    gates = jnp.zeros((t, N_EXPERTS), jnp.float32).at[rows, eidx].set(w).astype(h.dtype)
    y = swiglu(ht, sw_gate, sw_up, sw_down)
    for e in range(N_EXPERTS):
        y = y + gates[:, e:e + 1] * swiglu(ht, w_gate[e], w_up[e], w_down[e])
    return y.reshape(shape)


def setup_inputs(seed: int = 0) -> dict:
    key = jax.random.key(seed)
    ks = jax.random.split(key, 32)
    beta = (8.0 * DEPTH) ** -0.25

    def nrm(k, shape, s):
        return jax.random.normal(k, shape, jnp.float32) * s

    return {
        'x': nrm(ks[0], (BATCH, SEQ, D_MODEL), 1.0),
        'c': nrm(ks[1], (BATCH, D_MODEL), 1.0),
        'ctx': nrm(ks[2], (BATCH, CTX_LEN, D_MODEL), 1.0),
        'c_ctx': nrm(ks[3], (D_MODEL,), 1.0),
        'w_ada': nrm(ks[4], (DEPTH, D_MODEL, 6 * D_MODEL), 0.5 * D_MODEL ** -0.5),
        'b_ada': nrm(ks[5], (DEPTH, 6 * D_MODEL), 0.02),
        'w_in': nrm(ks[6], (DEPTH, D_MODEL, IN_COLS), D_MODEL ** -0.5),
        'hg_lb_fwd': nrm(ks[7], (DEPTH + 1, HG_WIDTH), 0.1),
        'hg_lb_bwd': nrm(ks[8], (DEPTH + 1, HG_WIDTH), 0.1),
        'hg_norm_w': 1.0 + nrm(ks[9], (DEPTH, HG_WIDTH), 0.02),
        'attn_sink': nrm(ks[10], (DEPTH, ATT_Q_HEADS), 0.5),
        'w_out': nrm(ks[11], (DEPTH, MIX_WIDTH, D_MODEL), beta * MIX_WIDTH ** -0.5),
        'ln1_w': 1.0 + nrm(ks[12], (DEPTH, D_MODEL), 0.02),
        'ln1_b': nrm(ks[13], (DEPTH, D_MODEL), 0.02),
        'router_w': nrm(ks[14], (DEPTH, D_MODEL, N_EXPERTS), D_MODEL ** -0.5),
        'router_bias': nrm(ks[15], (DEPTH, N_EXPERTS), 0.01),
        'exp_w_gate': nrm(ks[16], (DEPTH, N_EXPERTS, D_MODEL, EXPERT_FF), D_MODEL ** -0.5),
        'exp_w_up': nrm(ks[17], (DEPTH, N_EXPERTS, D_MODEL, EXPERT_FF), D_MODEL ** -0.5),
        'exp_w_down': nrm(ks[18], (DEPTH, N_EXPERTS, EXPERT_FF, D_MODEL), beta * EXPERT_FF ** -0.5),
        'shared_w_gate': nrm(ks[19], (DEPTH, D_MODEL, EXPERT_FF), D_MODEL ** -0.5),
        'shared_w_up': nrm(ks[20], (DEPTH, D_MODEL, EXPERT_FF), D_MODEL ** -0.5),
        'shared_w_down': nrm(ks[21], (DEPTH, EXPERT_FF, D_MODEL), beta * EXPERT_FF ** -0.5),
        'ln2_w': 1.0 + nrm(ks[22], (DEPTH, D_MODEL), 0.02),
        'ln2_b': nrm(ks[23], (DEPTH, D_MODEL), 0.02),
    }


def reference(x, c, ctx, c_ctx, w_ada, b_ada, w_in, hg_lb_fwd, hg_lb_bwd, hg_norm_w, attn_sink,
              w_out, ln1_w, ln1_b, router_w, router_bias, exp_w_gate, exp_w_up, exp_w_down,
              shared_w_gate, shared_w_up, shared_w_down, ln2_w, ln2_b):
    alpha = (2.0 * DEPTH) ** 0.25
    ang_row, ang_col = rope_2d_angles(x.shape[1])
    lb_fwd = jnp.cumsum(jax.nn.softmax(hg_lb_fwd.astype(jnp.float32), axis=0), axis=0)
    lb_bwd = jnp.cumsum(jax.nn.softmax(hg_lb_bwd.astype(jnp.float32), axis=0), axis=0)
    for layer in range(DEPTH):
        last = layer == DEPTH - 1
        mod = jax.nn.silu(c) @ w_ada[layer] + b_ada[layer]
        sh1, sc1, g1, sh2, sc2, g2 = jnp.split(mod[:, None, :], 6, axis=-1)
        mod_c = jax.nn.silu(c_ctx) @ w_ada[layer] + b_ada[layer]
        csh1, csc1, cg1, csh2, csc2, cg2 = jnp.split(mod_c, 6, axis=-1)
        y_ctx, k_ctx, v_ctx, s_f, s_b = mixer_context(
            modulate(ctx, csh1, csc1), w_in[layer], lb_fwd[layer], lb_bwd[layer],
            hg_norm_w[layer], attn_sink[layer], not last)
        y_lat = mixer_latent(modulate(x, sh1, sc1), w_in[layer], lb_fwd[layer], lb_bwd[layer],
                             hg_norm_w[layer], attn_sink[layer], k_ctx, v_ctx, s_f, s_b,
                             ang_row, ang_col)
        x = layer_norm(alpha * x + g1 * (y_lat @ w_out[layer]), ln1_w[layer], ln1_b[layer])
        ffn = moe(modulate(x, sh2, sc2), router_w[layer], router_bias[layer], exp_w_gate[layer],
                  exp_w_up[layer], exp_w_down[layer], shared_w_gate[layer], shared_w_up[layer],
                  shared_w_down[layer])
        x = layer_norm(alpha * x + g2 * ffn, ln2_w[layer], ln2_b[layer])
        if not last:
            ctx = layer_norm(alpha * ctx + cg1 * (y_ctx @ w_out[layer]), ln1_w[layer], ln1_b[layer])
            ffn_c = moe(modulate(ctx, csh2, csc2), router_w[layer], router_bias[layer],
                        exp_w_gate[layer], exp_w_up[layer], exp_w_down[layer], shared_w_gate[layer],
                        shared_w_up[layer], shared_w_down[layer])
            ctx = layer_norm(alpha * ctx + cg2 * ffn_c, ln2_w[layer], ln2_b[layer])
    return x
```

```python
import numpy as np
import concourse.bass as bass
import concourse.mybir as mybir
from concourse.bass_utils import run_bass_kernel_spmd

F32 = mybir.dt.float32
BF16 = mybir.dt.bfloat16
AF = mybir.ActivationFunctionType
ALU = mybir.AluOpType
AX = mybir.AxisListType

D = 1024
NE = 64
ALPHA = 2.0 ** 0.25
NEG = -30000.0


class Buf:
    __slots__ = ("t", "name", "lw", "rd", "dsem")

    def __init__(self, t, name):
        self.t = t
        self.name = name
        self.lw = None
        self.rd = {}
        self.dsem = None

    def __getitem__(self, idx):
        return self.t[idx]


class KB:
    def __init__(self, nc):
        self.nc = nc
        self.eng = dict(pe=nc.tensor, act=nc.scalar, dve=nc.vector, pool=nc.gpsimd, sp=nc.sync)
        self.sems = {}
        self.cnt = {}
        self.seen = {e: {} for e in self.eng}
        for e in ("pe", "act", "dve", "pool"):
            self._mksem("E_" + e)
        self.nbuf = 0
        self.ninstr = 0

    def _mksem(self, key):
        self.sems[key] = self.nc.alloc_semaphore(key)
        self.cnt[key] = 0
        return key

    def sb(self, shape, dtype, name=None):
        self.nbuf += 1
        name = name or f"sb{self.nbuf}"
        if getattr(self, "scope", None) is not None:
            return Buf(self.scope.enter_context(self.nc.sbuf_tensor(name, list(shape), dtype)), name)
        return Buf(self.nc.alloc_sbuf_tensor(name, list(shape), dtype), name)

    def barrier(self):
        deps = [(kk, v) for kk, v in self.cnt.items() if v > 0]
        for e in self.eng:
            self._need(e, deps)

    def ps(self, shape, dtype, name=None):
        self.nbuf += 1
        name = name or f"ps{self.nbuf}"
        return Buf(self.nc.alloc_psum_tensor(name, list(shape), dtype), name)

    def _need(self, e, deps):
        seen = self.seen[e]
        best = {}
        for d in deps:
            if d is None:
                continue
            k, v = d
            if seen.get(k, 0) >= v:
                continue
            if best.get(k, 0) < v:
                best[k] = v
        for k, v in best.items():
            self.eng[e].wait_ge(self.sems[k], v)
            seen[k] = v

    def _deps(self, reads, writes):
        deps = []
        for b in reads:
            deps.append(b.lw)
        for b in writes:
            deps.append(b.lw)
            for k, v in b.rd.items():
                deps.append((k, v))
        return deps

    def _record(self, reads, writes, tok):
        k, v = tok
        for b in reads:
            if b.rd.get(k, 0) < v:
                b.rd[k] = v
        for b in writes:
            b.lw = tok
            b.rd = {}

    def op(self, e, ins_fn, reads=(), writes=()):
        self._need(e, self._deps(reads, writes))
        ins = ins_fn(self.eng[e])
        k = "E_" + e
        self.cnt[k] += 1
        ins.then_inc(self.sems[k], 1)
        self._record(reads, writes, (k, self.cnt[k]))
        self.ninstr += 1
        return ins

    def pe(self, fns, reads, out):
        self._need("pe", self._deps(reads, [out]))
        n = len(fns)
        k = "E_pe"
        for i, f in enumerate(fns):
            ins = f(self.nc.tensor)
            self.ninstr += 1
            if i == n - 1:
                self.cnt[k] += 1
                ins.then_inc(self.sems[k], 1)
        self._record(reads, [out], (k, self.cnt[k]))

    def dma(self, q, out_ap, in_ap, reads=(), writes=(), stream=None, nowaw=False, **kw):
        sbuf = stream or (writes[0] if writes else reads[0])
        if sbuf.dsem is None:
            sbuf.dsem = self._mksem("D_" + sbuf.name)
        k = sbuf.dsem
        if nowaw and all(b.lw is not None and b.lw[0] == k and not b.rd for b in writes):
            self._need(q, self._deps(reads, []))
        else:
            self._need(q, self._deps(reads, writes))
        ins = self.eng[q].dma_start(out=out_ap, in_=in_ap, **kw)
        self.cnt[k] += 16
        ins.then_inc(self.sems[k], 16)
        self._record(reads, writes, (k, self.cnt[k]))
        self.ninstr += 1
        return ins

    def idma(self, out_ap, out_off, in_ap, in_off, reads=(), writes=(), stream=None, nowaw=False):
        sbuf = stream
        if sbuf.dsem is None:
            sbuf.dsem = self._mksem("D_" + sbuf.name)
        k = sbuf.dsem
        if nowaw and all(b.lw is not None and b.lw[0] == k and not b.rd for b in writes):
            self._need("pool", self._deps(reads, []))
        else:
            self._need("pool", self._deps(reads, writes))
        ins = self.nc.gpsimd.indirect_dma_start(out=out_ap, out_offset=out_off, in_=in_ap, in_offset=in_off)
        self.cnt[k] += 16
        ins.then_inc(self.sems[k], 16)
        self._record(reads, writes, (k, self.cnt[k]))
        self.ninstr += 1
        return ins

    def wait_all(self, e, bufs):
        deps = []
        for b in bufs:
            deps.append(b.lw)
            for k, v in b.rd.items():
                deps.append((k, v))
        self._need(e, deps)


def bc(ap, axis, shape):
    return ap.unsqueeze(axis).broadcast_to(list(shape))


import contextlib

FM_COLS = 512 * 5 + 128 * 2
TM_COLS = 512 * 2 + 128
FM_OFF = dict(zq=0, zff=512, zfb=1024, aq=1536, aqp=2048, ak=2560, akp=2688)
FM_W = dict(zq=512, zff=512, zfb=512, aq=512, aqp=512, ak=128, akp=128)
TM_OFF = dict(zi=0, zg=512, av=1024)
TM_W = dict(zi=512, zg=512, av=128)
BLK = 256
DEBUG = False
BT = BLK // 128


def build(NB, N, L, TBT=16):
    nc = bass.Bass("TRN2", target_bir_lowering=False)
    k = KB(nc)
    k.scope = None
    R = NB + 1
    NT = N // 128
    NBLK = N // BLK
    TOT = NB * NT
    assert TOT % TBT == 0 and L % 128 == 0 and L <= BLK

    def din(name, shape, dt=F32):
        return nc.dram_tensor(name, list(shape), dt, kind="ExternalInput").ap()

    x = din("x", [NB, N, D]); ctx = din("ctx", [NB, L, D]); c3T = din("c3T", [D, R])
    w_ada = din("w_ada", [D, 6 * D]); b_adaT = din("b_adaT", [128, 48]); bview = din("b_ada_g", [4, D])
    w_fm = din("w_fm", [D, FM_COLS]); w_tm = din("w_tm", [D, TM_COLS])
    lbT = din("lbT", [128, 2, 2, 4]); normw_b = din("normw_b", [128, 512]); sink_b = din("sink_b", [128, 8])
    w_out = din("w_out", [D, D])
    ln1w_b = din("ln1w_b", [128, D]); ln1b_b = din("ln1b_b", [128, D])
    ln2w_b = din("ln2w_b", [128, D]); ln2b_b = din("ln2b_b", [128, D])
    router_w = din("router_w", [D, NE]); rbias_b = din("rbias_b", [128, NE])
    ew_all = din("ew_all", [(NE + 1) * 128, 6144])
    NA = 9
    NTL = (TOT * 128 * NA) // 512 + NE + 1
    NSLOT = NTL * 512
    zrows = din("zrows", [512, D], BF16); ramp_d = din("ramp", [128, 256]); jv_d = din("jv", [128, NA]); pcol_d = din("pcol", [128, 1])
    ustrict_d = din("ustrict", [128, 128])
    ident_f = din("ident_f", [128, 128]); cosT = din("cosT", [128, N]); sinT = din("sinT", [128, N])
    mask_f = din("mask_f", [128, 128]); mask_b = din("mask_b", [128, 128])
    negm_prev = din("negm_prev", [128, 128]); negm_next = din("negm_next", [128, 128])
    rst64 = din("rst64", [128, 512]); rst16 = din("rst16", [128, 512]); sel_rows = din("sel_rows", [R, R, 128])
    out = nc.dram_tensor("out", [NB, N, D], F32, kind="ExternalOutput").ap()
    x1s = nc.dram_tensor("x1s", [NB, N, D], F32, kind="Internal").ap()
    obs = nc.dram_tensor("obs", [NB, N, 512], F32, kind="Internal").ap()
    wog = nc.dram_tensor("wog", [NB, D, D], BF16, kind="Internal").ap()
    rws = nc.dram_tensor("rws", [NB, 3, 128, D], F32, kind="Internal").ap()
    Xs = nc.dram_tensor("Xs", [NSLOT, D], BF16, kind="Internal").ap()
    Ys = nc.dram_tensor("Ys", [NSLOT, D], BF16, kind="Internal").ap()
    H2 = nc.dram_tensor("H2", [TOT * 128, D], BF16, kind="Internal").ap()

    pbs = [k.ps([128, 512], F32, f"pb{i}") for i in range(6)]
    pts = [k.ps([128, 1024], BF16, f"pt{i}") for i in range(2)]
    st = {"pb": 0, "pt": 0, "rr": {}}

    def PB():
        while True:
            st["pb"] = (st["pb"] + 1) % len(pbs)
            if st["pb"] not in st.get("excl", ()):
                return pbs[st["pb"]]

    def PT():
        st["pt"] = (st["pt"] + 1) % len(pts)
        return pts[st["pt"]]

    def ring(name, n, shape, dt):
        key = (name, id(k.scope))
        if key not in st["rr"]:
            st["rr"][key] = [[k.sb(shape, dt, f"{name}_{len(st['rr'])}_{i}") for i in range(n)], 0]
        ent = st["rr"][key]
        ent[1] = (ent[1] + 1) % n
        return ent[0][ent[1]]

    V = lambda fn, r=(), w=(): k.op("dve", fn, r, w)
    A = lambda fn, r=(), w=(): k.op("act", fn, r, w)
    G = lambda fn, r=(), w=(): k.op("pool", fn, r, w)

    def const(src, shape, dt=F32, q="sp", name=None):
        b = k.sb(shape, F32, name + "_f")
        k.dma(q, b[:], src, writes=[b])
        if dt == F32:
            return b
        b2 = k.sb(shape, dt, name + "_b")
        V(lambda e: e.tensor_copy(b2[:], b[:]), [b], [b2])
        return b2

    identF = const(ident_f, [128, 128], F32, name="identF")
    identB = k.sb([128, 128], BF16, "identB")
    V(lambda e: e.tensor_copy(identB[:], identF[:]), [identF], [identB])
    mod = k.sb([128, 48, R], F32, "mod")
    modp1 = k.sb([128, 48, R], F32, "modp1")
    xts_ring = [k.sb([128, D], F32, f"xt{i}") for i in range(3)]
    xst = [0]

    def XT():
        xst[0] = (xst[0] + 1) % 3
        return xts_ring[xst[0]]

    xh = k.sb([128, D], F32, "xh")
    u1 = k.sb([128, D], F32, "u1")
    x1r = [k.sb([128, D], F32, f"x1r{i}") for i in range(2)]
    x1i = [0]

    def X1():
        x1i[0] = (x1i[0] + 1) % 2
        return x1r[x1i[0]]

    lnst = k.sb([128, 2, 6], F32, "lnst"); lnmv = k.sb([128, 2], F32, "lnmv")
    lnrs = k.sb([128, 1], F32, "lnrs"); lnnm = k.sb([128, 1], F32, "lnnm")

    ctok = k.sb([1, 2], F32, "ctok")
    ztok = k.sb([1, 2], F32, "ztok")
    bg_list = []
    bg_pos = [0]

    def background_dmas(n):
        for _ in range(n):
            if bg_pos[0] >= len(bg_list):
                return
            kind, i = bg_list[bg_pos[0]]
            bg_pos[0] += 1
            k.dma("pool", Xs[i * 512:(i + 1) * 512, :], zrows, reads=[ztok], stream=ztok)

    k.scope = contextlib.ExitStack()
    selr = k.sb([R, R, 128], F32, "selr")
    k.dma("sp", selr[:], sel_rows, writes=[selr])
    c3 = k.sb([128, 8, R], F32, "c3")
    k.dma("sp", c3[:], c3T.rearrange("(kc p) r -> p kc r", p=128), writes=[c3])
    sC = k.sb([128, 8, R], F32, "sC")
    A(lambda e: e.activation(sC[:], c3[:], AF.Exp, scale=-1.0), [c3], [sC])
    V(lambda e: e.tensor_scalar(sC[:], sC[:], 1.0, None, ALU.add), [sC], [sC])
    V(lambda e: e.reciprocal(sC[:], sC[:]), [sC], [sC])
    V(lambda e: e.tensor_tensor(sC[:], sC[:], c3[:], ALU.mult), [sC, c3], [sC])
    badaT = k.sb([128, 48], F32, "badaT")
    k.dma("sp", badaT[:], b_adaT, writes=[badaT])
    modps = PB()
    st["excl"] = {st["pb"]}
    grow = k.sb([R, 4, D], F32, "grow")
    for cc in range(12):
        wa = ring("wa", 2, [128, 8, 512], F32)
        k.dma("act", wa[:], w_ada.rearrange("(kc p) n -> p kc n", p=128)[:, :, cc * 512:(cc + 1) * 512], writes=[wa])
        for j in range(4):
            jj = cc * 4 + j
            k.pe([(lambda e, kc=kc: e.matmul(modps[:, jj * R:(jj + 1) * R], wa[:, kc, j * 128:(j + 1) * 128], sC[:, kc, :],
                                           start=(kc == 0), stop=(kc == 7))) for kc in range(8)], [wa, sC], modps)
        if 4 <= cc <= 11:
            gi = (cc - 4) // 2
            hf = cc % 2
            pr = PB()
            k.pe([(lambda e, kc=kc: e.matmul(pr[0:R, :], sC[:, kc, :], wa[:, kc, :], start=(kc == 0), stop=(kc == 7)))
                  for kc in range(8)], [wa, sC], pr)
            V(lambda e: e.tensor_copy(grow[:, gi, hf * 512:(hf + 1) * 512], pr[0:R, :]), [pr], [grow])
    V(lambda e: e.tensor_tensor(mod[:], modps[:, 0:48 * R].rearrange("p (a r) -> p a r", r=R), bc(badaT[:], 2, [128, 48, R]), ALU.add),
      [modps, badaT], [mod])
    V(lambda e: e.tensor_scalar(modp1[:], mod[:], 1.0, None, ALU.add), [mod], [modp1])
    st["excl"] = set()
    badar = k.sb([R, 4, D], F32, "badar")
    for r in range(R):
        k.dma("sp", badar[r:r + 1, :, :], bview.rearrange("(o g) d -> o g d", o=1), writes=[badar])
    V(lambda e: e.tensor_tensor(grow[:], grow[:], badar[:], ALU.add), [grow, badar], [grow])
    grbt = k.sb([128, D], F32, "grbt")
    wob = k.sb([128, D], BF16, "wob")
    for b in range(NB):
        for gi in range(4):
            for hf in range(2):
                pr = PB()
                k.pe([lambda e: e.matmul(pr[:], selr[:, b, :], grow[:, gi, hf * 512:(hf + 1) * 512], start=True, stop=True)],
                     [selr, grow], pr)
                V(lambda e: e.tensor_copy(grbt[:, hf * 512:(hf + 1) * 512], pr[:]), [pr], [grbt])
            if gi >= 1:
                if gi == 2:
                    V(lambda e: e.tensor_scalar(grbt[:], grbt[:], 1.0, None, ALU.add), [grbt], [grbt])
                k.dma("sp", rws[b, gi - 1], grbt[:], reads=[grbt])
            else:
                for kc in range(8):
                    xt = XT()
                    k.dma("sp", xt[:], w_out[kc * 128:(kc + 1) * 128, :], writes=[xt])
                    V(lambda e: e.tensor_tensor(wob[:], xt[:], grbt[:], ALU.mult), [xt, grbt], [wob])
                    k.dma("sp", wog[b, kc * 128:(kc + 1) * 128, :], wob[:], reads=[wob])
    k.barrier()
    k.scope.close()

    k.scope = contextlib.ExitStack()
    maskF = const(mask_f, [128, 128], name="maskF"); maskB = const(mask_b, [128, 128], name="maskB")
    negP = const(negm_prev, [128, 128], BF16, name="negP"); negN = const(negm_next, [128, 128], BF16, name="negN")
    r64 = const(rst64[:, 0:BLK], [128, BLK], name="r64"); r16 = const(rst16[:, 0:BLK], [128, BLK], name="r16")
    normw = const(normw_b, [128, 512], name="normw")
    ln1w = const(ln1w_b, [128, D], name="ln1w"); ln1b = const(ln1b_b, [128, D], name="ln1b")
    esink = k.sb([128, 8], F32, "esink")
    k.dma("sp", esink[:], sink_b, writes=[esink])
    A(lambda e: e.activation(esink[:], esink[:], AF.Exp), [esink], [esink])
    lbt = k.sb([128, 2, 2, 4], F32, "lbt")
    k.dma("sp", lbt[:], lbT, writes=[lbt])
    oml = k.sb([128, 2, 4], F32, "oml"); noml = k.sb([128, 2, 4], F32, "noml")
    V(lambda e: e.tensor_tensor(oml[:], lbt[:, :, 0, :], lbt[:, :, 1, :], ALU.subtract), [lbt], [oml])
    A(lambda e: e.activation(oml[:], oml[:], AF.Exp), [oml], [oml])
    V(lambda e: e.tensor_scalar(oml[:], oml[:], 1.0, None, ALU.add), [oml], [oml])
    V(lambda e: e.reciprocal(oml[:], oml[:]), [oml], [oml])
    V(lambda e: e.tensor_scalar(noml[:], oml[:], -1.0, None, ALU.mult), [oml], [noml])

    W = k.sb([128, 8, 4096], BF16, "W")
    wmap = {}

    def loadw(fm_names, tm_names, with_wo=None):
        wmap.clear()
        col = 0
        eng_i = [0]

        def stage(src_rows, c0, wd_, kc, dcol):
            xt = XT()
            q = "sp" if eng_i[0] % 2 == 0 else "act"
            k.dma(q, xt[:, 0:wd_], src_rows[:, c0:c0 + wd_], writes=[xt])
            e = ("dve", "act", "pool")[eng_i[0] % 3]
            eng_i[0] += 1
            if e == "act":
                A(lambda en: en.copy(W[:, kc, dcol:dcol + wd_], xt[:, 0:wd_]), [xt], [W])
            elif e == "dve":
                V(lambda en: en.tensor_copy(W[:, kc, dcol:dcol + wd_], xt[:, 0:wd_]), [xt], [W])
            else:
                G(lambda en: en.tensor_copy(W[:, kc, dcol:dcol + wd_], xt[:, 0:wd_]), [xt], [W])

        for nm in fm_names:
            wmap[nm] = col
            col += FM_W[nm]
        for nm in tm_names:
            wmap[nm] = col
            col += TM_W[nm]
        for kc in range(8):
            for (src, OFF, WID, names) in ((w_fm, FM_OFF, FM_W, fm_names), (w_tm, TM_OFF, TM_W, tm_names)):
                for nm in names:
                    stage(src[kc * 128:(kc + 1) * 128, :], OFF[nm], WID[nm], kc, wmap[nm])
        if with_wo is not None:
            for kc in range(8):
                k.dma("sp" if kc % 2 == 0 else "act", W[:, kc, col:col + D], wog[with_wo, kc * 128:(kc + 1) * 128, :], writes=[W])
            wmap["wo"] = col
            col += D
        assert col <= 4096

    def ln_stats(src, eps):
        for a_ in range(2):
            V(lambda e: e.bn_stats(lnst[:, a_, :], src[:, a_ * 512:(a_ + 1) * 512]), [src], [lnst])
        V(lambda e: e.bn_aggr(lnmv[:], lnst[:].rearrange("p a b -> p (a b)")), [lnst], [lnmv])
        A(lambda e: e.activation(lnrs[:], lnmv[:, 1:2], AF.Ln, bias=float(eps)), [lnmv], [lnrs])
        A(lambda e: e.activation(lnrs[:], lnrs[:], AF.Exp, scale=-0.5), [lnrs], [lnrs])
        V(lambda e: e.tensor_scalar(lnnm[:], lnmv[:, 0:1], lnrs[:, 0:1], -1.0, ALU.mult, ALU.mult), [lnmv, lnrs], [lnnm])
        return lnrs, lnnm

    def ln_mod_T(src, r, sh_c, sc_c, dst, dcol, dstf=None):
        rstd, nmr = ln_stats(src, 1e-6)
        A(lambda e: e.activation(xh[:], src[:], AF.Identity, bias=nmr[:, 0:1], scale=rstd[:, 0:1]), [src, rstd, nmr], [xh])
        for hf in range(2):
            pp = PB()
            k.pe([(lambda e, j=j: e.transpose(pp[:, j * 128:(j + 1) * 128], xh[:, (hf * 4 + j) * 128:(hf * 4 + j + 1) * 128], identF[:]))
                  for j in range(4)], [xh, identF], pp)
            for j in range(4):
                kc = hf * 4 + j
                if dstf is not None:
                    A(lambda e: e.activation(dstf[:, kc, :], pp[:, j * 128:(j + 1) * 128], AF.Identity,
                                             bias=mod[:, sh_c + kc, r:r + 1], scale=modp1[:, sc_c + kc, r:r + 1]), [pp, mod, modp1], [dstf])
                elif j % 2 == 0:
                    A(lambda e: e.activation(dst[:, kc, dcol:dcol + 128], pp[:, j * 128:(j + 1) * 128], AF.Identity,
                                             bias=mod[:, sh_c + kc, r:r + 1], scale=modp1[:, sc_c + kc, r:r + 1]), [pp, mod, modp1], [dst])
                else:
                    V(lambda e: e.tensor_scalar(dst[:, kc, dcol:dcol + 128], pp[:, j * 128:(j + 1) * 128],
                                                modp1[:, sc_c + kc, r:r + 1], mod[:, sh_c + kc, r:r + 1], ALU.mult, ALU.add), [pp, mod, modp1], [dst])
        if dstf is not None and dst is not None:
            G(lambda e: e.tensor_copy(dst[:, :, dcol:dcol + 128], dstf[:]), [dstf], [dst])

    def proj_fm(hT, T, nm, j, dstps):
        col = wmap[nm] + j * 128
        k.pe([(lambda e, kc=kc: e.matmul(dstps[:, 0:T], W[:, kc, col:col + 128], hT[:, kc, 0:T], start=(kc == 0), stop=(kc == 7)))
              for kc in range(8)], [W, hT], dstps)

    def proj_tm(hT, ti, nm, ncol, dstps):
        col = wmap[nm]
        k.pe([(lambda e, kc=kc: e.matmul(dstps[:, 0:ncol], hT[:, kc, ti * 128:(ti + 1) * 128], W[:, kc, col:col + ncol],
                                        start=(kc == 0), stop=(kc == 7))) for kc in range(8)], [W, hT], dstps)

    Dpad = [k.sb([128, BT, 4, 2, 64], F32, f"Dpad{d}") for d in range(2)]
    Rref = [k.sb([128, 2 * BT, 4], F32, f"Rref{d}") for d in range(2)]
    for d in range(2):
        V(lambda e: e.memset(Dpad[d][:], 0.0), [], [Dpad[d]])
        V(lambda e: e.memset(Rref[d][:], 0.0), [], [Rref[d]])
    khat = [k.sb([128, BT, 4, 2, 64], BF16, f"khat{h}") for h in range(4)]
    qhat = [k.sb([128, BLK], BF16, f"qhat{h}") for h in range(4)]
    qd = [k.sb([128, BT, 2, 2, 64], BF16, f"qd{h}") for h in range(4)]
    for h in range(4):
        V(lambda e: e.memset(qd[h][:], 0.0), [], [qd[h]])
    kdT = k.sb([128, BT, 4, 128], BF16, "kdT")
    sdec = k.sb([128, 4, 2 * BT], F32, "sdec")
    vtm = k.sb([128, BT, 512], BF16, "vtm")
    Sst = [k.sb([128, 4, 128], F32, f"Sst{d}") for d in range(2)]
    kTs = k.sb([128, N], BF16, "kTs"); kcs = k.sb([128, L], BF16, "kcs")
    vaug = k.sb([128, NT, 2, 66], BF16, "vaug"); vcaug = k.sb([128, L // 128, 2, 66], BF16, "vcaug")
    V(lambda e: e.memset(vaug[:], 1.0), [], [vaug]); V(lambda e: e.memset(vcaug[:], 1.0), [], [vcaug])
    Sbfs = [k.sb([128, 4, 128], BF16, f"Sbf{i}") for i in range(4)]
    sbi = [0]

    def SBF():
        sbi[0] = (sbi[0] + 1) % 4
        return Sbfs[sbi[0]]

    T1 = lambda nm: ring(nm, 1, [128, BLK], F32)

    def hgrn_prep(hT, T, d, dosc=True):
        C = T // 64
        NTl = T // 128
        for h in range(4):
            zf = PB()
            proj_fm(hT, T, "zff" if d == 0 else "zfb", h, zf)
            e1 = T1("hp_e1")
            A(lambda e: e.activation(e1[:, 0:T], zf[:, 0:T], AF.Exp), [zf], [e1])
            rr = T1("hp_rr")
            V(lambda e: e.tensor_scalar(rr[:, 0:T], e1[:, 0:T], 1.0, None, ALU.add), [e1], [rr])
            V(lambda e: e.reciprocal(rr[:, 0:T], rr[:, 0:T]), [rr], [rr])
            g = T1("hp_g")
            A(lambda e: e.activation(g[:, 0:T], rr[:, 0:T], AF.Ln, bias=1.0, scale=noml[:, d, h:h + 1]), [rr, noml], [g])
            G(lambda e: e.tensor_scalar(g[:, 0:T], g[:, 0:T], -4.0, None, ALU.max), [g], [g])
            bcm = T1("hp_bc")
            V(lambda e: e.tensor_tensor_scan(bcm[:, 0:T], r64[:, 0:T], g[:, 0:T], 0.0, ALU.mult, ALU.add), [r64, g], [bcm])
            b3 = bcm[:, 0:T].rearrange("p (c j) -> p c j", j=64)
            A(lambda e: e.activation(sdec[:, h, 0:C], b3[:, :, 63], AF.Exp), [bcm], [sdec])
            dk = T1("hp_dk")
            dk3 = dk[:, 0:T].rearrange("p (c j) -> p c j", j=64)
            if d == 0:
                V(lambda e: e.tensor_tensor(dk3, bc(b3[:, :, 63], 2, [128, C, 64]), b3, ALU.subtract), [bcm], [dk])
            else:
                V(lambda e: e.tensor_tensor(dk[:, 0:T], bcm[:, 0:T], g[:, 0:T], ALU.subtract), [bcm, g], [dk])
            A(lambda e: e.activation(dk[:, 0:T], dk[:, 0:T], AF.Exp), [dk], [dk])
            kd = ring("hp_kd", 1, [128, BLK], BF16)
            V(lambda e: e.scalar_tensor_tensor(kd[:, 0:T], rr[:, 0:T], oml[:, d, h:h + 1], dk[:, 0:T], ALU.mult, ALU.mult), [rr, oml, dk], [kd])
            pk2 = PT()
            k.pe([(lambda e, ti=ti: e.transpose(pk2[:, ti * 128:(ti + 1) * 128], kd[:, ti * 128:(ti + 1) * 128], identB[:])) for ti in range(NTl)],
                 [kd, identB], pk2)
            A(lambda e: e.copy(kdT[:, 0:NTl, h, :], pk2[:, 0:NTl * 128].rearrange("p (t d) -> p t d", d=128)), [pk2], [kdT])
            if not dosc:
                continue
            zq = PB()
            proj_fm(hT, T, "zq", h, zq)
            lcm = T1("hp_lc")
            V(lambda e: e.tensor_tensor_scan(lcm[:, 0:T], r16[:, 0:T], g[:, 0:T], 0.0, ALU.mult, ALU.add), [r16, g], [lcm])
            Pq = bcm
            lq = lcm
            if d == 1:
                u = T1("hp_u")
                u3 = u[:, 0:T].rearrange("p (c j) -> p c j", j=64)
                V(lambda e: e.tensor_tensor(u3, bc(b3[:, :, 63], 2, [128, C, 64]), b3, ALU.subtract), [bcm], [u])
                V(lambda e: e.tensor_tensor(u[:, 0:T], u[:, 0:T], g[:, 0:T], ALU.add), [u, g], [u])
                l4 = lcm[:, 0:T].rearrange("p (c j) -> p c j", j=16)
                ls = T1("hp_ls")
                ls4 = ls[:, 0:T].rearrange("p (c j) -> p c j", j=16)
                V(lambda e: e.tensor_tensor(ls4, bc(l4[:, :, 15], 2, [128, T // 16, 16]), l4, ALU.subtract), [lcm], [ls])
                V(lambda e: e.tensor_tensor(ls[:, 0:T], ls[:, 0:T], g[:, 0:T], ALU.add), [ls, g], [ls])
                Pq = u
                lq = ls
            P3 = Pq[:, 0:T].rearrange("p (c j) -> p c j", j=64)
            if d == 0:
                V(lambda e: e.tensor_copy(Rref[0][:, 0:C, 1:4], P3[:, :, 15:63:16]), [Pq], [Rref[0]])
            else:
                V(lambda e: e.tensor_copy(Rref[1][:, 0:C, 0:3], P3[:, :, 16:64:16]), [Pq], [Rref[1]])
            Rr = Rref[d]
            R4 = Rr[:, 0:C, :].rearrange("p (t c) n -> p t c n", c=2)
            P4 = Pq[:, 0:T].rearrange("p (t c j) -> p t c j", c=2, j=64)
            for n in range(4):
                lo, hi = (0, 16 * (n + 1)) if d == 0 else (16 * n, 64)
                V(lambda e: e.tensor_tensor(Dpad[d][:, 0:NTl, n, :, lo:hi], bc(R4[:, :, :, n], 3, [128, NTl, 2, hi - lo]),
                                            P4[:, :, :, lo:hi], ALU.subtract), [Rr, Pq], [Dpad[d]])
            Ep = ring("hp_Ep", 1, [128, BT, 4, 2, 64], F32)
            A(lambda e: e.activation(Ep[:, 0:NTl].rearrange("p t n c j -> p (t n c j)"),
                                     Dpad[d][:, 0:NTl].rearrange("p t n c j -> p (t n c j)"), AF.Exp), [Dpad[d]], [Ep])
            rr4 = rr[:, 0:T].rearrange("p (t c j) -> p t c j", c=2, j=64)
            for n in range(4):
                V(lambda e: e.scalar_tensor_tensor(khat[h][:, 0:NTl, n, :, :], rr4, oml[:, d, h:h + 1], Ep[:, 0:NTl, n, :, :], ALU.mult, ALU.mult),
                  [rr, oml, Ep], [khat[h]])
            el = T1("hp_el")
            A(lambda e: e.activation(el[:, 0:T], lq[:, 0:T], AF.Exp), [lq], [el])
            V(lambda e: e.tensor_tensor(qhat[h][:, 0:T], zq[:, 0:T], el[:, 0:T], ALU.mult), [zq, el], [qhat[h]])
            eb = T1("hp_eb")
            A(lambda e: e.activation(eb[:, 0:T], Pq[:, 0:T], AF.Exp), [Pq], [eb])
            zq4 = zq[:, 0:T].rearrange("p (t c j) -> p t c j", c=2, j=64)
            eb4 = eb[:, 0:T].rearrange("p (t c j) -> p t c j", c=2, j=64)
            for c in range(2):
                V(lambda e: e.tensor_tensor(qd[h][:, 0:NTl, c, c, :], zq4[:, :, c, :], eb4[:, :, c, :], ALU.mult), [zq, eb], [qd[h]])

    def state_update(d, ti, c, Sbf_next):
        pS = PB()
        lo = c * 64
        k.pe([(lambda e, h=h: e.matmul(pS[:, h * 128:(h + 1) * 128], kdT[lo:lo + 64, ti, h, :], vtm[lo:lo + 64, ti, h * 128:(h + 1) * 128],
                                      start=True, stop=True)) for h in range(4)], [kdT, vtm], pS)
        S = Sst[d]
        ci = ti * 2 + c
        V(lambda e: e.tensor_tensor(S[:], S[:], bc(sdec[:, :, ci], 2, [128, 4, 128]), ALU.mult), [S, sdec], [S])
        V(lambda e: e.tensor_tensor(S[:], S[:], pS[:].rearrange("p (h e) -> p h e", e=128), ALU.add), [S, pS], [S])
        if Sbf_next is not None:
            A(lambda e: e.copy(Sbf_next[:], S[:]), [S], [Sbf_next])

    def hgrn_tile(d, ti, Sbf_in):
        pA = PB()
        fns = []
        for h in range(4):
            for cq in range(2):
                for n in range(4):
                    q0 = ti * 128 + cq * 64 + n * 16
                    c0 = h * 128 + cq * 64 + n * 16
                    fns.append(lambda e, h=h, n=n, q0=q0, c0=c0: e.matmul(
                        pA[:, c0:c0 + 16], khat[h][:, ti, n, :, :].rearrange("p c j -> p (c j)"), qhat[h][:, q0:q0 + 16], start=True, stop=True))
        k.pe(fns, khat + qhat, pA)
        Asb = ring("Asb", 2, [128, 4, 128], BF16)
        mk = maskF if d == 0 else maskB
        V(lambda e: e.tensor_tensor(Asb[:], pA[:].rearrange("p (h t) -> p h t", t=128), bc(mk[:], 1, [128, 4, 128]), ALU.mult), [pA, mk], [Asb])
        order = (0, 1) if d == 0 else (1, 0)
        Smid = SBF()
        state_update(d, ti, order[0], Smid)
        Sout = SBF()
        state_update(d, ti, order[1], Sout)
        Sin = {order[0]: Sbf_in, order[1]: Smid}
        pO = PB()
        fns = []
        for h in range(4):
            o_ap = pO[:, h * 128:(h + 1) * 128]
            fns.append(lambda e, h=h, o_ap=o_ap: e.matmul(o_ap, Asb[:, h, :], vtm[:, ti, h * 128:(h + 1) * 128], start=True, stop=False))
            for ci, c in enumerate((0, 1)):
                fns.append(lambda e, h=h, c=c, o_ap=o_ap, ci=ci: e.matmul(o_ap, qd[h][:, ti, c, :, :].rearrange("p a j -> p (a j)"),
                                                                      Sin[c][:, h, :], start=False, stop=(ci == 1)))
        k.pe(fns, [Asb, vtm, Sbf_in, Smid] + qd, pO)
        return pO, Sout

    def load_block_hT(src3, b, r, t0, T, keep=None):
        hT = ring("hT", 1, [128, 8, BLK], BF16)
        for ti in range(T // 128):
            xt = XT()
            k.dma("sp", xt[:], src3[b, t0 + ti * 128:t0 + (ti + 1) * 128, :], writes=[xt])
            if keep is not None:
                keep.append(xt)
            ln_mod_T(xt, r, 0, 8, hT, ti * 128)
        return hT

    def v_block(hT, T):
        for ti in range(T // 128):
            pz = PB()
            proj_tm(hT, ti, "zi", 512, pz)
            A(lambda e: e.copy(vtm[:, ti, :], pz[:]), [pz], [vtm])

    def rope_to(dst_ap, dst_buf, pa, pb_, t0, T):
        cs = ring("cs", 1, [128, 2, BLK], F32)
        k.dma("act", cs[:, 0, 0:T], cosT[:, t0:t0 + T], writes=[cs])
        k.dma("act", cs[:, 1, 0:T], sinT[:, t0:t0 + T], writes=[cs])
        t1 = T1("rp1"); t2 = T1("rp2")
        V(lambda e: e.tensor_tensor(t1[:, 0:T], pa[:, 0:T], cs[:, 0, 0:T], ALU.mult), [pa, cs], [t1])
        V(lambda e: e.tensor_tensor(t2[:, 0:T], pb_[:, 0:T], cs[:, 1, 0:T], ALU.mult), [pb_, cs], [t2])
        G(lambda e: e.tensor_tensor(dst_ap, t1[:, 0:T], t2[:, 0:T], ALU.add), [t1, t2], [dst_buf])

    for b in range(NB):
        loadw(["zff", "zfb", "ak"], ["zi", "av"])
        hTc = load_block_hT(ctx, b, NB, 0, L)
        v_block(hTc, L)
        pk_ = PB()
        proj_fm(hTc, L, "ak", 0, pk_)
        A(lambda e: e.copy(kcs[:, 0:L], pk_[:, 0:L]), [pk_], [kcs])
        for ti in range(L // 128):
            pv = PB()
            proj_tm(hTc, ti, "av", 128, pv)
            V(lambda e: e.tensor_copy(vcaug[:, ti, :, 0:64], pv[:, 0:128].rearrange("p (g d) -> p g d", d=64)), [pv], [vcaug])
        for d in range(2):
            hgrn_prep(hTc, L, d, dosc=False)
            V(lambda e: e.memset(Sst[d][:], 0.0), [], [Sst[d]])
            tiles = list(range(L // 128)) if d == 0 else list(reversed(range(L // 128)))
            for ti in tiles:
                for c in ((0, 1) if d == 0 else (1, 0)):
                    state_update(d, ti, c, None)
        loadw(["zq", "zfb", "ak", "akp"], ["zi", "av"])
        Sbf = SBF()
        A(lambda e: e.copy(Sbf[:], Sst[1][:]), [Sst[1]], [Sbf])
        for blk in reversed(range(NBLK)):
            t0 = blk * BLK
            background_dmas(-(-len(bg_list) // (2 * NBLK)))
            hT = load_block_hT(x, b, b, t0, BLK)
            v_block(hT, BLK)
            pk_ = PB(); pkp = PB()
            proj_fm(hT, BLK, "ak", 0, pk_)
            proj_fm(hT, BLK, "akp", 0, pkp)
            rope_to(kTs[:, t0:t0 + BLK], kTs, pk_, pkp, t0, BLK)
            for ti in range(BT):
                pv = PB()
                proj_tm(hT, ti, "av", 128, pv)
                V(lambda e: e.tensor_copy(vaug[:, blk * BT + ti, :, 0:64], pv[:, 0:128].rearrange("p (g d) -> p g d", d=64)), [pv], [vaug])
            hgrn_prep(hT, BLK, 1)
            for ti in reversed(range(BT)):
                pO, Sbf = hgrn_tile(1, ti, Sbf)
                obt = ring("obt", 1, [128, 512], F32)
                A(lambda e: e.copy(obt[:], pO[:]), [pO], [obt])
                gt = blk * BT + ti
                k.dma("sp", obs[b, gt * 128:(gt + 1) * 128, :], obt[:], reads=[obt])
        k.barrier()
        loadw(["zq", "zff", "aq", "aqp"], ["zi", "zg"], with_wo=b)
        Sbf = SBF()
        A(lambda e: e.copy(Sbf[:], Sst[0][:]), [Sst[0]], [Sbf])
        for blk in range(NBLK):
            t0 = blk * BLK
            background_dmas(-(-len(bg_list) // (2 * NBLK)))
            xts = []
            hT = load_block_hT(x, b, b, t0, BLK, keep=xts)
            v_block(hT, BLK)
            qT = ring("qT", 1, [128, 4, BLK], BF16)
            for j in range(4):
                pq = PB(); pqp = PB()
                proj_fm(hT, BLK, "aq", j, pq)
                proj_fm(hT, BLK, "aqp", j, pqp)
                rope_to(qT[:, j, :], qT, pq, pqp, t0, BLK)
            hgrn_prep(hT, BLK, 0)
            for ti in range(BT):
                gt = blk * BT + ti
                ylat = ring("ylat", 1, [128, D], BF16)
                obt = ring("obt", 1, [128, 512], F32)
                k.dma("act", obt[:], obs[b, gt * 128:(gt + 1) * 128, :], writes=[obt])
                pO, Sbf = hgrn_tile(0, ti, Sbf)
                o = ring("ro_o", 1, [128, 512], F32)
                V(lambda e: e.tensor_tensor(o[:], pO[:], obt[:], ALU.add), [pO, obt], [o])
                sq = ring("ro_sq", 1, [128, 512], F32)
                G(lambda e: e.tensor_tensor(sq[:], o[:], o[:], ALU.mult), [o], [sq])
                ss = ring("ro_ss", 1, [128, 4], F32)
                V(lambda e: e.reduce_sum(ss[:], sq[:].rearrange("p (h e) -> p h e", e=128), AX.X), [sq], [ss])
                A(lambda e: e.activation(ss[:], ss[:], AF.Ln, bias=1e-6, scale=1.0 / 128.0), [ss], [ss])
                A(lambda e: e.activation(ss[:], ss[:], AF.Exp, scale=-0.5), [ss], [ss])
                V(lambda e: e.tensor_tensor(o[:].rearrange("p (h e) -> p h e", e=128), o[:].rearrange("p (h e) -> p h e", e=128),
                                            bc(ss[:], 2, [128, 4, 128]), ALU.mult), [o, ss], [o])
                G(lambda e: e.tensor_tensor(o[:], o[:], normw[:], ALU.mult), [o, normw], [o])
                pg = PB()
                proj_tm(hT, ti, "zg", 512, pg)
                sg = ring("ro_sg", 1, [128, 512], F32)
                A(lambda e: e.activation(sg[:], pg[:], AF.Exp, scale=-1.0), [pg], [sg])
                V(lambda e: e.tensor_scalar(sg[:], sg[:], 1.0, None, ALU.add), [sg], [sg])
                V(lambda e: e.reciprocal(sg[:], sg[:]), [sg], [sg])
                V(lambda e: e.tensor_tensor(sg[:], sg[:], pg[:], ALU.mult), [sg, pg], [sg])
                V(lambda e: e.tensor_tensor(ylat[:, 0:512], o[:], sg[:], ALU.mult), [o, sg], [ylat])
                for kh in range(2):
                    po = kh * 64
                    kts = [("c", i) for i in range(L // 128)]
                    if gt > 0:
                        kts.append(("p", gt - 1))
                    kts.append(("s", gt))
                    if gt < NT - 1:
                        kts.append(("n", gt + 1))
                    Pl = []
                    for ki, (kind, kt) in enumerate(kts):
                        pS = PB()
                        kap = kcs[po:po + 64, kt * 128:(kt + 1) * 128] if kind == "c" else kTs[po:po + 64, kt * 128:(kt + 1) * 128]
                        qap = qT[po:po + 64, :, ti * 128:(ti + 1) * 128]
                        msk = kind in ("p", "n")
                        pS3 = pS[:].rearrange("p (h q) -> p h q", q=128)
                        fns = [lambda e, kap=kap, qap=qap, msk=msk, pS3=pS3: e.matmul(pS3, kap, qap, start=True, stop=not msk)]
                        if msk:
                            nm = negP if kind == "p" else negN
                            fns.append(lambda e, nm=nm, pS3=pS3: e.matmul(pS3, identB[:], bc(nm[:], 1, [128, 4, 128]), start=False, stop=True))
                        k.pe(fns, [kcs, kTs, qT, identB, negP, negN], pS)
                        Psb = ring("Psb", 5, [128, 512], BF16)
                        A(lambda e: e.activation(Psb[:], pS[:], AF.Exp, scale=0.125), [pS], [Psb])
                        Pl.append((Psb, vcaug if kind == "c" else vaug, kt))
                    pOa = PB()
                    fns = []
                    for hq in range(4):
                        for ki, (Psb, va, kt) in enumerate(Pl):
                            fns.append(lambda e, hq=hq, Psb=Psb, va=va, kt=kt, ki=ki: e.matmul(
                                pOa[:, hq * 80:hq * 80 + 65], Psb[:, hq * 128:(hq + 1) * 128], va[:, kt, kh, 0:65],
                                start=(ki == 0), stop=(ki == len(Pl) - 1)))
                    k.pe(fns, [p[0] for p in Pl] + [vaug, vcaug], pOa)
                    den = ring("at_den", 1, [128, 4], F32)
                    pO4 = pOa[:, 0:320].rearrange("p (h c) -> p h c", c=80)
                    V(lambda e: e.tensor_tensor(den[:], pO4[:, :, 64], esink[:, kh * 4:(kh + 1) * 4], ALU.add), [pOa, esink], [den])
                    V(lambda e: e.reciprocal(den[:], den[:]), [den], [den])
                    V(lambda e: e.tensor_tensor(ylat[:, 512 + kh * 256:512 + (kh + 1) * 256].rearrange("p (h c) -> p h c", c=64), pO4[:, :, 0:64],
                                                bc(den[:], 2, [128, 4, 64]), ALU.mult), [pOa, den], [ylat])
                pt = PT()
                k.pe([(lambda e, kc=kc: e.transpose(pt[:, kc * 128:(kc + 1) * 128], ylat[:, kc * 128:(kc + 1) * 128], identB[:])) for kc in range(8)],
                     [ylat, identB], pt)
                yT = ring("yT", 1, [128, 8, 128], BF16)
                A(lambda e: e.copy(yT[:], pt[:].rearrange("p (c t) -> p c t", t=128)), [pt], [yT])
                xt = xts[ti]
                wo = wmap["wo"]
                for hf in range(2):
                    pp = PB()
                    k.pe([(lambda e, kc=kc: e.matmul(pp[:], yT[:, kc, :], W[:, kc, wo + hf * 512:wo + (hf + 1) * 512], start=(kc == 0), stop=(kc == 7)))
                          for kc in range(8)], [yT, W], pp)
                    V(lambda e: e.scalar_tensor_tensor(u1[:, hf * 512:(hf + 1) * 512], xt[:, hf * 512:(hf + 1) * 512], ALPHA, pp[:], ALU.mult, ALU.add),
                      [xt, pp], [u1])
                rstd, nmr = ln_stats(u1, 1e-5)
                x1 = X1()
                A(lambda e: e.activation(x1[:], u1[:], AF.Identity, bias=nmr[:, 0:1], scale=rstd[:, 0:1]), [u1, rstd, nmr], [x1])
                G(lambda e: e.tensor_tensor(x1[:], x1[:], ln1w[:], ALU.mult), [x1, ln1w], [x1])
                G(lambda e: e.tensor_tensor(x1[:], x1[:], ln1b[:], ALU.add), [x1, ln1b], [x1])
                k.dma("sp", x1s[b, gt * 128:(gt + 1) * 128, :], x1[:], reads=[x1], stream=x1)
        k.barrier()
    k.scope.close()

    background_dmas(len(bg_list))
    for j in range(NTL):
        k.dma("pool", Xs[j * 512:(j + 1) * 512, :], zrows, reads=[ztok], stream=ztok)
    I32 = mybir.dt.int32
    moe_scope = contextlib.ExitStack()
    k.scope = moe_scope
    NEX = NE + 1
    ln2w = const(ln2w_b, [128, D], name="ln2w"); ln2b = const(ln2b_b, [128, D], name="ln2b")
    rowsb = [k.sb([128, D], F32, f"rowsb{i}") for i in range(3)]
    rows_cur = [None]

    def load_rows(b):
        if rows_cur[0] != b:
            for i in range(3):
                k.dma("sp", rowsb[i][:], rws[b, i], writes=[rowsb[i]])
            rows_cur[0] = b

    slall = k.sb([128, TOT, NA], I32, "slall")
    gjall = k.sb([128, TOT, NA], F32, "gjall")
    idxw = k.sb([128, NTL], I32, "idxw")
    sub = contextlib.ExitStack()
    k.scope = sub
    rbias = const(rbias_b, [128, NE], name="rbias")
    ramp = const(ramp_d, [128, 256], name="ramp"); jv = const(jv_d, [128, NA], name="jv"); pcol = const(pcol_d, [128, 1], name="pcol")
    ustr = const(ustrict_d, [128, 128], BF16, name="ustr")
    onesB = k.sb([128, 128], BF16, "onesB")
    V(lambda e: e.memset(onesB[:], 1.0), [], [onesB])
    rw = k.sb([128, 8, NE], F32, "rw")
    k.dma("sp", rw[:], router_w.rearrange("(kc p) n -> p kc n", p=128), writes=[rw])
    emask = k.sb([128, TOT, NEX], F32, "emask")
    gall = k.sb([128, TOT, NEX], F32, "gall")
    V(lambda e: e.memset(emask[:], 1.0), [], [emask])
    V(lambda e: e.memset(gall[:], 1.0), [], [gall])
    cnt = k.sb([128, NEX], F32, "cnt")
    V(lambda e: e.memset(cnt[:], 0.0), [], [cnt])
    RT = lambda nm, w: ring(nm, 1, [128, w], F32)

    for gti in range(TOT):
        b = gti // NT
        ti = gti % NT
        load_rows(b)
        xt = XT()
        k.dma("sp", xt[:], x1s[b, ti * 128:(ti + 1) * 128, :], writes=[xt])
        hf32 = ring("h2f", 1, [128, 8, 128], F32)
        ln_mod_T(xt, b, 24, 32, None, 0, dstf=hf32)
        h2b = ring("h2b", 2, [128, D], BF16)
        V(lambda e: e.tensor_tensor(u1[:], xh[:], rowsb[1][:], ALU.mult), [xh, rowsb[1]], [u1])
        V(lambda e: e.tensor_tensor(h2b[:], u1[:], rowsb[0][:], ALU.add), [u1, rowsb[0]], [h2b])
        k.dma("sp", H2[gti * 128:(gti + 1) * 128, :], h2b[:], reads=[h2b])
        pr = PB()
        k.pe([(lambda e, kc=kc: e.matmul(pr[:, 0:NE], hf32[:, kc, :], rw[:, kc, :], start=(kc == 0), stop=(kc == 7))) for kc in range(8)], [hf32, rw], pr)
        if DEBUG and gti == 0:
            dbg_h = nc.dram_tensor("dbg_h", [128, 8, 128], F32, kind="ExternalOutput").ap()
            dbg_lg = nc.dram_tensor("dbg_lg", [128, NE], F32, kind="ExternalOutput").ap()
            k.dma("sp", dbg_h, hf32[:], reads=[hf32])
            lgs = k.sb([128, NE], F32, "lgs")
            V(lambda e: e.tensor_copy(lgs[:], pr[:, 0:NE]), [pr], [lgs])
            k.dma("sp", dbg_lg, lgs[:], reads=[lgs])
        sc = RT("rt_sc", NE)
        A(lambda e: e.activation(sc[:], pr[:, 0:NE], AF.Exp, scale=-1.0), [pr], [sc])
        V(lambda e: e.tensor_scalar(sc[:], sc[:], 1.0, None, ALU.add), [sc], [sc])
        V(lambda e: e.reciprocal(sc[:], sc[:]), [sc], [sc])
        sel = RT("rt_sel", NE)
        V(lambda e: e.tensor_tensor(sel[:], sc[:], rbias[:], ALU.add), [sc, rbias], [sel])
        sel3 = sel[:].rearrange("p (g i) -> p g i", i=8)
        m1 = RT("rt_m1", 8); m2 = RT("rt_m2", 8)
        V(lambda e: e.reduce_max(m1[:], sel3, AX.X), [sel], [m1])
        eq = RT("rt_eq", NE)
        eq3 = eq[:].rearrange("p (g i) -> p g i", i=8)
        V(lambda e: e.tensor_tensor(eq3, sel3, bc(m1[:], 2, [128, 8, 8]), ALU.is_ge), [sel, m1], [eq])
        V(lambda e: e.scalar_tensor_tensor(eq[:], eq[:], -1e9, sel[:], ALU.mult, ALU.add), [eq, sel], [eq])
        V(lambda e: e.reduce_max(m2[:], eq3, AX.X), [eq], [m2])
        V(lambda e: e.tensor_tensor(m1[:], m1[:], m2[:], ALU.add), [m1, m2], [m1])
        top8 = RT("rt_t8", 8)
        V(lambda e: e.max(top8[:], m1[:]), [m1], [top8])
        gm = RT("rt_gm", 8); gneg = RT("rt_gn", 8)
        V(lambda e: e.tensor_scalar(gm[:], m1[:], top8[:, 3:4], None, ALU.is_ge), [m1, top8], [gm])
        V(lambda e: e.tensor_scalar(gneg[:], gm[:], 1e9, -1e9, ALU.mult, ALU.add), [gm], [gneg])
        V(lambda e: e.tensor_tensor(eq3, sel3, bc(gm[:], 2, [128, 8, 8]), ALU.mult), [sel, gm], [eq])
        V(lambda e: e.tensor_tensor(eq3, eq3, bc(gneg[:], 2, [128, 8, 8]), ALU.add), [eq, gneg], [eq])
        t8b = RT("rt_t8b", 8)
        V(lambda e: e.max(t8b[:], eq[:]), [eq], [t8b])
        V(lambda e: e.tensor_scalar(emask[:, gti, 0:NE], eq[:], t8b[:, 7:8], None, ALU.is_ge), [eq, t8b], [emask])
        V(lambda e: e.tensor_tensor(eq[:], emask[:, gti, 0:NE], sc[:], ALU.mult), [emask, sc], [eq])
        ws = RT("rt_ws", 1)
        V(lambda e: e.reduce_sum(ws[:], eq[:], AX.X), [eq], [ws])
        V(lambda e: e.reciprocal(ws[:], ws[:]), [ws], [ws])
        V(lambda e: e.tensor_scalar(gall[:, gti, 0:NE], eq[:], ws[:, 0:1], 2.5, ALU.mult, ALU.mult), [eq, ws], [gall])
        emb = ring("emb", 2, [128, NEX], BF16)
        V(lambda e: e.tensor_copy(emb[:], emask[:, gti, :]), [emask], [emb])
        pc = PB()
        k.pe([lambda e: e.matmul(pc[:, 0:NEX], onesB[:], emb[:], start=True, stop=True)], [onesB, emb], pc)
        V(lambda e: e.tensor_tensor(cnt[:], cnt[:], pc[:, 0:NEX], ALU.add), [cnt, pc], [cnt])

    MT = (TOT * 128) // 512 + 1
    assert MT <= 256 and NTL <= 256
    cmpb = k.sb([128, NEX, MT], F32, "cmpb")
    V(lambda e: e.tensor_tensor(cmpb[:], bc(cnt[:], 2, [128, NEX, MT]), bc(ramp[:, 0:MT], 1, [128, NEX, MT]), ALU.is_gt), [cnt, ramp], [cmpb])
    pcn = k.sb([128, NEX], F32, "pcn")
    V(lambda e: e.reduce_sum(pcn[:], cmpb[:], AX.X), [cmpb], [pcn])
    V(lambda e: e.tensor_scalar(pcn[:], pcn[:], 512.0, None, ALU.mult), [pcn], [pcn])
    ends = k.sb([128, NEX], F32, "ends"); starts = k.sb([128, NEX], F32, "starts")
    onesf = k.sb([128, NEX], F32, "onesf")
    V(lambda e: e.memset(onesf[:], 1.0), [], [onesf])
    V(lambda e: e.tensor_tensor_scan(ends[:], onesf[:], pcn[:], 0.0, ALU.mult, ALU.add), [onesf, pcn], [ends])
    V(lambda e: e.tensor_tensor(starts[:], ends[:], pcn[:], ALU.subtract), [ends, pcn], [starts])
    ej = k.sb([128, NTL], F32, "ej")
    JC = 16
    cmp2 = k.sb([128, JC, NEX], F32, "cmp2")
    for j0 in range(0, NTL, JC):
        jn = min(JC, NTL - j0)
        V(lambda e: e.tensor_tensor(cmp2[:, 0:jn, :], bc(ends[:], 1, [128, jn, NEX]), bc(ramp[:, j0:j0 + jn], 2, [128, jn, NEX]), ALU.is_le),
          [ends, ramp], [cmp2])
        V(lambda e: e.reduce_sum(ej[:, j0:j0 + jn], cmp2[:, 0:jn, :], AX.X), [cmp2], [ej])
    V(lambda e: e.tensor_scalar(ej[:], ej[:], float(NE), 128.0, ALU.min, ALU.mult), [ej], [ej])
    V(lambda e: e.tensor_scalar(ej[:], ej[:], pcol[:, 0:1], None, ALU.add), [ej, pcol], [ej])
    V(lambda e: e.tensor_copy(idxw[:], ej[:]), [ej], [idxw])

    k.barrier()
    offs = k.sb([128, NEX], F32, "offs")
    V(lambda e: e.tensor_copy(offs[:], starts[:]), [starts], [offs])
    sct = k.sb([1, 2], F32, "sct")
    for gti in range(TOT):
        emb = ring("emb", 2, [128, NEX], BF16)
        V(lambda e: e.tensor_copy(emb[:], emask[:, gti, :]), [emask], [emb])
        pc = PB()
        k.pe([lambda e: e.matmul(pc[:, 0:NEX], ustr[:], emb[:], start=True, stop=True),
              lambda e: e.matmul(pc[:, 128:128 + NEX], onesB[:], emb[:], start=True, stop=True)], [ustr, onesB, emb], pc)
        slot = RT("p2_slot", NEX)
        V(lambda e: e.tensor_tensor(slot[:], pc[:, 0:NEX], offs[:], ALU.add), [pc, offs], [slot])
        V(lambda e: e.tensor_tensor(offs[:], offs[:], pc[:, 128:128 + NEX], ALU.add), [offs, pc], [offs])
        ks = RT("p2_ks", NEX)
        V(lambda e: e.tensor_tensor_scan(ks[:], onesf[:], emask[:, gti, :], 0.0, ALU.mult, ALU.add), [onesf, emask], [ks])
        s3 = ring("p2_s3", 1, [128, NA, NEX], F32)
        V(lambda e: e.tensor_tensor(s3[:], bc(ks[:], 1, [128, NA, NEX]), bc(jv[:], 2, [128, NA, NEX]), ALU.is_equal), [ks, jv], [s3])
        V(lambda e: e.tensor_tensor(s3[:], s3[:], bc(emask[:, gti, :], 1, [128, NA, NEX]), ALU.mult), [s3, emask], [s3])
        t3 = ring("p2_t3", 1, [128, NA, NEX], F32)
        V(lambda e: e.tensor_tensor(t3[:], s3[:], bc(slot[:], 1, [128, NA, NEX]), ALU.mult), [s3, slot], [t3])
        slf = RT("p2_slf", NA)
        V(lambda e: e.reduce_sum(slf[:], t3[:], AX.X), [t3], [slf])
        V(lambda e: e.tensor_copy(slall[:, gti, :], slf[:]), [slf], [slall])
        V(lambda e: e.tensor_tensor(t3[:], s3[:], bc(gall[:, gti, :], 1, [128, NA, NEX]), ALU.mult), [s3, gall], [t3])
        V(lambda e: e.reduce_sum(gjall[:, gti, :], t3[:], AX.X), [t3], [gjall])
        h2s = ring("h2s", 2, [128, D], BF16)
        k.dma("sp", h2s[:], H2[gti * 128:(gti + 1) * 128, :], writes=[h2s])
        for j in range(NA):
            k.idma(Xs[:, :], bass.IndirectOffsetOnAxis(ap=slall[:, gti, j:j + 1], axis=0), h2s[:, :], None, reads=[h2s, slall], stream=sct)
    k.barrier()
    sub.close()
    k.scope = moe_scope

    if DEBUG:
        dbg_sl = nc.dram_tensor("dbg_sl", [128, TOT, NA], I32, kind="ExternalOutput").ap()
        dbg_gj = nc.dram_tensor("dbg_gj", [128, TOT, NA], F32, kind="ExternalOutput").ap()
        dbg_iw = nc.dram_tensor("dbg_iw", [128, NTL], I32, kind="ExternalOutput").ap()
        k.dma("sp", dbg_sl, slall[:], reads=[slall]); k.dma("sp", dbg_gj, gjall[:], reads=[gjall]); k.dma("sp", dbg_iw, idxw[:], reads=[idxw])
    wbfs = [k.sb([128, 6144], BF16, f"wbf{i}") for i in range(2)]
    wsts = [k.sb([128, 6144], F32, f"wst{i}") for i in range(2)]
    NSUB = NTL * 4
    stt = {}
    wcur = {}

    def w_gather(j):
        wst = wsts[j % 2]
        k.idma(wst[:, :], None, ew_all[:, :], bass.IndirectOffsetOnAxis(ap=idxw[:, j:j + 1], axis=0), reads=[idxw], writes=[wst], stream=wst)
        xst = ring("xst", 3, [128, 4, D], BF16)
        k.dma("sp", xst[:], Xs[j * 512:(j + 1) * 512, :].rearrange("(s p) d -> p s d", p=128), writes=[xst])
        wcur[j] = [None, xst]

    def w_cast(j):
        wst = wsts[j % 2]
        wbf = wbfs[j % 2]
        V(lambda e: e.tensor_copy(wbf[:, 0:2048], wst[:, 0:2048]), [wst], [wbf])
        A(lambda e: e.copy(wbf[:, 2048:4096], wst[:, 2048:4096]), [wst], [wbf])
        V(lambda e: e.tensor_copy(wbf[:, 4096:6144], wst[:, 4096:6144]), [wst], [wbf])
        wcur[j][0] = wbf

    def stA(i):
        j, s_ = divmod(i, 4)
        if s_ == 0 and j == 0:
            w_gather(0)
            if NTL > 1:
                w_gather(1)
            w_cast(0)
        if s_ == 2:
            if j + 1 < NTL:
                w_cast(j + 1)
            if j + 2 < NTL:
                w_gather(j + 2)
        wbf, xst = wcur[j]
        pt = PT()
        k.pe([(lambda e, kc=kc: e.transpose(pt[:, kc * 128:(kc + 1) * 128], xst[:, s_, kc * 128:(kc + 1) * 128], identB[:])) for kc in range(8)], [xst, identB], pt)
        hsT = ring("hsT", 3, [128, 8, 128], BF16)
        A(lambda e: e.copy(hsT[:], pt[:].rearrange("p (c t) -> p c t", t=128)), [pt], [hsT])
        p1 = PB()
        k.pe([(lambda e, kc=kc: e.matmul(p1[:], hsT[:, kc, :], wbf[:, kc * 512:(kc + 1) * 512], start=(kc == 0), stop=(kc == 7))) for kc in range(8)],
             [hsT, wbf], p1)
        sg = ring("ex_sg", 4, [128, 256], F32)
        A(lambda e: e.activation(sg[:], p1[:, 0:256], AF.Silu), [p1], [sg])
        h1 = ring("ex_h1", 4, [128, 256], BF16)
        V(lambda e: e.tensor_tensor(h1[:], p1[:, 256:512], sg[:], ALU.mult), [p1, sg], [h1])
        stt[i] = [h1, None]

    def stB(i):
        h1 = stt[i][0]
        pt = PT()
        k.pe([(lambda e, fc=fc: e.transpose(pt[:, fc * 128:(fc + 1) * 128], h1[:, fc * 128:(fc + 1) * 128], identB[:])) for fc in range(2)], [h1, identB], pt)
        h1T = ring("ex_h1T", 4, [128, 2, 128], BF16)
        A(lambda e: e.copy(h1T[:], pt[:, 0:256].rearrange("p (c t) -> p c t", t=128)), [pt], [h1T])
        stt[i][1] = h1T

    def stC(i):
        j, s_ = divmod(i, 4)
        wbf, xst = wcur[j]
        h1T = stt[i][1]
        ysb = ring("ysb", 3, [128, D], BF16)
        for hf in range(2):
            p2 = PB()
            k.pe([(lambda e, fc=fc: e.matmul(p2[:], h1T[:, fc, :], wbf[:, 4096 + fc * 1024 + hf * 512:4096 + fc * 1024 + (hf + 1) * 512],
                                            start=(fc == 0), stop=(fc == 1))) for fc in range(2)], [h1T, wbf], p2)
            V(lambda e: e.tensor_copy(ysb[:, hf * 512:(hf + 1) * 512], p2[:]), [p2], [ysb])
        k.dma("sp", Ys[i * 128:(i + 1) * 128, :], ysb[:], reads=[ysb])
        del stt[i]

    for i in range(NSUB + 2):
        if i < NSUB:
            stA(i)
        if 0 <= i - 1 < NSUB:
            stB(i - 1)
        if 0 <= i - 2 < NSUB:
            stC(i - 2)
    k.barrier()

    ybs = {}

    def gath(g_):
        yb_ = ring("ybuf", 2, [128, NA, D], BF16)
        for j in range(NA):
            k.idma(yb_[:, j, :], None, Ys[:, :], bass.IndirectOffsetOnAxis(ap=slall[:, g_, j:j + 1], axis=0), reads=[slall], writes=[yb_], stream=yb_,
                   nowaw=(j > 0))
        ybs[g_] = yb_

    gath(0)
    for gti in range(TOT):
        b = gti // NT
        ti = gti % NT
        load_rows(b)
        if gti + 1 < TOT:
            gath(gti + 1)
        yb = ybs.pop(gti)
        xt = XT()
        k.dma("sp", xt[:], x1s[b, ti * 128:(ti + 1) * 128, :], writes=[xt])
        V(lambda e: e.tensor_scalar(xh[:], yb[:, 0, :], gjall[:, gti, 0:1], None, ALU.mult), [yb, gjall], [xh])
        for j in range(1, NA):
            V(lambda e: e.scalar_tensor_tensor(xh[:], yb[:, j, :], gjall[:, gti, j:j + 1], xh[:], ALU.mult, ALU.add), [yb, gjall, xh], [xh])
        V(lambda e: e.tensor_tensor(u1[:], xh[:], rowsb[2][:], ALU.mult), [xh, rowsb[2]], [u1])
        V(lambda e: e.scalar_tensor_tensor(u1[:], xt[:], ALPHA, u1[:], ALU.mult, ALU.add), [xt, u1], [u1])
        rstd, nmr = ln_stats(u1, 1e-5)
        x2 = X1()
        A(lambda e: e.activation(x2[:], u1[:], AF.Identity, bias=nmr[:, 0:1], scale=rstd[:, 0:1]), [u1, rstd, nmr], [x2])
        V(lambda e: e.tensor_tensor(x2[:], x2[:], ln2w[:], ALU.mult), [x2, ln2w], [x2])
        V(lambda e: e.tensor_tensor(x2[:], x2[:], ln2b[:], ALU.add), [x2, ln2b], [x2])
        k.dma("sp", out[b, ti * 128:(ti + 1) * 128, :], x2[:], reads=[x2], stream=x2)
    k.barrier()
    moe_scope.close()
    build.ninstr = k.ninstr
    return nc


def host_constants(N, R):
    GRID_W = 64
    rows = N // GRID_W
    row = np.repeat(np.arange(rows), GRID_W).astype(np.float32)
    col = np.tile(np.arange(GRID_W), rows).astype(np.float32)
    freqs = (10000.0 ** (-np.arange(16, dtype=np.float32) / 16)).astype(np.float32)
    ar = row[None, :] * freqs[:, None]
    ac = col[None, :] * freqs[:, None]
    ang = np.concatenate([ar, ar, ac, ac], axis=0)
    sign = np.concatenate([-np.ones(16), np.ones(16), -np.ones(16), np.ones(16)]).astype(np.float32)[:, None]
    cos64 = np.cos(ang).astype(np.float32)
    sin64 = (np.sin(ang) * sign).astype(np.float32)
    cosT = np.concatenate([cos64, cos64], 0)
    sinT = np.concatenate([sin64, sin64], 0)
    j = np.arange(128)[:, None]
    t = np.arange(128)[None, :]
    same = (j // 64) == (t // 64)
    mask_f = (same & (j <= t)).astype(np.float32)
    mask_b = (same & (j >= t)).astype(np.float32)
    negm_prev = np.where(j >= t, 0.0, NEG).astype(np.float32)
    negm_next = np.where(j <= t, 0.0, NEG).astype(np.float32)
    rst64 = np.ones((128, 512), np.float32); rst64[:, ::64] = 0
    rst16 = np.ones((128, 512), np.float32); rst16[:, ::16] = 0
    sel_rows = np.zeros((R, R, 128), np.float32)
    for r in range(R):
        sel_rows[r, r, :] = 1.0
    import ml_dtypes
    extra = dict(zrows=np.zeros((512, D), ml_dtypes.bfloat16),
                 ramp=np.ascontiguousarray(np.broadcast_to((512.0 * np.arange(256, dtype=np.float32))[None, :], (128, 256))),
                 jv=np.ascontiguousarray(np.broadcast_to(np.arange(1, 10, dtype=np.float32)[None, :], (128, 9))),
                 pcol=np.arange(128, dtype=np.float32).reshape(128, 1),
                 ustrict=(np.arange(128)[:, None] < np.arange(128)[None, :]).astype(np.float32))
    return dict(**extra, ident_f=np.eye(128, dtype=np.float32), cosT=np.ascontiguousarray(cosT), sinT=np.ascontiguousarray(sinT),
                mask_f=mask_f, mask_b=mask_b, negm_prev=negm_prev, negm_next=negm_next, rst64=rst64, rst16=rst16, sel_rows=sel_rows)


def rope_perm():
    p = np.arange(64)
    p[0:16] += 16
    p[16:32] -= 16
    p[32:48] += 16
    p[48:64] -= 16
    return p


def expert_weight_rows(inp):
    g = np.concatenate([np.asarray(inp["exp_w_gate"][0]), np.asarray(inp["shared_w_gate"])], 0)
    u = np.concatenate([np.asarray(inp["exp_w_up"][0]), np.asarray(inp["shared_w_up"])], 0)
    d = np.concatenate([np.asarray(inp["exp_w_down"][0]), np.asarray(inp["shared_w_down"])], 0)
    ne = g.shape[0]
    g4 = g.reshape(ne, 8, 128, 256).transpose(0, 2, 1, 3)
    u4 = u.reshape(ne, 8, 128, 256).transpose(0, 2, 1, 3)
    gu = np.concatenate([g4, u4], axis=3).reshape(ne, 128, 4096)
    d3 = d.reshape(ne, 2, 128, 1024).transpose(0, 2, 1, 3).reshape(ne, 128, 2048)
    return np.ascontiguousarray(np.concatenate([gu, d3], axis=2).reshape(ne * 128, 6144))


def shared_inputs(inp):
    w_in = np.asarray(inp["w_in"][0])
    zq, zff, zfb, zi, zg = [w_in[:, i * 512:(i + 1) * 512] for i in range(5)]
    aq = w_in[:, 2560:3072]; ak = w_in[:, 3072:3200]; av = w_in[:, 3200:3328]
    perm = rope_perm()
    hord = [0, 4, 1, 5, 2, 6, 3, 7]
    aq8 = aq.reshape(D, 8, 64)
    aqo = aq8[:, hord, :].reshape(D, 512)
    aqp = aq8[:, :, perm][:, hord, :].reshape(D, 512)
    akp = ak.reshape(D, 2, 64)[:, :, perm].reshape(D, 128)
    w_fm = np.ascontiguousarray(np.concatenate([zq, zff, zfb, aqo, aqp, ak, akp], axis=1))
    w_tm = np.ascontiguousarray(np.concatenate([zi, zg, av], axis=1))
    rep = lambda v: np.ascontiguousarray(np.broadcast_to(np.asarray(v).reshape(1, -1), (128, np.asarray(v).size)))
    lb = np.stack([np.asarray(inp["hg_lb_fwd"]), np.asarray(inp["hg_lb_bwd"])], 0)
    lbT = np.ascontiguousarray(lb.reshape(2, 2, 4, 128).transpose(3, 0, 1, 2))
    b_ada = np.asarray(inp["b_ada"][0])
    sh = dict(
        w_ada=np.asarray(inp["w_ada"][0]), b_adaT=np.ascontiguousarray(b_ada.reshape(48, 128).T),
        b_ada_g=np.ascontiguousarray(np.stack([b_ada[2048:3072], b_ada[3072:4096], b_ada[4096:5120], b_ada[5120:6144]], 0)),
        w_fm=w_fm, w_tm=w_tm, lbT=lbT, normw_b=rep(inp["hg_norm_w"][0]), sink_b=rep(inp["attn_sink"][0]),
        w_out=np.asarray(inp["w_out"][0]),
        ln1w_b=rep(inp["ln1_w"][0]), ln1b_b=rep(inp["ln1_b"][0]), ln2w_b=rep(inp["ln2_w"][0]), ln2b_b=rep(inp["ln2_b"][0]),
        router_w=np.asarray(inp["router_w"][0]), rbias_b=rep(inp["router_bias"][0]),
        ew_all=expert_weight_rows(inp),
    )
    return sh


def core_inputs(inp, b0, NB):
    x = np.ascontiguousarray(np.asarray(inp["x"][b0:b0 + NB]))
    ctx = np.ascontiguousarray(np.asarray(inp["ctx"][b0:b0 + NB]))
    c3 = np.concatenate([np.asarray(inp["c"][b0:b0 + NB]), np.asarray(inp["c_ctx"])[None, :]], 0)
    return dict(x=x, ctx=ctx, c3T=np.ascontiguousarray(c3.T))


def kernel(**inputs):
    B, N, _ = inputs["x"].shape
    L = inputs["ctx"].shape[1]
    ncores = 8
    NB = B // ncores
    nc = build(NB, N, L, TBT=16)
    sh = shared_inputs(inputs)
    sh.update(host_constants(N, NB + 1))
    in_maps = []
    for c in range(ncores):
        m = dict(sh)
        m.update(core_inputs(inputs, c * NB, NB))
        in_maps.append(m)
    res = run_bass_kernel_spmd(nc, in_maps, core_ids=list(range(ncores)))
    return np.concatenate([np.asarray(r["out"]) for r in res.results], axis=0).astype(np.float32)
```

```python
import numpy as np
import concourse.bass as bass
import concourse.mybir as mybir
from concourse.bass_utils import run_bass_kernel_spmd

F32 = mybir.dt.float32
BF16 = mybir.dt.bfloat16
AF = mybir.ActivationFunctionType
ALU = mybir.AluOpType
AX = mybir.AxisListType

D = 1024
NE = 64
ALPHA = 2.0 ** 0.25
NEG = -30000.0


class Buf:
    __slots__ = ("t", "name", "lw", "rd", "dsem")

    def __init__(self, t, name):
        self.t = t
        self.name = name
        self.lw = None
        self.rd = {}
        self.dsem = None

    def __getitem__(self, idx):
        return self.t[idx]


class KB:
    def __init__(self, nc):
        self.nc = nc
        self.eng = dict(pe=nc.tensor, act=nc.scalar, dve=nc.vector, pool=nc.gpsimd, sp=nc.sync)
        self.sems = {}
        self.cnt = {}
        self.seen = {e: {} for e in self.eng}
        for e in ("pe", "act", "dve", "pool"):
            self._mksem("E_" + e)
        self.nbuf = 0
        self.ninstr = 0

    def _mksem(self, key):
        self.sems[key] = self.nc.alloc_semaphore(key)
        self.cnt[key] = 0
        return key

    def sb(self, shape, dtype, name=None):
        self.nbuf += 1
        name = name or f"sb{self.nbuf}"
        if getattr(self, "scope", None) is not None:
            return Buf(self.scope.enter_context(self.nc.sbuf_tensor(name, list(shape), dtype)), name)
        return Buf(self.nc.alloc_sbuf_tensor(name, list(shape), dtype), name)

    def barrier(self):
        deps = [(kk, v) for kk, v in self.cnt.items() if v > 0]
        for e in self.eng:
            self._need(e, deps)

    def ps(self, shape, dtype, name=None):
        self.nbuf += 1
        name = name or f"ps{self.nbuf}"
        return Buf(self.nc.alloc_psum_tensor(name, list(shape), dtype), name)

    def _need(self, e, deps):
        seen = self.seen[e]
        best = {}
        for d in deps:
            if d is None:
                continue
            k, v = d
            if seen.get(k, 0) >= v:
                continue
            if best.get(k, 0) < v:
                best[k] = v
        for k, v in best.items():
            self.eng[e].wait_ge(self.sems[k], v)
            seen[k] = v

    def _deps(self, reads, writes):
        deps = []
        for b in reads:
            deps.append(b.lw)
        for b in writes:
            deps.append(b.lw)
            for k, v in b.rd.items():
                deps.append((k, v))
        return deps

    def _record(self, reads, writes, tok):
        k, v = tok
        for b in reads:
            if b.rd.get(k, 0) < v:
                b.rd[k] = v
        for b in writes:
            b.lw = tok
            b.rd = {}

    def op(self, e, ins_fn, reads=(), writes=()):
        self._need(e, self._deps(reads, writes))
        ins = ins_fn(self.eng[e])
        k = "E_" + e
        self.cnt[k] += 1
        ins.then_inc(self.sems[k], 1)
        self._record(reads, writes, (k, self.cnt[k]))
        self.ninstr += 1
        return ins

    def pe(self, fns, reads, out):
        self._need("pe", self._deps(reads, [out]))
        n = len(fns)
        k = "E_pe"
        for i, f in enumerate(fns):
            ins = f(self.nc.tensor)
            self.ninstr += 1
            if i == n - 1:
                self.cnt[k] += 1
                ins.then_inc(self.sems[k], 1)
        self._record(reads, [out], (k, self.cnt[k]))

    def dma(self, q, out_ap, in_ap, reads=(), writes=(), stream=None, nowaw=False, **kw):
        sbuf = stream or (writes[0] if writes else reads[0])
        if sbuf.dsem is None:
            sbuf.dsem = self._mksem("D_" + sbuf.name)
        k = sbuf.dsem
        if nowaw and all(b.lw is not None and b.lw[0] == k and not b.rd for b in writes):
            self._need(q, self._deps(reads, []))
        else:
            self._need(q, self._deps(reads, writes))
        ins = self.eng[q].dma_start(out=out_ap, in_=in_ap, **kw)
        self.cnt[k] += 16
        ins.then_inc(self.sems[k], 16)
        self._record(reads, writes, (k, self.cnt[k]))
        self.ninstr += 1
        return ins

    def idma(self, out_ap, out_off, in_ap, in_off, reads=(), writes=(), stream=None, nowaw=False):
        sbuf = stream
        if sbuf.dsem is None:
            sbuf.dsem = self._mksem("D_" + sbuf.name)
        k = sbuf.dsem
        if nowaw and all(b.lw is not None and b.lw[0] == k and not b.rd for b in writes):
            self._need("pool", self._deps(reads, []))
        else:
            self._need("pool", self._deps(reads, writes))
        ins = self.nc.gpsimd.indirect_dma_start(out=out_ap, out_offset=out_off, in_=in_ap, in_offset=in_off)
        self.cnt[k] += 16
        ins.then_inc(self.sems[k], 16)
        self._record(reads, writes, (k, self.cnt[k]))
        self.ninstr += 1
        return ins

    def wait_all(self, e, bufs):
        deps = []
        for b in bufs:
            deps.append(b.lw)
            for k, v in b.rd.items():
                deps.append((k, v))
        self._need(e, deps)


def bc(ap, axis, shape):
    return ap.unsqueeze(axis).broadcast_to(list(shape))


import contextlib

FM_COLS = 512 * 5 + 128 * 2
TM_COLS = 512 * 2 + 128
FM_OFF = dict(zq=0, zff=512, zfb=1024, aq=1536, aqp=2048, ak=2560, akp=2688)
FM_W = dict(zq=512, zff=512, zfb=512, aq=512, aqp=512, ak=128, akp=128)
TM_OFF = dict(zi=0, zg=512, av=1024)
TM_W = dict(zi=512, zg=512, av=128)
BLK = 256
DEBUG = False
BT = BLK // 128


def build(NB, N, L, TBT=16):
    nc = bass.Bass("TRN2", target_bir_lowering=False)
    k = KB(nc)
    k.scope = None
    R = NB + 1
    NT = N // 128
    NBLK = N // BLK
    TOT = NB * NT
    assert TOT % TBT == 0 and L % 128 == 0 and L <= BLK

    def din(name, shape, dt=F32):
        return nc.dram_tensor(name, list(shape), dt, kind="ExternalInput").ap()

    x = din("x", [NB, N, D]); ctx = din("ctx", [NB, L, D]); c3T = din("c3T", [D, R])
    w_ada = din("w_ada", [D, 6 * D]); b_adaT = din("b_adaT", [128, 48]); bview = din("b_ada_g", [4, D])
    w_fm = din("w_fm", [D, FM_COLS]); w_tm = din("w_tm", [D, TM_COLS])
    lbT = din("lbT", [128, 2, 2, 4]); normw_b = din("normw_b", [128, 512]); sink_b = din("sink_b", [128, 8])
    w_out = din("w_out", [D, D])
    ln1w_b = din("ln1w_b", [128, D]); ln1b_b = din("ln1b_b", [128, D])
    ln2w_b = din("ln2w_b", [128, D]); ln2b_b = din("ln2b_b", [128, D])
    router_w = din("router_w", [D, NE]); rbias_b = din("rbias_b", [128, NE])
    ew_all = din("ew_all", [(NE + 1) * 128, 6144])
    NA = 9
    NTL = (TOT * 128 * NA) // 512 + NE + 1
    NSLOT = NTL * 512
    zrows = din("zrows", [512, D], BF16); ramp_d = din("ramp", [128, 256]); jv_d = din("jv", [128, NA]); pcol_d = din("pcol", [128, 1])
    ustrict_d = din("ustrict", [128, 128])
    ident_f = din("ident_f", [128, 128]); cosT = din("cosT", [128, N]); sinT = din("sinT", [128, N])
    mask_f = din("mask_f", [128, 128]); mask_b = din("mask_b", [128, 128])
    negm_prev = din("negm_prev", [128, 128]); negm_next = din("negm_next", [128, 128])
    rst64 = din("rst64", [128, 512]); rst16 = din("rst16", [128, 512]); sel_rows = din("sel_rows", [R, R, 128])
    out = nc.dram_tensor("out", [NB, N, D], F32, kind="ExternalOutput").ap()
    x1s = nc.dram_tensor("x1s", [NB, N, D], F32, kind="Internal").ap()
    obs = nc.dram_tensor("obs", [NB, N, 512], F32, kind="Internal").ap()
    wog = nc.dram_tensor("wog", [NB, D, D], BF16, kind="Internal").ap()
    rws = nc.dram_tensor("rws", [NB, 3, 128, D], F32, kind="Internal").ap()
    Xs = nc.dram_tensor("Xs", [NSLOT, D], BF16, kind="Internal").ap()
    Ys = nc.dram_tensor("Ys", [NSLOT, D], BF16, kind="Internal").ap()
    H2 = nc.dram_tensor("H2", [TOT * 128, D], BF16, kind="Internal").ap()

    pbs = [k.ps([128, 512], F32, f"pb{i}") for i in range(6)]
    pts = [k.ps([128, 1024], BF16, f"pt{i}") for i in range(2)]
    st = {"pb": 0, "pt": 0, "rr": {}}

    def PB():
        while True:
            st["pb"] = (st["pb"] + 1) % len(pbs)
            if st["pb"] not in st.get("excl", ()):
                return pbs[st["pb"]]

    def PT():
        st["pt"] = (st["pt"] + 1) % len(pts)
        return pts[st["pt"]]

    def ring(name, n, shape, dt):
        key = (name, id(k.scope))
        if key not in st["rr"]:
            st["rr"][key] = [[k.sb(shape, dt, f"{name}_{len(st['rr'])}_{i}") for i in range(n)], 0]
        ent = st["rr"][key]
        ent[1] = (ent[1] + 1) % n
        return ent[0][ent[1]]

    V = lambda fn, r=(), w=(): k.op("dve", fn, r, w)
    A = lambda fn, r=(), w=(): k.op("act", fn, r, w)
    G = lambda fn, r=(), w=(): k.op("pool", fn, r, w)

    def const(src, shape, dt=F32, q="sp", name=None):
        b = k.sb(shape, F32, name + "_f")
        k.dma(q, b[:], src, writes=[b])
        if dt == F32:
            return b
        b2 = k.sb(shape, dt, name + "_b")
        V(lambda e: e.tensor_copy(b2[:], b[:]), [b], [b2])
        return b2

    identF = const(ident_f, [128, 128], F32, name="identF")
    identB = k.sb([128, 128], BF16, "identB")
    V(lambda e: e.tensor_copy(identB[:], identF[:]), [identF], [identB])
    mod = k.sb([128, 48, R], F32, "mod")
    modp1 = k.sb([128, 48, R], F32, "modp1")
    xts_ring = [k.sb([128, D], F32, f"xt{i}") for i in range(3)]
    xst = [0]

    def XT():
        xst[0] = (xst[0] + 1) % 3
        return xts_ring[xst[0]]

    xh = k.sb([128, D], F32, "xh")
    u1 = k.sb([128, D], F32, "u1")
    x1r = [k.sb([128, D], F32, f"x1r{i}") for i in range(2)]
    x1i = [0]

    def X1():
        x1i[0] = (x1i[0] + 1) % 2
        return x1r[x1i[0]]

    lnst = k.sb([128, 2, 6], F32, "lnst"); lnmv = k.sb([128, 2], F32, "lnmv")
    lnrs = k.sb([128, 1], F32, "lnrs"); lnnm = k.sb([128, 1], F32, "lnnm")

    ewb = nc.dram_tensor("ewb", [(NE + 1) * 128, 6144], BF16, kind="Internal").ap()
    ctok = k.sb([1, 2], F32, "ctok")
    ztok = k.sb([1, 2], F32, "ztok")
    bg_list = [("c", ex) for ex in range(NE + 1)]
    zf_pos = [0]

    def zero_fill(n):
        for _ in range(n):
            if zf_pos[0] >= NTL:
                return
            j = zf_pos[0]
            zf_pos[0] += 1
            k.dma("act", Xs[j * 512:(j + 1) * 512, :], zrows, reads=[ztok], stream=ztok)

    bg_pos = [0]

    def background_dmas(n):
        for _ in range(n):
            if bg_pos[0] >= len(bg_list):
                return
            kind, i = bg_list[bg_pos[0]]
            bg_pos[0] += 1
            if kind == "c":
                k.dma("pool", ewb[i * 128:(i + 1) * 128, :], ew_all[i * 128:(i + 1) * 128, :], reads=[ctok], stream=ctok)
            else:
                k.dma("pool", Xs[i * 512:(i + 1) * 512, :], zrows, reads=[ztok], stream=ztok)

    k.scope = contextlib.ExitStack()
    selr = k.sb([R, R, 128], F32, "selr")
    k.dma("sp", selr[:], sel_rows, writes=[selr])
    c3 = k.sb([128, 8, R], F32, "c3")
    k.dma("sp", c3[:], c3T.rearrange("(kc p) r -> p kc r", p=128), writes=[c3])
    sC = k.sb([128, 8, R], F32, "sC")
    A(lambda e: e.activation(sC[:], c3[:], AF.Exp, scale=-1.0), [c3], [sC])
    V(lambda e: e.tensor_scalar(sC[:], sC[:], 1.0, None, ALU.add), [sC], [sC])
    V(lambda e: e.reciprocal(sC[:], sC[:]), [sC], [sC])
    V(lambda e: e.tensor_tensor(sC[:], sC[:], c3[:], ALU.mult), [sC, c3], [sC])
    badaT = k.sb([128, 48], F32, "badaT")
    k.dma("sp", badaT[:], b_adaT, writes=[badaT])
    modps = PB()
    st["excl"] = {st["pb"]}
    grow = k.sb([R, 4, D], F32, "grow")
    for cc in range(12):
        wa = ring("wa", 2, [128, 8, 512], F32)
        k.dma("act", wa[:], w_ada.rearrange("(kc p) n -> p kc n", p=128)[:, :, cc * 512:(cc + 1) * 512], writes=[wa])
        for j in range(4):
            jj = cc * 4 + j
            k.pe([(lambda e, kc=kc: e.matmul(modps[:, jj * R:(jj + 1) * R], wa[:, kc, j * 128:(j + 1) * 128], sC[:, kc, :],
                                           start=(kc == 0), stop=(kc == 7))) for kc in range(8)], [wa, sC], modps)
        if 4 <= cc <= 11:
            gi = (cc - 4) // 2
            hf = cc % 2
            pr = PB()
            k.pe([(lambda e, kc=kc: e.matmul(pr[0:R, :], sC[:, kc, :], wa[:, kc, :], start=(kc == 0), stop=(kc == 7)))
                  for kc in range(8)], [wa, sC], pr)
            V(lambda e: e.tensor_copy(grow[:, gi, hf * 512:(hf + 1) * 512], pr[0:R, :]), [pr], [grow])
    V(lambda e: e.tensor_tensor(mod[:], modps[:, 0:48 * R].rearrange("p (a r) -> p a r", r=R), bc(badaT[:], 2, [128, 48, R]), ALU.add),
      [modps, badaT], [mod])
    V(lambda e: e.tensor_scalar(modp1[:], mod[:], 1.0, None, ALU.add), [mod], [modp1])
    st["excl"] = set()
    badar = k.sb([R, 4, D], F32, "badar")
    for r in range(R):
        k.dma("sp", badar[r:r + 1, :, :], bview.rearrange("(o g) d -> o g d", o=1), writes=[badar])
    V(lambda e: e.tensor_tensor(grow[:], grow[:], badar[:], ALU.add), [grow, badar], [grow])
    grbt = k.sb([128, D], F32, "grbt")
    wob = k.sb([128, D], BF16, "wob")
    for b in range(NB):
        for gi in range(4):
            for hf in range(2):
                pr = PB()
                k.pe([lambda e: e.matmul(pr[:], selr[:, b, :], grow[:, gi, hf * 512:(hf + 1) * 512], start=True, stop=True)],
                     [selr, grow], pr)
                V(lambda e: e.tensor_copy(grbt[:, hf * 512:(hf + 1) * 512], pr[:]), [pr], [grbt])
            if gi >= 1:
                if gi == 2:
                    V(lambda e: e.tensor_scalar(grbt[:], grbt[:], 1.0, None, ALU.add), [grbt], [grbt])
                k.dma("sp", rws[b, gi - 1], grbt[:], reads=[grbt])
            else:
                for kc in range(8):
                    xt = XT()
                    k.dma("sp", xt[:], w_out[kc * 128:(kc + 1) * 128, :], writes=[xt])
                    V(lambda e: e.tensor_tensor(wob[:], xt[:], grbt[:], ALU.mult), [xt, grbt], [wob])
                    k.dma("sp", wog[b, kc * 128:(kc + 1) * 128, :], wob[:], reads=[wob])
    k.barrier()
    k.scope.close()

    k.scope = contextlib.ExitStack()
    maskF = const(mask_f, [128, 128], name="maskF"); maskB = const(mask_b, [128, 128], name="maskB")
    negP = const(negm_prev, [128, 128], BF16, name="negP"); negN = const(negm_next, [128, 128], BF16, name="negN")
    r64 = const(rst64[:, 0:BLK], [128, BLK], name="r64"); r16 = const(rst16[:, 0:BLK], [128, BLK], name="r16")
    normw = const(normw_b, [128, 512], name="normw")
    ln1w = const(ln1w_b, [128, D], name="ln1w"); ln1b = const(ln1b_b, [128, D], name="ln1b")
    esink = k.sb([128, 8], F32, "esink")
    k.dma("sp", esink[:], sink_b, writes=[esink])
    A(lambda e: e.activation(esink[:], esink[:], AF.Exp), [esink], [esink])
    lbt = k.sb([128, 2, 2, 4], F32, "lbt")
    k.dma("sp", lbt[:], lbT, writes=[lbt])
    oml = k.sb([128, 2, 4], F32, "oml"); noml = k.sb([128, 2, 4], F32, "noml")
    V(lambda e: e.tensor_tensor(oml[:], lbt[:, :, 0, :], lbt[:, :, 1, :], ALU.subtract), [lbt], [oml])
    A(lambda e: e.activation(oml[:], oml[:], AF.Exp), [oml], [oml])
    V(lambda e: e.tensor_scalar(oml[:], oml[:], 1.0, None, ALU.add), [oml], [oml])
    V(lambda e: e.reciprocal(oml[:], oml[:]), [oml], [oml])
    V(lambda e: e.tensor_scalar(noml[:], oml[:], -1.0, None, ALU.mult), [oml], [noml])

    W = k.sb([128, 8, 4096], BF16, "W")
    wmap = {}

    def loadw(fm_names, tm_names, with_wo=None):
        wmap.clear()
        col = 0
        eng_i = [0]

        def stage(src_rows, c0, wd_, kc, dcol):
            xt = XT()
            q = "sp" if eng_i[0] % 2 == 0 else "act"
            k.dma(q, xt[:, 0:wd_], src_rows[:, c0:c0 + wd_], writes=[xt])
            e = ("dve", "act", "pool")[eng_i[0] % 3]
            eng_i[0] += 1
            if e == "act":
                A(lambda en: en.copy(W[:, kc, dcol:dcol + wd_], xt[:, 0:wd_]), [xt], [W])
            elif e == "dve":
                V(lambda en: en.tensor_copy(W[:, kc, dcol:dcol + wd_], xt[:, 0:wd_]), [xt], [W])
            else:
                G(lambda en: en.tensor_copy(W[:, kc, dcol:dcol + wd_], xt[:, 0:wd_]), [xt], [W])

        for nm in fm_names:
            wmap[nm] = col
            col += FM_W[nm]
        for nm in tm_names:
            wmap[nm] = col
            col += TM_W[nm]
        for kc in range(8):
            for (src, OFF, WID, names) in ((w_fm, FM_OFF, FM_W, fm_names), (w_tm, TM_OFF, TM_W, tm_names)):
                for nm in names:
                    stage(src[kc * 128:(kc + 1) * 128, :], OFF[nm], WID[nm], kc, wmap[nm])
        if with_wo is not None:
            for kc in range(8):
                k.dma("sp" if kc % 2 == 0 else "act", W[:, kc, col:col + D], wog[with_wo, kc * 128:(kc + 1) * 128, :], writes=[W])
            wmap["wo"] = col
            col += D
        assert col <= 4096

    def ln_stats(src, eps):
        for a_ in range(2):
            V(lambda e: e.bn_stats(lnst[:, a_, :], src[:, a_ * 512:(a_ + 1) * 512]), [src], [lnst])
        V(lambda e: e.bn_aggr(lnmv[:], lnst[:].rearrange("p a b -> p (a b)")), [lnst], [lnmv])
        A(lambda e: e.activation(lnrs[:], lnmv[:, 1:2], AF.Ln, bias=float(eps)), [lnmv], [lnrs])
        A(lambda e: e.activation(lnrs[:], lnrs[:], AF.Exp, scale=-0.5), [lnrs], [lnrs])
        V(lambda e: e.tensor_scalar(lnnm[:], lnmv[:, 0:1], lnrs[:, 0:1], -1.0, ALU.mult, ALU.mult), [lnmv, lnrs], [lnnm])
        return lnrs, lnnm

    def ln_mod_T(src, r, sh_c, sc_c, dst, dcol, dstf=None):
        rstd, nmr = ln_stats(src, 1e-6)
        A(lambda e: e.activation(xh[:], src[:], AF.Identity, bias=nmr[:, 0:1], scale=rstd[:, 0:1]), [src, rstd, nmr], [xh])
        for hf in range(2):
            pp = PB()
            k.pe([(lambda e, j=j: e.transpose(pp[:, j * 128:(j + 1) * 128], xh[:, (hf * 4 + j) * 128:(hf * 4 + j + 1) * 128], identF[:]))
                  for j in range(4)], [xh, identF], pp)
            for j in range(4):
                kc = hf * 4 + j
                if dstf is not None:
                    A(lambda e: e.activation(dstf[:, kc, :], pp[:, j * 128:(j + 1) * 128], AF.Identity,
                                             bias=mod[:, sh_c + kc, r:r + 1], scale=modp1[:, sc_c + kc, r:r + 1]), [pp, mod, modp1], [dstf])
                elif j % 2 == 0:
                    A(lambda e: e.activation(dst[:, kc, dcol:dcol + 128], pp[:, j * 128:(j + 1) * 128], AF.Identity,
                                             bias=mod[:, sh_c + kc, r:r + 1], scale=modp1[:, sc_c + kc, r:r + 1]), [pp, mod, modp1], [dst])
                else:
                    V(lambda e: e.tensor_scalar(dst[:, kc, dcol:dcol + 128], pp[:, j * 128:(j + 1) * 128],
                                                modp1[:, sc_c + kc, r:r + 1], mod[:, sh_c + kc, r:r + 1], ALU.mult, ALU.add), [pp, mod, modp1], [dst])
        if dstf is not None and dst is not None:
            G(lambda e: e.tensor_copy(dst[:, :, dcol:dcol + 128], dstf[:]), [dstf], [dst])

    def proj_fm(hT, T, nm, j, dstps):
        col = wmap[nm] + j * 128
        k.pe([(lambda e, kc=kc: e.matmul(dstps[:, 0:T], W[:, kc, col:col + 128], hT[:, kc, 0:T], start=(kc == 0), stop=(kc == 7)))
              for kc in range(8)], [W, hT], dstps)

    def proj_tm(hT, ti, nm, ncol, dstps):
        col = wmap[nm]
        k.pe([(lambda e, kc=kc: e.matmul(dstps[:, 0:ncol], hT[:, kc, ti * 128:(ti + 1) * 128], W[:, kc, col:col + ncol],
                                        start=(kc == 0), stop=(kc == 7))) for kc in range(8)], [W, hT], dstps)

    Dpad = [k.sb([128, BT, 4, 2, 64], F32, f"Dpad{d}") for d in range(2)]
    Rref = [k.sb([128, 2 * BT, 4], F32, f"Rref{d}") for d in range(2)]
    for d in range(2):
        V(lambda e: e.memset(Dpad[d][:], 0.0), [], [Dpad[d]])
        V(lambda e: e.memset(Rref[d][:], 0.0), [], [Rref[d]])
    khat = [k.sb([128, BT, 4, 2, 64], BF16, f"khat{h}") for h in range(4)]
    qhat = [k.sb([128, BLK], BF16, f"qhat{h}") for h in range(4)]
    qd = [k.sb([128, BT, 2, 2, 64], BF16, f"qd{h}") for h in range(4)]
    for h in range(4):
        V(lambda e: e.memset(qd[h][:], 0.0), [], [qd[h]])
    kdT = k.sb([128, BT, 4, 128], BF16, "kdT")
    sdec = k.sb([128, 4, 2 * BT], F32, "sdec")
    vtm = k.sb([128, BT, 512], BF16, "vtm")
    Sst = [k.sb([128, 4, 128], F32, f"Sst{d}") for d in range(2)]
    kTs = k.sb([128, N], BF16, "kTs"); kcs = k.sb([128, L], BF16, "kcs")
    vaug = k.sb([128, NT, 2, 66], BF16, "vaug"); vcaug = k.sb([128, L // 128, 2, 66], BF16, "vcaug")
    V(lambda e: e.memset(vaug[:], 1.0), [], [vaug]); V(lambda e: e.memset(vcaug[:], 1.0), [], [vcaug])
    Sbfs = [k.sb([128, 4, 128], BF16, f"Sbf{i}") for i in range(4)]
    sbi = [0]

    def SBF():
        sbi[0] = (sbi[0] + 1) % 4
        return Sbfs[sbi[0]]

    T1 = lambda nm: ring(nm, 1, [128, BLK], F32)

    def hgrn_prep(hT, T, d, dosc=True):
        C = T // 64
        NTl = T // 128
        for h in range(4):
            zf = PB()
            proj_fm(hT, T, "zff" if d == 0 else "zfb", h, zf)
            e1 = T1("hp_e1")
            A(lambda e: e.activation(e1[:, 0:T], zf[:, 0:T], AF.Exp), [zf], [e1])
            rr = T1("hp_rr")
            V(lambda e: e.tensor_scalar(rr[:, 0:T], e1[:, 0:T], 1.0, None, ALU.add), [e1], [rr])
            V(lambda e: e.reciprocal(rr[:, 0:T], rr[:, 0:T]), [rr], [rr])
            g = T1("hp_g")
            A(lambda e: e.activation(g[:, 0:T], rr[:, 0:T], AF.Ln, bias=1.0, scale=noml[:, d, h:h + 1]), [rr, noml], [g])
            G(lambda e: e.tensor_scalar(g[:, 0:T], g[:, 0:T], -4.0, None, ALU.max), [g], [g])
            bcm = T1("hp_bc")
            V(lambda e: e.tensor_tensor_scan(bcm[:, 0:T], r64[:, 0:T], g[:, 0:T], 0.0, ALU.mult, ALU.add), [r64, g], [bcm])
            b3 = bcm[:, 0:T].rearrange("p (c j) -> p c j", j=64)
            A(lambda e: e.activation(sdec[:, h, 0:C], b3[:, :, 63], AF.Exp), [bcm], [sdec])
            dk = T1("hp_dk")
            dk3 = dk[:, 0:T].rearrange("p (c j) -> p c j", j=64)
            if d == 0:
                V(lambda e: e.tensor_tensor(dk3, bc(b3[:, :, 63], 2, [128, C, 64]), b3, ALU.subtract), [bcm], [dk])
            else:
                V(lambda e: e.tensor_tensor(dk[:, 0:T], bcm[:, 0:T], g[:, 0:T], ALU.subtract), [bcm, g], [dk])
            A(lambda e: e.activation(dk[:, 0:T], dk[:, 0:T], AF.Exp), [dk], [dk])
            kd = ring("hp_kd", 1, [128, BLK], BF16)
            V(lambda e: e.scalar_tensor_tensor(kd[:, 0:T], rr[:, 0:T], oml[:, d, h:h + 1], dk[:, 0:T], ALU.mult, ALU.mult), [rr, oml, dk], [kd])
            pk2 = PT()
            k.pe([(lambda e, ti=ti: e.transpose(pk2[:, ti * 128:(ti + 1) * 128], kd[:, ti * 128:(ti + 1) * 128], identB[:])) for ti in range(NTl)],
                 [kd, identB], pk2)
            A(lambda e: e.copy(kdT[:, 0:NTl, h, :], pk2[:, 0:NTl * 128].rearrange("p (t d) -> p t d", d=128)), [pk2], [kdT])
            if not dosc:
                continue
            zq = PB()
            proj_fm(hT, T, "zq", h, zq)
            lcm = T1("hp_lc")
            V(lambda e: e.tensor_tensor_scan(lcm[:, 0:T], r16[:, 0:T], g[:, 0:T], 0.0, ALU.mult, ALU.add), [r16, g], [lcm])
            Pq = bcm
            lq = lcm
            if d == 1:
                u = T1("hp_u")
                u3 = u[:, 0:T].rearrange("p (c j) -> p c j", j=64)
                V(lambda e: e.tensor_tensor(u3, bc(b3[:, :, 63], 2, [128, C, 64]), b3, ALU.subtract), [bcm], [u])
                V(lambda e: e.tensor_tensor(u[:, 0:T], u[:, 0:T], g[:, 0:T], ALU.add), [u, g], [u])
                l4 = lcm[:, 0:T].rearrange("p (c j) -> p c j", j=16)
                ls = T1("hp_ls")
                ls4 = ls[:, 0:T].rearrange("p (c j) -> p c j", j=16)
                V(lambda e: e.tensor_tensor(ls4, bc(l4[:, :, 15], 2, [128, T // 16, 16]), l4, ALU.subtract), [lcm], [ls])
                V(lambda e: e.tensor_tensor(ls[:, 0:T], ls[:, 0:T], g[:, 0:T], ALU.add), [ls, g], [ls])
                Pq = u
                lq = ls
            P3 = Pq[:, 0:T].rearrange("p (c j) -> p c j", j=64)
            if d == 0:
                V(lambda e: e.tensor_copy(Rref[0][:, 0:C, 1:4], P3[:, :, 15:63:16]), [Pq], [Rref[0]])
            else:
                V(lambda e: e.tensor_copy(Rref[1][:, 0:C, 0:3], P3[:, :, 16:64:16]), [Pq], [Rref[1]])
            Rr = Rref[d]
            R4 = Rr[:, 0:C, :].rearrange("p (t c) n -> p t c n", c=2)
            P4 = Pq[:, 0:T].rearrange("p (t c j) -> p t c j", c=2, j=64)
            for n in range(4):
                lo, hi = (0, 16 * (n + 1)) if d == 0 else (16 * n, 64)
                V(lambda e: e.tensor_tensor(Dpad[d][:, 0:NTl, n, :, lo:hi], bc(R4[:, :, :, n], 3, [128, NTl, 2, hi - lo]),
                                            P4[:, :, :, lo:hi], ALU.subtract), [Rr, Pq], [Dpad[d]])
            Ep = ring("hp_Ep", 1, [128, BT, 4, 2, 64], F32)
            A(lambda e: e.activation(Ep[:, 0:NTl].rearrange("p t n c j -> p (t n c j)"),
                                     Dpad[d][:, 0:NTl].rearrange("p t n c j -> p (t n c j)"), AF.Exp), [Dpad[d]], [Ep])
            rr4 = rr[:, 0:T].rearrange("p (t c j) -> p t c j", c=2, j=64)
            for n in range(4):
                V(lambda e: e.scalar_tensor_tensor(khat[h][:, 0:NTl, n, :, :], rr4, oml[:, d, h:h + 1], Ep[:, 0:NTl, n, :, :], ALU.mult, ALU.mult),
                  [rr, oml, Ep], [khat[h]])
            el = T1("hp_el")
            A(lambda e: e.activation(el[:, 0:T], lq[:, 0:T], AF.Exp), [lq], [el])
            V(lambda e: e.tensor_tensor(qhat[h][:, 0:T], zq[:, 0:T], el[:, 0:T], ALU.mult), [zq, el], [qhat[h]])
            eb = T1("hp_eb")
            A(lambda e: e.activation(eb[:, 0:T], Pq[:, 0:T], AF.Exp), [Pq], [eb])
            zq4 = zq[:, 0:T].rearrange("p (t c j) -> p t c j", c=2, j=64)
            eb4 = eb[:, 0:T].rearrange("p (t c j) -> p t c j", c=2, j=64)
            for c in range(2):
                V(lambda e: e.tensor_tensor(qd[h][:, 0:NTl, c, c, :], zq4[:, :, c, :], eb4[:, :, c, :], ALU.mult), [zq, eb], [qd[h]])

    def state_update(d, ti, c, Sbf_next):
        pS = PB()
        lo = c * 64
        k.pe([(lambda e, h=h: e.matmul(pS[:, h * 128:(h + 1) * 128], kdT[lo:lo + 64, ti, h, :], vtm[lo:lo + 64, ti, h * 128:(h + 1) * 128],
                                      start=True, stop=True)) for h in range(4)], [kdT, vtm], pS)
        S = Sst[d]
        ci = ti * 2 + c
        V(lambda e: e.tensor_tensor(S[:], S[:], bc(sdec[:, :, ci], 2, [128, 4, 128]), ALU.mult), [S, sdec], [S])
        V(lambda e: e.tensor_tensor(S[:], S[:], pS[:].rearrange("p (h e) -> p h e", e=128), ALU.add), [S, pS], [S])
        if Sbf_next is not None:
            A(lambda e: e.copy(Sbf_next[:], S[:]), [S], [Sbf_next])

    def hgrn_tile(d, ti, Sbf_in):
        pA = PB()
        fns = []
        for h in range(4):
            for cq in range(2):
                for n in range(4):
                    q0 = ti * 128 + cq * 64 + n * 16
                    c0 = h * 128 + cq * 64 + n * 16
                    fns.append(lambda e, h=h, n=n, q0=q0, c0=c0: e.matmul(
                        pA[:, c0:c0 + 16], khat[h][:, ti, n, :, :].rearrange("p c j -> p (c j)"), qhat[h][:, q0:q0 + 16], start=True, stop=True))
        k.pe(fns, khat + qhat, pA)
        Asb = ring("Asb", 2, [128, 4, 128], BF16)
        mk = maskF if d == 0 else maskB
        V(lambda e: e.tensor_tensor(Asb[:], pA[:].rearrange("p (h t) -> p h t", t=128), bc(mk[:], 1, [128, 4, 128]), ALU.mult), [pA, mk], [Asb])
        order = (0, 1) if d == 0 else (1, 0)
        Smid = SBF()
        state_update(d, ti, order[0], Smid)
        Sout = SBF()
        state_update(d, ti, order[1], Sout)
        Sin = {order[0]: Sbf_in, order[1]: Smid}
        pO = PB()
        fns = []
        for h in range(4):
            o_ap = pO[:, h * 128:(h + 1) * 128]
            fns.append(lambda e, h=h, o_ap=o_ap: e.matmul(o_ap, Asb[:, h, :], vtm[:, ti, h * 128:(h + 1) * 128], start=True, stop=False))
            for ci, c in enumerate((0, 1)):
                fns.append(lambda e, h=h, c=c, o_ap=o_ap, ci=ci: e.matmul(o_ap, qd[h][:, ti, c, :, :].rearrange("p a j -> p (a j)"),
                                                                      Sin[c][:, h, :], start=False, stop=(ci == 1)))
        k.pe(fns, [Asb, vtm, Sbf_in, Smid] + qd, pO)
        return pO, Sout

    def load_block_hT(src3, b, r, t0, T, keep=None):
        hT = ring("hT", 1, [128, 8, BLK], BF16)
        for ti in range(T // 128):
            xt = XT()
            k.dma("sp", xt[:], src3[b, t0 + ti * 128:t0 + (ti + 1) * 128, :], writes=[xt])
            if keep is not None:
                keep.append(xt)
            ln_mod_T(xt, r, 0, 8, hT, ti * 128)
        return hT

    def v_block(hT, T):
        for ti in range(T // 128):
            pz = PB()
            proj_tm(hT, ti, "zi", 512, pz)
            A(lambda e: e.copy(vtm[:, ti, :], pz[:]), [pz], [vtm])

    def rope_to(dst_ap, dst_buf, pa, pb_, t0, T):
        cs = ring("cs", 1, [128, 2, BLK], F32)
        k.dma("act", cs[:, 0, 0:T], cosT[:, t0:t0 + T], writes=[cs])
        k.dma("act", cs[:, 1, 0:T], sinT[:, t0:t0 + T], writes=[cs])
        t1 = T1("rp1"); t2 = T1("rp2")
        V(lambda e: e.tensor_tensor(t1[:, 0:T], pa[:, 0:T], cs[:, 0, 0:T], ALU.mult), [pa, cs], [t1])
        V(lambda e: e.tensor_tensor(t2[:, 0:T], pb_[:, 0:T], cs[:, 1, 0:T], ALU.mult), [pb_, cs], [t2])
        G(lambda e: e.tensor_tensor(dst_ap, t1[:, 0:T], t2[:, 0:T], ALU.add), [t1, t2], [dst_buf])

    for b in range(NB):
        loadw(["zff", "zfb", "ak"], ["zi", "av"])
        hTc = load_block_hT(ctx, b, NB, 0, L)
        v_block(hTc, L)
        pk_ = PB()
        proj_fm(hTc, L, "ak", 0, pk_)
        A(lambda e: e.copy(kcs[:, 0:L], pk_[:, 0:L]), [pk_], [kcs])
        for ti in range(L // 128):
            pv = PB()
            proj_tm(hTc, ti, "av", 128, pv)
            V(lambda e: e.tensor_copy(vcaug[:, ti, :, 0:64], pv[:, 0:128].rearrange("p (g d) -> p g d", d=64)), [pv], [vcaug])
        for d in range(2):
            hgrn_prep(hTc, L, d, dosc=False)
            V(lambda e: e.memset(Sst[d][:], 0.0), [], [Sst[d]])
            tiles = list(range(L // 128)) if d == 0 else list(reversed(range(L // 128)))
            for ti in tiles:
                for c in ((0, 1) if d == 0 else (1, 0)):
                    state_update(d, ti, c, None)
        loadw(["zq", "zfb", "ak", "akp"], ["zi", "av"])
        Sbf = SBF()
        A(lambda e: e.copy(Sbf[:], Sst[1][:]), [Sst[1]], [Sbf])
        for blk in reversed(range(NBLK)):
            t0 = blk * BLK
            background_dmas(-(-len(bg_list) // (2 * NBLK)))
            if b == NB - 1:
                zero_fill(-(-NTL // (2 * NBLK)))
            hT = load_block_hT(x, b, b, t0, BLK)
            v_block(hT, BLK)
            pk_ = PB(); pkp = PB()
            proj_fm(hT, BLK, "ak", 0, pk_)
            proj_fm(hT, BLK, "akp", 0, pkp)
            rope_to(kTs[:, t0:t0 + BLK], kTs, pk_, pkp, t0, BLK)
            for ti in range(BT):
                pv = PB()
                proj_tm(hT, ti, "av", 128, pv)
                V(lambda e: e.tensor_copy(vaug[:, blk * BT + ti, :, 0:64], pv[:, 0:128].rearrange("p (g d) -> p g d", d=64)), [pv], [vaug])
            hgrn_prep(hT, BLK, 1)
            for ti in reversed(range(BT)):
                pO, Sbf = hgrn_tile(1, ti, Sbf)
                obt = ring("obt", 1, [128, 512], F32)
                A(lambda e: e.copy(obt[:], pO[:]), [pO], [obt])
                gt = blk * BT + ti
                k.dma("sp", obs[b, gt * 128:(gt + 1) * 128, :], obt[:], reads=[obt])
        k.barrier()
        loadw(["zq", "zff", "aq", "aqp"], ["zi", "zg"], with_wo=b)
        Sbf = SBF()
        A(lambda e: e.copy(Sbf[:], Sst[0][:]), [Sst[0]], [Sbf])
        for blk in range(NBLK):
            t0 = blk * BLK
            background_dmas(-(-len(bg_list) // (2 * NBLK)))
            if b == NB - 1:
                zero_fill(-(-NTL // (2 * NBLK)))
            xts = []
            hT = load_block_hT(x, b, b, t0, BLK, keep=xts)
            v_block(hT, BLK)
            qT = ring("qT", 1, [128, 4, BLK], BF16)
            for j in range(4):
                pq = PB(); pqp = PB()
                proj_fm(hT, BLK, "aq", j, pq)
                proj_fm(hT, BLK, "aqp", j, pqp)
                rope_to(qT[:, j, :], qT, pq, pqp, t0, BLK)
            hgrn_prep(hT, BLK, 0)
            for ti in range(BT):
                gt = blk * BT + ti
                ylat = ring("ylat", 1, [128, D], BF16)
                obt = ring("obt", 1, [128, 512], F32)
                k.dma("act", obt[:], obs[b, gt * 128:(gt + 1) * 128, :], writes=[obt])
                pO, Sbf = hgrn_tile(0, ti, Sbf)
                o = ring("ro_o", 1, [128, 512], F32)
                V(lambda e: e.tensor_tensor(o[:], pO[:], obt[:], ALU.add), [pO, obt], [o])
                sq = ring("ro_sq", 1, [128, 512], F32)
                G(lambda e: e.tensor_tensor(sq[:], o[:], o[:], ALU.mult), [o], [sq])
                ss = ring("ro_ss", 1, [128, 4], F32)
                V(lambda e: e.reduce_sum(ss[:], sq[:].rearrange("p (h e) -> p h e", e=128), AX.X), [sq], [ss])
                A(lambda e: e.activation(ss[:], ss[:], AF.Ln, bias=1e-6, scale=1.0 / 128.0), [ss], [ss])
                A(lambda e: e.activation(ss[:], ss[:], AF.Exp, scale=-0.5), [ss], [ss])
                V(lambda e: e.tensor_tensor(o[:].rearrange("p (h e) -> p h e", e=128), o[:].rearrange("p (h e) -> p h e", e=128),
                                            bc(ss[:], 2, [128, 4, 128]), ALU.mult), [o, ss], [o])
                G(lambda e: e.tensor_tensor(o[:], o[:], normw[:], ALU.mult), [o, normw], [o])
                pg = PB()
                proj_tm(hT, ti, "zg", 512, pg)
                sg = ring("ro_sg", 1, [128, 512], F32)
                A(lambda e: e.activation(sg[:], pg[:], AF.Exp, scale=-1.0), [pg], [sg])
                V(lambda e: e.tensor_scalar(sg[:], sg[:], 1.0, None, ALU.add), [sg], [sg])
                V(lambda e: e.reciprocal(sg[:], sg[:]), [sg], [sg])
                V(lambda e: e.tensor_tensor(sg[:], sg[:], pg[:], ALU.mult), [sg, pg], [sg])
                V(lambda e: e.tensor_tensor(ylat[:, 0:512], o[:], sg[:], ALU.mult), [o, sg], [ylat])
                for kh in range(2):
                    po = kh * 64
                    kts = [("c", i) for i in range(L // 128)]
                    if gt > 0:
                        kts.append(("p", gt - 1))
                    kts.append(("s", gt))
                    if gt < NT - 1:
                        kts.append(("n", gt + 1))
                    Pl = []
                    for ki, (kind, kt) in enumerate(kts):
                        pS = PB()
                        kap = kcs[po:po + 64, kt * 128:(kt + 1) * 128] if kind == "c" else kTs[po:po + 64, kt * 128:(kt + 1) * 128]
                        qap = qT[po:po + 64, :, ti * 128:(ti + 1) * 128]
                        msk = kind in ("p", "n")
                        pS3 = pS[:].rearrange("p (h q) -> p h q", q=128)
                        fns = [lambda e, kap=kap, qap=qap, msk=msk, pS3=pS3: e.matmul(pS3, kap, qap, start=True, stop=not msk)]
                        if msk:
                            nm = negP if kind == "p" else negN
                            fns.append(lambda e, nm=nm, pS3=pS3: e.matmul(pS3, identB[:], bc(nm[:], 1, [128, 4, 128]), start=False, stop=True))
                        k.pe(fns, [kcs, kTs, qT, identB, negP, negN], pS)
                        Psb = ring("Psb", 5, [128, 512], BF16)
                        A(lambda e: e.activation(Psb[:], pS[:], AF.Exp, scale=0.125), [pS], [Psb])
                        Pl.append((Psb, vcaug if kind == "c" else vaug, kt))
                    pOa = PB()
                    fns = []
                    for hq in range(4):
                        for ki, (Psb, va, kt) in enumerate(Pl):
                            fns.append(lambda e, hq=hq, Psb=Psb, va=va, kt=kt, ki=ki: e.matmul(
                                pOa[:, hq * 80:hq * 80 + 65], Psb[:, hq * 128:(hq + 1) * 128], va[:, kt, kh, 0:65],
                                start=(ki == 0), stop=(ki == len(Pl) - 1)))
                    k.pe(fns, [p[0] for p in Pl] + [vaug, vcaug], pOa)
                    den = ring("at_den", 1, [128, 4], F32)
                    pO4 = pOa[:, 0:320].rearrange("p (h c) -> p h c", c=80)
                    V(lambda e: e.tensor_tensor(den[:], pO4[:, :, 64], esink[:, kh * 4:(kh + 1) * 4], ALU.add), [pOa, esink], [den])
                    V(lambda e: e.reciprocal(den[:], den[:]), [den], [den])
                    V(lambda e: e.tensor_tensor(ylat[:, 512 + kh * 256:512 + (kh + 1) * 256].rearrange("p (h c) -> p h c", c=64), pO4[:, :, 0:64],
                                                bc(den[:], 2, [128, 4, 64]), ALU.mult), [pOa, den], [ylat])
                pt = PT()
                k.pe([(lambda e, kc=kc: e.transpose(pt[:, kc * 128:(kc + 1) * 128], ylat[:, kc * 128:(kc + 1) * 128], identB[:])) for kc in range(8)],
                     [ylat, identB], pt)
                yT = ring("yT", 1, [128, 8, 128], BF16)
                A(lambda e: e.copy(yT[:], pt[:].rearrange("p (c t) -> p c t", t=128)), [pt], [yT])
                xt = xts[ti]
                wo = wmap["wo"]
                for hf in range(2):
                    pp = PB()
                    k.pe([(lambda e, kc=kc: e.matmul(pp[:], yT[:, kc, :], W[:, kc, wo + hf * 512:wo + (hf + 1) * 512], start=(kc == 0), stop=(kc == 7)))
                          for kc in range(8)], [yT, W], pp)
                    V(lambda e: e.scalar_tensor_tensor(u1[:, hf * 512:(hf + 1) * 512], xt[:, hf * 512:(hf + 1) * 512], ALPHA, pp[:], ALU.mult, ALU.add),
                      [xt, pp], [u1])
                rstd, nmr = ln_stats(u1, 1e-5)
                x1 = X1()
                A(lambda e: e.activation(x1[:], u1[:], AF.Identity, bias=nmr[:, 0:1], scale=rstd[:, 0:1]), [u1, rstd, nmr], [x1])
                G(lambda e: e.tensor_tensor(x1[:], x1[:], ln1w[:], ALU.mult), [x1, ln1w], [x1])
                G(lambda e: e.tensor_tensor(x1[:], x1[:], ln1b[:], ALU.add), [x1, ln1b], [x1])
                k.dma("sp", x1s[b, gt * 128:(gt + 1) * 128, :], x1[:], reads=[x1], stream=x1)
        k.barrier()
    k.scope.close()

    background_dmas(len(bg_list))
    zero_fill(NTL)
    I32 = mybir.dt.int32
    moe_scope = contextlib.ExitStack()
    k.scope = moe_scope
    NEX = NE + 1
    ln2w = const(ln2w_b, [128, D], name="ln2w"); ln2b = const(ln2b_b, [128, D], name="ln2b")
    rowsb = [k.sb([128, D], F32, f"rowsb{i}") for i in range(3)]
    rows_cur = [None]

    def load_rows(b):
        if rows_cur[0] != b:
            for i in range(3):
                k.dma("sp", rowsb[i][:], rws[b, i], writes=[rowsb[i]])
            rows_cur[0] = b

    slall = k.sb([128, TOT, NA], I32, "slall")
    gjall = k.sb([128, TOT, NA], F32, "gjall")
    idxw = k.sb([128, NTL], I32, "idxw")
    sub = contextlib.ExitStack()
    k.scope = sub
    rbias = const(rbias_b, [128, NE], name="rbias")
    ramp = const(ramp_d, [128, 256], name="ramp"); jv = const(jv_d, [128, NA], name="jv"); pcol = const(pcol_d, [128, 1], name="pcol")
    ustr = const(ustrict_d, [128, 128], BF16, name="ustr")
    onesB = k.sb([128, 128], BF16, "onesB")
    V(lambda e: e.memset(onesB[:], 1.0), [], [onesB])
    rw = k.sb([128, 8, NE], F32, "rw")
    k.dma("sp", rw[:], router_w.rearrange("(kc p) n -> p kc n", p=128), writes=[rw])
    emask = k.sb([128, TOT, NEX], F32, "emask")
    gall = k.sb([128, TOT, NEX], F32, "gall")
    V(lambda e: e.memset(emask[:], 1.0), [], [emask])
    V(lambda e: e.memset(gall[:], 1.0), [], [gall])
    cnt = k.sb([128, NEX], F32, "cnt")
    V(lambda e: e.memset(cnt[:], 0.0), [], [cnt])
    RT = lambda nm, w: ring(nm, 1, [128, w], F32)

    for gti in range(TOT):
        b = gti // NT
        ti = gti % NT
        load_rows(b)
        xt = XT()
        k.dma("sp", xt[:], x1s[b, ti * 128:(ti + 1) * 128, :], writes=[xt])
        hf32 = ring("h2f", 1, [128, 8, 128], F32)
        ln_mod_T(xt, b, 24, 32, None, 0, dstf=hf32)
        h2b = ring("h2b", 2, [128, D], BF16)
        V(lambda e: e.tensor_tensor(u1[:], xh[:], rowsb[1][:], ALU.mult), [xh, rowsb[1]], [u1])
        V(lambda e: e.tensor_tensor(h2b[:], u1[:], rowsb[0][:], ALU.add), [u1, rowsb[0]], [h2b])
        k.dma("sp", H2[gti * 128:(gti + 1) * 128, :], h2b[:], reads=[h2b])
        pr = PB()
        k.pe([(lambda e, kc=kc: e.matmul(pr[:, 0:NE], hf32[:, kc, :], rw[:, kc, :], start=(kc == 0), stop=(kc == 7))) for kc in range(8)], [hf32, rw], pr)
        if DEBUG and gti == 0:
            dbg_h = nc.dram_tensor("dbg_h", [128, 8, 128], F32, kind="ExternalOutput").ap()
            dbg_lg = nc.dram_tensor("dbg_lg", [128, NE], F32, kind="ExternalOutput").ap()
            k.dma("sp", dbg_h, hf32[:], reads=[hf32])
            lgs = k.sb([128, NE], F32, "lgs")
            V(lambda e: e.tensor_copy(lgs[:], pr[:, 0:NE]), [pr], [lgs])
            k.dma("sp", dbg_lg, lgs[:], reads=[lgs])
        sc = RT("rt_sc", NE)
        A(lambda e: e.activation(sc[:], pr[:, 0:NE], AF.Exp, scale=-1.0), [pr], [sc])
        V(lambda e: e.tensor_scalar(sc[:], sc[:], 1.0, None, ALU.add), [sc], [sc])
        V(lambda e: e.reciprocal(sc[:], sc[:]), [sc], [sc])
        sel = RT("rt_sel", NE)
        V(lambda e: e.tensor_tensor(sel[:], sc[:], rbias[:], ALU.add), [sc, rbias], [sel])
        sel3 = sel[:].rearrange("p (g i) -> p g i", i=8)
        m1 = RT("rt_m1", 8); m2 = RT("rt_m2", 8)
        V(lambda e: e.reduce_max(m1[:], sel3, AX.X), [sel], [m1])
        eq = RT("rt_eq", NE)
        eq3 = eq[:].rearrange("p (g i) -> p g i", i=8)
        V(lambda e: e.tensor_tensor(eq3, sel3, bc(m1[:], 2, [128, 8, 8]), ALU.is_ge), [sel, m1], [eq])
        V(lambda e: e.scalar_tensor_tensor(eq[:], eq[:], -1e9, sel[:], ALU.mult, ALU.add), [eq, sel], [eq])
        V(lambda e: e.reduce_max(m2[:], eq3, AX.X), [eq], [m2])
        V(lambda e: e.tensor_tensor(m1[:], m1[:], m2[:], ALU.add), [m1, m2], [m1])
        top8 = RT("rt_t8", 8)
        V(lambda e: e.max(top8[:], m1[:]), [m1], [top8])
        gm = RT("rt_gm", 8); gneg = RT("rt_gn", 8)
        V(lambda e: e.tensor_scalar(gm[:], m1[:], top8[:, 3:4], None, ALU.is_ge), [m1, top8], [gm])
        V(lambda e: e.tensor_scalar(gneg[:], gm[:], 1e9, -1e9, ALU.mult, ALU.add), [gm], [gneg])
        V(lambda e: e.tensor_tensor(eq3, sel3, bc(gm[:], 2, [128, 8, 8]), ALU.mult), [sel, gm], [eq])
        V(lambda e: e.tensor_tensor(eq3, eq3, bc(gneg[:], 2, [128, 8, 8]), ALU.add), [eq, gneg], [eq])
        t8b = RT("rt_t8b", 8)
        V(lambda e: e.max(t8b[:], eq[:]), [eq], [t8b])
        V(lambda e: e.tensor_scalar(emask[:, gti, 0:NE], eq[:], t8b[:, 7:8], None, ALU.is_ge), [eq, t8b], [emask])
        V(lambda e: e.tensor_tensor(eq[:], emask[:, gti, 0:NE], sc[:], ALU.mult), [emask, sc], [eq])
        ws = RT("rt_ws", 1)
        V(lambda e: e.reduce_sum(ws[:], eq[:], AX.X), [eq], [ws])
        V(lambda e: e.reciprocal(ws[:], ws[:]), [ws], [ws])
        V(lambda e: e.tensor_scalar(gall[:, gti, 0:NE], eq[:], ws[:, 0:1], 2.5, ALU.mult, ALU.mult), [eq, ws], [gall])
        emb = ring("emb", 2, [128, NEX], BF16)
        V(lambda e: e.tensor_copy(emb[:], emask[:, gti, :]), [emask], [emb])
        pc = PB()
        k.pe([lambda e: e.matmul(pc[:, 0:NEX], onesB[:], emb[:], start=True, stop=True)], [onesB, emb], pc)
        V(lambda e: e.tensor_tensor(cnt[:], cnt[:], pc[:, 0:NEX], ALU.add), [cnt, pc], [cnt])

    MT = (TOT * 128) // 512 + 1
    assert MT <= 256 and NTL <= 256
    cmpb = k.sb([128, NEX, MT], F32, "cmpb")
    V(lambda e: e.tensor_tensor(cmpb[:], bc(cnt[:], 2, [128, NEX, MT]), bc(ramp[:, 0:MT], 1, [128, NEX, MT]), ALU.is_gt), [cnt, ramp], [cmpb])
    pcn = k.sb([128, NEX], F32, "pcn")
    V(lambda e: e.reduce_sum(pcn[:], cmpb[:], AX.X), [cmpb], [pcn])
    V(lambda e: e.tensor_scalar(pcn[:], pcn[:], 512.0, None, ALU.mult), [pcn], [pcn])
    ends = k.sb([128, NEX], F32, "ends"); starts = k.sb([128, NEX], F32, "starts")
    onesf = k.sb([128, NEX], F32, "onesf")
    V(lambda e: e.memset(onesf[:], 1.0), [], [onesf])
    V(lambda e: e.tensor_tensor_scan(ends[:], onesf[:], pcn[:], 0.0, ALU.mult, ALU.add), [onesf, pcn], [ends])
    V(lambda e: e.tensor_tensor(starts[:], ends[:], pcn[:], ALU.subtract), [ends, pcn], [starts])
    ej = k.sb([128, NTL], F32, "ej")
    JC = 16
    cmp2 = k.sb([128, JC, NEX], F32, "cmp2")
    for j0 in range(0, NTL, JC):
        jn = min(JC, NTL - j0)
        V(lambda e: e.tensor_tensor(cmp2[:, 0:jn, :], bc(ends[:], 1, [128, jn, NEX]), bc(ramp[:, j0:j0 + jn], 2, [128, jn, NEX]), ALU.is_le),
          [ends, ramp], [cmp2])
        V(lambda e: e.reduce_sum(ej[:, j0:j0 + jn], cmp2[:, 0:jn, :], AX.X), [cmp2], [ej])
    V(lambda e: e.tensor_scalar(ej[:], ej[:], float(NE), 128.0, ALU.min, ALU.mult), [ej], [ej])
    V(lambda e: e.tensor_scalar(ej[:], ej[:], pcol[:, 0:1], None, ALU.add), [ej, pcol], [ej])
    V(lambda e: e.tensor_copy(idxw[:], ej[:]), [ej], [idxw])

    k.barrier()
    offs = k.sb([128, NEX], F32, "offs")
    V(lambda e: e.tensor_copy(offs[:], starts[:]), [starts], [offs])
    sct = k.sb([1, 2], F32, "sct")
    for gti in range(TOT):
        emb = ring("emb", 2, [128, NEX], BF16)
        V(lambda e: e.tensor_copy(emb[:], emask[:, gti, :]), [emask], [emb])
        pc = PB()
        k.pe([lambda e: e.matmul(pc[:, 0:NEX], ustr[:], emb[:], start=True, stop=True),
              lambda e: e.matmul(pc[:, 128:128 + NEX], onesB[:], emb[:], start=True, stop=True)], [ustr, onesB, emb], pc)
        slot = RT("p2_slot", NEX)
        V(lambda e: e.tensor_tensor(slot[:], pc[:, 0:NEX], offs[:], ALU.add), [pc, offs], [slot])
        V(lambda e: e.tensor_tensor(offs[:], offs[:], pc[:, 128:128 + NEX], ALU.add), [offs, pc], [offs])
        ks = RT("p2_ks", NEX)
        V(lambda e: e.tensor_tensor_scan(ks[:], onesf[:], emask[:, gti, :], 0.0, ALU.mult, ALU.add), [onesf, emask], [ks])
        s3 = ring("p2_s3", 1, [128, NA, NEX], F32)
        V(lambda e: e.tensor_tensor(s3[:], bc(ks[:], 1, [128, NA, NEX]), bc(jv[:], 2, [128, NA, NEX]), ALU.is_equal), [ks, jv], [s3])
        V(lambda e: e.tensor_tensor(s3[:], s3[:], bc(emask[:, gti, :], 1, [128, NA, NEX]), ALU.mult), [s3, emask], [s3])
        t3 = ring("p2_t3", 1, [128, NA, NEX], F32)
        V(lambda e: e.tensor_tensor(t3[:], s3[:], bc(slot[:], 1, [128, NA, NEX]), ALU.mult), [s3, slot], [t3])
        slf = RT("p2_slf", NA)
        V(lambda e: e.reduce_sum(slf[:], t3[:], AX.X), [t3], [slf])
        V(lambda e: e.tensor_copy(slall[:, gti, :], slf[:]), [slf], [slall])
        V(lambda e: e.tensor_tensor(t3[:], s3[:], bc(gall[:, gti, :], 1, [128, NA, NEX]), ALU.mult), [s3, gall], [t3])
        V(lambda e: e.reduce_sum(gjall[:, gti, :], t3[:], AX.X), [t3], [gjall])
        h2s = ring("h2s", 2, [128, D], BF16)
        k.dma("sp", h2s[:], H2[gti * 128:(gti + 1) * 128, :], writes=[h2s])
        for j in range(NA):
            k.idma(Xs[:, :], bass.IndirectOffsetOnAxis(ap=slall[:, gti, j:j + 1], axis=0), h2s[:, :], None, reads=[h2s, slall], stream=sct)
    k.barrier()
    sub.close()
    k.scope = moe_scope

    if DEBUG:
        dbg_sl = nc.dram_tensor("dbg_sl", [128, TOT, NA], I32, kind="ExternalOutput").ap()
        dbg_gj = nc.dram_tensor("dbg_gj", [128, TOT, NA], F32, kind="ExternalOutput").ap()
        dbg_iw = nc.dram_tensor("dbg_iw", [128, NTL], I32, kind="ExternalOutput").ap()
        k.dma("sp", dbg_sl, slall[:], reads=[slall]); k.dma("sp", dbg_gj, gjall[:], reads=[gjall]); k.dma("sp", dbg_iw, idxw[:], reads=[idxw])
    wbfs = [k.sb([128, 6144], BF16, f"wbf{i}") for i in range(3)]
    NSUB = NTL * 4
    stt = {}
    wcur = {}

    def tile_prologue(j):
        wbf = wbfs[j % 3]
        k.idma(wbf[:, :], None, ewb[:, :], bass.IndirectOffsetOnAxis(ap=idxw[:, j:j + 1], axis=0), reads=[idxw], writes=[wbf], stream=wbf)
        xst = ring("xst", 3, [128, 4, D], BF16)
        k.dma("sp", xst[:], Xs[j * 512:(j + 1) * 512, :].rearrange("(s p) d -> p s d", p=128), writes=[xst])
        wcur[j] = (wbf, xst)

    def stA(i):
        j, s_ = divmod(i, 4)
        if s_ == 0 and j == 0:
            tile_prologue(0)
            if NTL > 1:
                tile_prologue(1)
        if s_ == 2 and j + 2 < NTL:
            tile_prologue(j + 2)
        wbf, xst = wcur[j]
        pt = PT()
        k.pe([(lambda e, kc=kc: e.transpose(pt[:, kc * 128:(kc + 1) * 128], xst[:, s_, kc * 128:(kc + 1) * 128], identB[:])) for kc in range(8)], [xst, identB], pt)
        hsT = ring("hsT", 3, [128, 8, 128], BF16)
        A(lambda e: e.copy(hsT[:], pt[:].rearrange("p (c t) -> p c t", t=128)), [pt], [hsT])
        p1 = PB()
        k.pe([(lambda e, kc=kc: e.matmul(p1[:], hsT[:, kc, :], wbf[:, kc * 512:(kc + 1) * 512], start=(kc == 0), stop=(kc == 7))) for kc in range(8)],
             [hsT, wbf], p1)
        sg = ring("ex_sg", 4, [128, 256], F32)
        A(lambda e: e.activation(sg[:], p1[:, 0:256], AF.Silu), [p1], [sg])
        h1 = ring("ex_h1", 4, [128, 256], BF16)
        V(lambda e: e.tensor_tensor(h1[:], p1[:, 256:512], sg[:], ALU.mult), [p1, sg], [h1])
        stt[i] = [h1, None]

    def stB(i):
        h1 = stt[i][0]
        pt = PT()
        k.pe([(lambda e, fc=fc: e.transpose(pt[:, fc * 128:(fc + 1) * 128], h1[:, fc * 128:(fc + 1) * 128], identB[:])) for fc in range(2)], [h1, identB], pt)
        h1T = ring("ex_h1T", 4, [128, 2, 128], BF16)
        A(lambda e: e.copy(h1T[:], pt[:, 0:256].rearrange("p (c t) -> p c t", t=128)), [pt], [h1T])
        stt[i][1] = h1T

    def stC(i):
        j, s_ = divmod(i, 4)
        wbf, xst = wcur[j]
        h1T = stt[i][1]
        ysb = ring("ysb", 3, [128, D], BF16)
        for hf in range(2):
            p2 = PB()
            k.pe([(lambda e, fc=fc: e.matmul(p2[:], h1T[:, fc, :], wbf[:, 4096 + fc * 1024 + hf * 512:4096 + fc * 1024 + (hf + 1) * 512],
                                            start=(fc == 0), stop=(fc == 1))) for fc in range(2)], [h1T, wbf], p2)
            V(lambda e: e.tensor_copy(ysb[:, hf * 512:(hf + 1) * 512], p2[:]), [p2], [ysb])
        k.dma("sp", Ys[i * 128:(i + 1) * 128, :], ysb[:], reads=[ysb])
        del stt[i]

    for i in range(NSUB + 2):
        if i < NSUB:
            stA(i)
        if 0 <= i - 1 < NSUB:
            stB(i - 1)
        if 0 <= i - 2 < NSUB:
            stC(i - 2)
    k.barrier()

    ybs = {}

    def gath(g_):
        yb_ = ring("ybuf", 2, [128, NA, D], BF16)
        for j in range(NA):
            k.idma(yb_[:, j, :], None, Ys[:, :], bass.IndirectOffsetOnAxis(ap=slall[:, g_, j:j + 1], axis=0), reads=[slall], writes=[yb_], stream=yb_,
                   nowaw=(j > 0))
        ybs[g_] = yb_

    gath(0)
    for gti in range(TOT):
        b = gti // NT
        ti = gti % NT
        load_rows(b)
        if gti + 1 < TOT:
            gath(gti + 1)
        yb = ybs.pop(gti)
        xt = XT()
        k.dma("sp", xt[:], x1s[b, ti * 128:(ti + 1) * 128, :], writes=[xt])
        V(lambda e: e.tensor_scalar(xh[:], yb[:, 0, :], gjall[:, gti, 0:1], None, ALU.mult), [yb, gjall], [xh])
        for j in range(1, NA):
            V(lambda e: e.scalar_tensor_tensor(xh[:], yb[:, j, :], gjall[:, gti, j:j + 1], xh[:], ALU.mult, ALU.add), [yb, gjall, xh], [xh])
        V(lambda e: e.tensor_tensor(u1[:], xh[:], rowsb[2][:], ALU.mult), [xh, rowsb[2]], [u1])
        V(lambda e: e.scalar_tensor_tensor(u1[:], xt[:], ALPHA, u1[:], ALU.mult, ALU.add), [xt, u1], [u1])
        rstd, nmr = ln_stats(u1, 1e-5)
        x2 = X1()
        A(lambda e: e.activation(x2[:], u1[:], AF.Identity, bias=nmr[:, 0:1], scale=rstd[:, 0:1]), [u1, rstd, nmr], [x2])
        V(lambda e: e.tensor_tensor(x2[:], x2[:], ln2w[:], ALU.mult), [x2, ln2w], [x2])
        V(lambda e: e.tensor_tensor(x2[:], x2[:], ln2b[:], ALU.add), [x2, ln2b], [x2])
        k.dma("sp", out[b, ti * 128:(ti + 1) * 128, :], x2[:], reads=[x2], stream=x2)
    k.barrier()
    moe_scope.close()
    build.ninstr = k.ninstr
    return nc


def host_constants(N, R):
    GRID_W = 64
    rows = N // GRID_W
    row = np.repeat(np.arange(rows), GRID_W).astype(np.float32)
    col = np.tile(np.arange(GRID_W), rows).astype(np.float32)
    freqs = (10000.0 ** (-np.arange(16, dtype=np.float32) / 16)).astype(np.float32)
    ar = row[None, :] * freqs[:, None]
    ac = col[None, :] * freqs[:, None]
    ang = np.concatenate([ar, ar, ac, ac], axis=0)
    sign = np.concatenate([-np.ones(16), np.ones(16), -np.ones(16), np.ones(16)]).astype(np.float32)[:, None]
    cos64 = np.cos(ang).astype(np.float32)
    sin64 = (np.sin(ang) * sign).astype(np.float32)
    cosT = np.concatenate([cos64, cos64], 0)
    sinT = np.concatenate([sin64, sin64], 0)
    j = np.arange(128)[:, None]
    t = np.arange(128)[None, :]
    same = (j // 64) == (t // 64)
    mask_f = (same & (j <= t)).astype(np.float32)
    mask_b = (same & (j >= t)).astype(np.float32)
    negm_prev = np.where(j >= t, 0.0, NEG).astype(np.float32)
    negm_next = np.where(j <= t, 0.0, NEG).astype(np.float32)
    rst64 = np.ones((128, 512), np.float32); rst64[:, ::64] = 0
    rst16 = np.ones((128, 512), np.float32); rst16[:, ::16] = 0
    sel_rows = np.zeros((R, R, 128), np.float32)
    for r in range(R):
        sel_rows[r, r, :] = 1.0
    import ml_dtypes
    extra = dict(zrows=np.zeros((512, D), ml_dtypes.bfloat16),
                 ramp=np.ascontiguousarray(np.broadcast_to((512.0 * np.arange(256, dtype=np.float32))[None, :], (128, 256))),
                 jv=np.ascontiguousarray(np.broadcast_to(np.arange(1, 10, dtype=np.float32)[None, :], (128, 9))),
                 pcol=np.arange(128, dtype=np.float32).reshape(128, 1),
                 ustrict=(np.arange(128)[:, None] < np.arange(128)[None, :]).astype(np.float32))
    return dict(**extra, ident_f=np.eye(128, dtype=np.float32), cosT=np.ascontiguousarray(cosT), sinT=np.ascontiguousarray(sinT),
                mask_f=mask_f, mask_b=mask_b, negm_prev=negm_prev, negm_next=negm_next, rst64=rst64, rst16=rst16, sel_rows=sel_rows)


def rope_perm():
    p = np.arange(64)
    p[0:16] += 16
    p[16:32] -= 16
    p[32:48] += 16
    p[48:64] -= 16
    return p


def expert_weight_rows(inp):
    g = np.concatenate([np.asarray(inp["exp_w_gate"][0]), np.asarray(inp["shared_w_gate"])], 0)
    u = np.concatenate([np.asarray(inp["exp_w_up"][0]), np.asarray(inp["shared_w_up"])], 0)
    d = np.concatenate([np.asarray(inp["exp_w_down"][0]), np.asarray(inp["shared_w_down"])], 0)
    ne = g.shape[0]
    g4 = g.reshape(ne, 8, 128, 256).transpose(0, 2, 1, 3)
    u4 = u.reshape(ne, 8, 128, 256).transpose(0, 2, 1, 3)
    gu = np.concatenate([g4, u4], axis=3).reshape(ne, 128, 4096)
    d3 = d.reshape(ne, 2, 128, 1024).transpose(0, 2, 1, 3).reshape(ne, 128, 2048)
    return np.ascontiguousarray(np.concatenate([gu, d3], axis=2).reshape(ne * 128, 6144))


def shared_inputs(inp):
    w_in = np.asarray(inp["w_in"][0])
    zq, zff, zfb, zi, zg = [w_in[:, i * 512:(i + 1) * 512] for i in range(5)]
    aq = w_in[:, 2560:3072]; ak = w_in[:, 3072:3200]; av = w_in[:, 3200:3328]
    perm = rope_perm()
    hord = [0, 4, 1, 5, 2, 6, 3, 7]
    aq8 = aq.reshape(D, 8, 64)
    aqo = aq8[:, hord, :].reshape(D, 512)
    aqp = aq8[:, :, perm][:, hord, :].reshape(D, 512)
    akp = ak.reshape(D, 2, 64)[:, :, perm].reshape(D, 128)
    w_fm = np.ascontiguousarray(np.concatenate([zq, zff, zfb, aqo, aqp, ak, akp], axis=1))
    w_tm = np.ascontiguousarray(np.concatenate([zi, zg, av], axis=1))
    rep = lambda v: np.ascontiguousarray(np.broadcast_to(np.asarray(v).reshape(1, -1), (128, np.asarray(v).size)))
    lb = np.stack([np.asarray(inp["hg_lb_fwd"]), np.asarray(inp["hg_lb_bwd"])], 0)
    lbT = np.ascontiguousarray(lb.reshape(2, 2, 4, 128).transpose(3, 0, 1, 2))
    b_ada = np.asarray(inp["b_ada"][0])
    sh = dict(
        w_ada=np.asarray(inp["w_ada"][0]), b_adaT=np.ascontiguousarray(b_ada.reshape(48, 128).T),
        b_ada_g=np.ascontiguousarray(np.stack([b_ada[2048:3072], b_ada[3072:4096], b_ada[4096:5120], b_ada[5120:6144]], 0)),
        w_fm=w_fm, w_tm=w_tm, lbT=lbT, normw_b=rep(inp["hg_norm_w"][0]), sink_b=rep(inp["attn_sink"][0]),
        w_out=np.asarray(inp["w_out"][0]),
        ln1w_b=rep(inp["ln1_w"][0]), ln1b_b=rep(inp["ln1_b"][0]), ln2w_b=rep(inp["ln2_w"][0]), ln2b_b=rep(inp["ln2_b"][0]),
        router_w=np.asarray(inp["router_w"][0]), rbias_b=rep(inp["router_bias"][0]),
        ew_all=expert_weight_rows(inp),
    )
    return sh


def core_inputs(inp, b0, NB):
    x = np.ascontiguousarray(np.asarray(inp["x"][b0:b0 + NB]))
    ctx = np.ascontiguousarray(np.asarray(inp["ctx"][b0:b0 + NB]))
    c3 = np.concatenate([np.asarray(inp["c"][b0:b0 + NB]), np.asarray(inp["c_ctx"])[None, :]], 0)
    return dict(x=x, ctx=ctx, c3T=np.ascontiguousarray(c3.T))


def kernel(**inputs):
    B, N, _ = inputs["x"].shape
    L = inputs["ctx"].shape[1]
    ncores = 8
    NB = B // ncores
    nc = build(NB, N, L, TBT=16)
    sh = shared_inputs(inputs)
    sh.update(host_constants(N, NB + 1))
    in_maps = []
    for c in range(ncores):
        m = dict(sh)
        m.update(core_inputs(inputs, c * NB, NB))
        in_maps.append(m)
    res = run_bass_kernel_spmd(nc, in_maps, core_ids=list(range(ncores)))
    return np.concatenate([np.asarray(r["out"]) for r in res.results], axis=0).astype(np.float32)
```

```python
import numpy as np
import concourse.bass as bass
import concourse.mybir as mybir
from concourse.bass_utils import run_bass_kernel_spmd

F32 = mybir.dt.float32
BF16 = mybir.dt.bfloat16
AF = mybir.ActivationFunctionType
ALU = mybir.AluOpType
AX = mybir.AxisListType

D = 1024
NE = 64
ALPHA = 2.0 ** 0.25
NEG = -30000.0


class Buf:
    __slots__ = ("t", "name", "lw", "rd", "dsem")

    def __init__(self, t, name):
        self.t = t
        self.name = name
        self.lw = None
        self.rd = {}
        self.dsem = None

    def __getitem__(self, idx):
        return self.t[idx]


class KB:
    def __init__(self, nc):
        self.nc = nc
        self.eng = dict(pe=nc.tensor, act=nc.scalar, dve=nc.vector, pool=nc.gpsimd, sp=nc.sync)
        self.sems = {}
        self.cnt = {}
        self.seen = {e: {} for e in self.eng}
        for e in ("pe", "act", "dve", "pool"):
            self._mksem("E_" + e)
        self.nbuf = 0
        self.ninstr = 0

    def _mksem(self, key):
        self.sems[key] = self.nc.alloc_semaphore(key)
        self.cnt[key] = 0
        return key

    def sb(self, shape, dtype, name=None):
        self.nbuf += 1
        name = name or f"sb{self.nbuf}"
        if getattr(self, "scope", None) is not None:
            return Buf(self.scope.enter_context(self.nc.sbuf_tensor(name, list(shape), dtype)), name)
        return Buf(self.nc.alloc_sbuf_tensor(name, list(shape), dtype), name)

    def barrier(self):
        deps = [(kk, v) for kk, v in self.cnt.items() if v > 0]
        for e in self.eng:
            self._need(e, deps)

    def ps(self, shape, dtype, name=None):
        self.nbuf += 1
        name = name or f"ps{self.nbuf}"
        return Buf(self.nc.alloc_psum_tensor(name, list(shape), dtype), name)

    def _need(self, e, deps):
        seen = self.seen[e]
        best = {}
        for d in deps:
            if d is None:
                continue
            k, v = d
            if seen.get(k, 0) >= v:
                continue
            if best.get(k, 0) < v:
                best[k] = v
        for k, v in best.items():
            self.eng[e].wait_ge(self.sems[k], v)
            seen[k] = v

    def _deps(self, reads, writes):
        deps = []
        for b in reads:
            deps.append(b.lw)
        for b in writes:
            deps.append(b.lw)
            for k, v in b.rd.items():
                deps.append((k, v))
        return deps

    def _record(self, reads, writes, tok):
        k, v = tok
        for b in reads:
            if b.rd.get(k, 0) < v:
                b.rd[k] = v
        for b in writes:
            b.lw = tok
            b.rd = {}

    def op(self, e, ins_fn, reads=(), writes=()):
        self._need(e, self._deps(reads, writes))
        ins = ins_fn(self.eng[e])
        k = "E_" + e
        self.cnt[k] += 1
        ins.then_inc(self.sems[k], 1)
        self._record(reads, writes, (k, self.cnt[k]))
        self.ninstr += 1
        return ins

    def pe(self, fns, reads, out):
        self._need("pe", self._deps(reads, [out]))
        n = len(fns)
        k = "E_pe"
        for i, f in enumerate(fns):
            ins = f(self.nc.tensor)
            self.ninstr += 1
            if i == n - 1:
                self.cnt[k] += 1
                ins.then_inc(self.sems[k], 1)
        self._record(reads, [out], (k, self.cnt[k]))

    def dma(self, q, out_ap, in_ap, reads=(), writes=(), stream=None, nowaw=False, **kw):
        sbuf = stream or (writes[0] if writes else reads[0])
        if sbuf.dsem is None:
            sbuf.dsem = self._mksem("D_" + sbuf.name)
        k = sbuf.dsem
        if nowaw and all(b.lw is not None and b.lw[0] == k and not b.rd for b in writes):
            self._need(q, self._deps(reads, []))
        else:
            self._need(q, self._deps(reads, writes))
        ins = self.eng[q].dma_start(out=out_ap, in_=in_ap, **kw)
        self.cnt[k] += 16
        ins.then_inc(self.sems[k], 16)
        self._record(reads, writes, (k, self.cnt[k]))
        self.ninstr += 1
        return ins

    def idma(self, out_ap, out_off, in_ap, in_off, reads=(), writes=(), stream=None, nowaw=False):
        sbuf = stream
        if sbuf.dsem is None:
            sbuf.dsem = self._mksem("D_" + sbuf.name)
        k = sbuf.dsem
        if nowaw and all(b.lw is not None and b.lw[0] == k and not b.rd for b in writes):
            self._need("pool", self._deps(reads, []))
        else:
            self._need("pool", self._deps(reads, writes))
        ins = self.nc.gpsimd.indirect_dma_start(out=out_ap, out_offset=out_off, in_=in_ap, in_offset=in_off)
        self.cnt[k] += 16
        ins.then_inc(self.sems[k], 16)
        self._record(reads, writes, (k, self.cnt[k]))
        self.ninstr += 1
        return ins

    def wait_all(self, e, bufs):
        deps = []
        for b in bufs:
            deps.append(b.lw)
            for k, v in b.rd.items():
                deps.append((k, v))
        self._need(e, deps)


def bc(ap, axis, shape):
    return ap.unsqueeze(axis).broadcast_to(list(shape))


import contextlib

FM_COLS = 512 * 5 + 128 * 2
TM_COLS = 512 * 2 + 128
FM_OFF = dict(zq=0, zff=512, zfb=1024, aq=1536, aqp=2048, ak=2560, akp=2688)
FM_W = dict(zq=512, zff=512, zfb=512, aq=512, aqp=512, ak=128, akp=128)
TM_OFF = dict(zi=0, zg=512, av=1024)
TM_W = dict(zi=512, zg=512, av=128)
BLK = 256
DEBUG = False
BT = BLK // 128


def build(NB, N, L, TBT=16):
    nc = bass.Bass("TRN2", target_bir_lowering=False)
    k = KB(nc)
    k.scope = None
    R = NB + 1
    NT = N // 128
    NBLK = N // BLK
    TOT = NB * NT
    assert TOT % TBT == 0 and L % 128 == 0 and L <= BLK

    def din(name, shape, dt=F32):
        return nc.dram_tensor(name, list(shape), dt, kind="ExternalInput").ap()

    x = din("x", [NB, N, D]); ctx = din("ctx", [NB, L, D]); c3T = din("c3T", [D, R])
    w_ada = din("w_ada", [D, 6 * D]); b_adaT = din("b_adaT", [128, 48]); bview = din("b_ada_g", [4, D])
    w_fm = din("w_fm", [D, FM_COLS]); w_tm = din("w_tm", [D, TM_COLS])
    lbT = din("lbT", [128, 2, 2, 4]); normw_b = din("normw_b", [128, 512]); sink_b = din("sink_b", [128, 8])
    w_out = din("w_out", [D, D])
    ln1w_b = din("ln1w_b", [128, D]); ln1b_b = din("ln1b_b", [128, D])
    ln2w_b = din("ln2w_b", [128, D]); ln2b_b = din("ln2b_b", [128, D])
    router_w = din("router_w", [D, NE]); rbias_b = din("rbias_b", [128, NE])
    ew_all = din("ew_all", [(NE + 1) * 128, 6144])
    NA = 9
    NTL = (TOT * 128 * NA) // 512 + NE + 1
    NSLOT = NTL * 512
    zrows = din("zrows", [512, D], BF16); ramp_d = din("ramp", [128, 256]); jv_d = din("jv", [128, NA]); pcol_d = din("pcol", [128, 1])
    ustrict_d = din("ustrict", [128, 128])
    ident_f = din("ident_f", [128, 128]); cosT = din("cosT", [128, N]); sinT = din("sinT", [128, N])
    mask_f = din("mask_f", [128, 128]); mask_b = din("mask_b", [128, 128])
    negm_prev = din("negm_prev", [128, 128]); negm_next = din("negm_next", [128, 128])
    rst64 = din("rst64", [128, 512]); rst16 = din("rst16", [128, 512]); sel_rows = din("sel_rows", [R, R, 128])
    out = nc.dram_tensor("out", [NB, N, D], F32, kind="ExternalOutput").ap()
    x1s = nc.dram_tensor("x1s", [NB, N, D], F32, kind="Internal").ap()
    obs = nc.dram_tensor("obs", [NB, N, 512], F32, kind="Internal").ap()
    wog = nc.dram_tensor("wog", [NB, D, D], BF16, kind="Internal").ap()
    rws = nc.dram_tensor("rws", [NB, 3, 128, D], F32, kind="Internal").ap()
    Xs = nc.dram_tensor("Xs", [NSLOT, D], BF16, kind="Internal").ap()
    Ys = nc.dram_tensor("Ys", [NSLOT, D], BF16, kind="Internal").ap()
    H2 = nc.dram_tensor("H2", [TOT * 128, D], BF16, kind="Internal").ap()

    pbs = [k.ps([128, 512], F32, f"pb{i}") for i in range(6)]
    pts = [k.ps([128, 1024], BF16, f"pt{i}") for i in range(2)]
    st = {"pb": 0, "pt": 0, "rr": {}}

    def PB():
        while True:
            st["pb"] = (st["pb"] + 1) % len(pbs)
            if st["pb"] not in st.get("excl", ()):
                return pbs[st["pb"]]

    def PT():
        st["pt"] = (st["pt"] + 1) % len(pts)
        return pts[st["pt"]]

    def ring(name, n, shape, dt):
        key = (name, id(k.scope))
        if key not in st["rr"]:
            st["rr"][key] = [[k.sb(shape, dt, f"{name}_{len(st['rr'])}_{i}") for i in range(n)], 0]
        ent = st["rr"][key]
        ent[1] = (ent[1] + 1) % n
        return ent[0][ent[1]]

    V = lambda fn, r=(), w=(): k.op("dve", fn, r, w)
    A = lambda fn, r=(), w=(): k.op("act", fn, r, w)
    G = lambda fn, r=(), w=(): k.op("pool", fn, r, w)

    def const(src, shape, dt=F32, q="sp", name=None):
        b = k.sb(shape, F32, name + "_f")
        k.dma(q, b[:], src, writes=[b])
        if dt == F32:
            return b
        b2 = k.sb(shape, dt, name + "_b")
        V(lambda e: e.tensor_copy(b2[:], b[:]), [b], [b2])
        return b2

    identF = const(ident_f, [128, 128], F32, name="identF")
    identB = k.sb([128, 128], BF16, "identB")
    V(lambda e: e.tensor_copy(identB[:], identF[:]), [identF], [identB])
    mod = k.sb([128, 48, R], F32, "mod")
    modp1 = k.sb([128, 48, R], F32, "modp1")
    xts_ring = [k.sb([128, D], F32, f"xt{i}") for i in range(3)]
    xst = [0]

    def XT():
        xst[0] = (xst[0] + 1) % 3
        return xts_ring[xst[0]]

    xh = k.sb([128, D], F32, "xh")
    u1 = k.sb([128, D], F32, "u1")
    x1r = [k.sb([128, D], F32, f"x1r{i}") for i in range(2)]
    x1i = [0]

    def X1():
        x1i[0] = (x1i[0] + 1) % 2
        return x1r[x1i[0]]

    lnst = k.sb([128, 2, 6], F32, "lnst"); lnmv = k.sb([128, 2], F32, "lnmv")
    lnrs = k.sb([128, 1], F32, "lnrs"); lnnm = k.sb([128, 1], F32, "lnnm")

    ewb = nc.dram_tensor("ewb", [(NE + 1) * 128, 6144], BF16, kind="Internal").ap()
    ctok = k.sb([1, 2], F32, "ctok")
    ztok = k.sb([1, 2], F32, "ztok")
    bg_list = [("c", ex) for ex in range(NE + 1)]
    bg_pos = [0]

    def background_dmas(n):
        for _ in range(n):
            if bg_pos[0] >= len(bg_list):
                return
            kind, i = bg_list[bg_pos[0]]
            bg_pos[0] += 1
            if kind == "c":
                k.dma("pool", ewb[i * 128:(i + 1) * 128, :], ew_all[i * 128:(i + 1) * 128, :], reads=[ctok], stream=ctok)
            else:
                k.dma("pool", Xs[i * 512:(i + 1) * 512, :], zrows, reads=[ztok], stream=ztok)

    k.scope = contextlib.ExitStack()
    selr = k.sb([R, R, 128], F32, "selr")
    k.dma("sp", selr[:], sel_rows, writes=[selr])
    c3 = k.sb([128, 8, R], F32, "c3")
    k.dma("sp", c3[:], c3T.rearrange("(kc p) r -> p kc r", p=128), writes=[c3])
    sC = k.sb([128, 8, R], F32, "sC")
    A(lambda e: e.activation(sC[:], c3[:], AF.Exp, scale=-1.0), [c3], [sC])
    V(lambda e: e.tensor_scalar(sC[:], sC[:], 1.0, None, ALU.add), [sC], [sC])
    V(lambda e: e.reciprocal(sC[:], sC[:]), [sC], [sC])
    V(lambda e: e.tensor_tensor(sC[:], sC[:], c3[:], ALU.mult), [sC, c3], [sC])
    badaT = k.sb([128, 48], F32, "badaT")
    k.dma("sp", badaT[:], b_adaT, writes=[badaT])
    modps = PB()
    st["excl"] = {st["pb"]}
    grow = k.sb([R, 4, D], F32, "grow")
    for cc in range(12):
        wa = ring("wa", 2, [128, 8, 512], F32)
        k.dma("act", wa[:], w_ada.rearrange("(kc p) n -> p kc n", p=128)[:, :, cc * 512:(cc + 1) * 512], writes=[wa])
        for j in range(4):
            jj = cc * 4 + j
            k.pe([(lambda e, kc=kc: e.matmul(modps[:, jj * R:(jj + 1) * R], wa[:, kc, j * 128:(j + 1) * 128], sC[:, kc, :],
                                           start=(kc == 0), stop=(kc == 7))) for kc in range(8)], [wa, sC], modps)
        if 4 <= cc <= 11:
            gi = (cc - 4) // 2
            hf = cc % 2
            pr = PB()
            k.pe([(lambda e, kc=kc: e.matmul(pr[0:R, :], sC[:, kc, :], wa[:, kc, :], start=(kc == 0), stop=(kc == 7)))
                  for kc in range(8)], [wa, sC], pr)
            V(lambda e: e.tensor_copy(grow[:, gi, hf * 512:(hf + 1) * 512], pr[0:R, :]), [pr], [grow])
    V(lambda e: e.tensor_tensor(mod[:], modps[:, 0:48 * R].rearrange("p (a r) -> p a r", r=R), bc(badaT[:], 2, [128, 48, R]), ALU.add),
      [modps, badaT], [mod])
    V(lambda e: e.tensor_scalar(modp1[:], mod[:], 1.0, None, ALU.add), [mod], [modp1])
    st["excl"] = set()
    badar = k.sb([R, 4, D], F32, "badar")
    for r in range(R):
        k.dma("sp", badar[r:r + 1, :, :], bview.rearrange("(o g) d -> o g d", o=1), writes=[badar])
    V(lambda e: e.tensor_tensor(grow[:], grow[:], badar[:], ALU.add), [grow, badar], [grow])
    grbt = k.sb([128, D], F32, "grbt")
    wob = k.sb([128, D], BF16, "wob")
    for b in range(NB):
        for gi in range(4):
            for hf in range(2):
                pr = PB()
                k.pe([lambda e: e.matmul(pr[:], selr[:, b, :], grow[:, gi, hf * 512:(hf + 1) * 512], start=True, stop=True)],
                     [selr, grow], pr)
                V(lambda e: e.tensor_copy(grbt[:, hf * 512:(hf + 1) * 512], pr[:]), [pr], [grbt])
            if gi >= 1:
                if gi == 2:
                    V(lambda e: e.tensor_scalar(grbt[:], grbt[:], 1.0, None, ALU.add), [grbt], [grbt])
                k.dma("sp", rws[b, gi - 1], grbt[:], reads=[grbt])
            else:
                for kc in range(8):
                    xt = XT()
                    k.dma("sp", xt[:], w_out[kc * 128:(kc + 1) * 128, :], writes=[xt])
                    V(lambda e: e.tensor_tensor(wob[:], xt[:], grbt[:], ALU.mult), [xt, grbt], [wob])
                    k.dma("sp", wog[b, kc * 128:(kc + 1) * 128, :], wob[:], reads=[wob])
    k.barrier()
    k.scope.close()

    k.scope = contextlib.ExitStack()
    maskF = const(mask_f, [128, 128], name="maskF"); maskB = const(mask_b, [128, 128], name="maskB")
    negP = const(negm_prev, [128, 128], BF16, name="negP"); negN = const(negm_next, [128, 128], BF16, name="negN")
    r64 = const(rst64[:, 0:BLK], [128, BLK], name="r64"); r16 = const(rst16[:, 0:BLK], [128, BLK], name="r16")
    normw = const(normw_b, [128, 512], name="normw")
    ln1w = const(ln1w_b, [128, D], name="ln1w"); ln1b = const(ln1b_b, [128, D], name="ln1b")
    esink = k.sb([128, 8], F32, "esink")
    k.dma("sp", esink[:], sink_b, writes=[esink])
    A(lambda e: e.activation(esink[:], esink[:], AF.Exp), [esink], [esink])
    lbt = k.sb([128, 2, 2, 4], F32, "lbt")
    k.dma("sp", lbt[:], lbT, writes=[lbt])
    oml = k.sb([128, 2, 4], F32, "oml"); noml = k.sb([128, 2, 4], F32, "noml")
    V(lambda e: e.tensor_tensor(oml[:], lbt[:, :, 0, :], lbt[:, :, 1, :], ALU.subtract), [lbt], [oml])
    A(lambda e: e.activation(oml[:], oml[:], AF.Exp), [oml], [oml])
    V(lambda e: e.tensor_scalar(oml[:], oml[:], 1.0, None, ALU.add), [oml], [oml])
    V(lambda e: e.reciprocal(oml[:], oml[:]), [oml], [oml])
    V(lambda e: e.tensor_scalar(noml[:], oml[:], -1.0, None, ALU.mult), [oml], [noml])

    W = k.sb([128, 8, 4096], BF16, "W")
    wmap = {}

    def loadw(fm_names, tm_names, with_wo=None):
        wmap.clear()
        col = 0
        eng_i = [0]

        def stage(src_rows, c0, wd_, kc, dcol):
            xt = XT()
            q = "sp" if eng_i[0] % 2 == 0 else "act"
            k.dma(q, xt[:, 0:wd_], src_rows[:, c0:c0 + wd_], writes=[xt])
            e = ("dve", "act", "pool")[eng_i[0] % 3]
            eng_i[0] += 1
            if e == "act":
                A(lambda en: en.copy(W[:, kc, dcol:dcol + wd_], xt[:, 0:wd_]), [xt], [W])
            elif e == "dve":
                V(lambda en: en.tensor_copy(W[:, kc, dcol:dcol + wd_], xt[:, 0:wd_]), [xt], [W])
            else:
                G(lambda en: en.tensor_copy(W[:, kc, dcol:dcol + wd_], xt[:, 0:wd_]), [xt], [W])

        for nm in fm_names:
            wmap[nm] = col
            col += FM_W[nm]
        for nm in tm_names:
            wmap[nm] = col
            col += TM_W[nm]
        for kc in range(8):
            for (src, OFF, WID, names) in ((w_fm, FM_OFF, FM_W, fm_names), (w_tm, TM_OFF, TM_W, tm_names)):
                for nm in names:
                    stage(src[kc * 128:(kc + 1) * 128, :], OFF[nm], WID[nm], kc, wmap[nm])
        if with_wo is not None:
            for kc in range(8):
                k.dma("sp" if kc % 2 == 0 else "act", W[:, kc, col:col + D], wog[with_wo, kc * 128:(kc + 1) * 128, :], writes=[W])
            wmap["wo"] = col
            col += D
        assert col <= 4096

    def ln_stats(src, eps):
        for a_ in range(2):
            V(lambda e: e.bn_stats(lnst[:, a_, :], src[:, a_ * 512:(a_ + 1) * 512]), [src], [lnst])
        V(lambda e: e.bn_aggr(lnmv[:], lnst[:].rearrange("p a b -> p (a b)")), [lnst], [lnmv])
        A(lambda e: e.activation(lnrs[:], lnmv[:, 1:2], AF.Ln, bias=float(eps)), [lnmv], [lnrs])
        A(lambda e: e.activation(lnrs[:], lnrs[:], AF.Exp, scale=-0.5), [lnrs], [lnrs])
        V(lambda e: e.tensor_scalar(lnnm[:], lnmv[:, 0:1], lnrs[:, 0:1], -1.0, ALU.mult, ALU.mult), [lnmv, lnrs], [lnnm])
        return lnrs, lnnm

    def ln_mod_T(src, r, sh_c, sc_c, dst, dcol, dstf=None):
        rstd, nmr = ln_stats(src, 1e-6)
        A(lambda e: e.activation(xh[:], src[:], AF.Identity, bias=nmr[:, 0:1], scale=rstd[:, 0:1]), [src, rstd, nmr], [xh])
        for hf in range(2):
            pp = PB()
            k.pe([(lambda e, j=j: e.transpose(pp[:, j * 128:(j + 1) * 128], xh[:, (hf * 4 + j) * 128:(hf * 4 + j + 1) * 128], identF[:]))
                  for j in range(4)], [xh, identF], pp)
            for j in range(4):
                kc = hf * 4 + j
                if dstf is not None:
                    A(lambda e: e.activation(dstf[:, kc, :], pp[:, j * 128:(j + 1) * 128], AF.Identity,
                                             bias=mod[:, sh_c + kc, r:r + 1], scale=modp1[:, sc_c + kc, r:r + 1]), [pp, mod, modp1], [dstf])
                elif j % 2 == 0:
                    A(lambda e: e.activation(dst[:, kc, dcol:dcol + 128], pp[:, j * 128:(j + 1) * 128], AF.Identity,
                                             bias=mod[:, sh_c + kc, r:r + 1], scale=modp1[:, sc_c + kc, r:r + 1]), [pp, mod, modp1], [dst])
                else:
                    V(lambda e: e.tensor_scalar(dst[:, kc, dcol:dcol + 128], pp[:, j * 128:(j + 1) * 128],
                                                modp1[:, sc_c + kc, r:r + 1], mod[:, sh_c + kc, r:r + 1], ALU.mult, ALU.add), [pp, mod, modp1], [dst])
        if dstf is not None and dst is not None:
            G(lambda e: e.tensor_copy(dst[:, :, dcol:dcol + 128], dstf[:]), [dstf], [dst])

    def proj_fm(hT, T, nm, j, dstps):
        col = wmap[nm] + j * 128
        k.pe([(lambda e, kc=kc: e.matmul(dstps[:, 0:T], W[:, kc, col:col + 128], hT[:, kc, 0:T], start=(kc == 0), stop=(kc == 7)))
              for kc in range(8)], [W, hT], dstps)

    def proj_tm(hT, ti, nm, ncol, dstps):
        col = wmap[nm]
        k.pe([(lambda e, kc=kc: e.matmul(dstps[:, 0:ncol], hT[:, kc, ti * 128:(ti + 1) * 128], W[:, kc, col:col + ncol],
                                        start=(kc == 0), stop=(kc == 7))) for kc in range(8)], [W, hT], dstps)

    Dpad = [k.sb([128, BT, 4, 2, 64], F32, f"Dpad{d}") for d in range(2)]
    Rref = [k.sb([128, 2 * BT, 4], F32, f"Rref{d}") for d in range(2)]
    for d in range(2):
        V(lambda e: e.memset(Dpad[d][:], 0.0), [], [Dpad[d]])
        V(lambda e: e.memset(Rref[d][:], 0.0), [], [Rref[d]])
    khat = [k.sb([128, BT, 4, 2, 64], BF16, f"khat{h}") for h in range(4)]
    qhat = [k.sb([128, BLK], BF16, f"qhat{h}") for h in range(4)]
    qd = [k.sb([128, BT, 2, 2, 64], BF16, f"qd{h}") for h in range(4)]
    for h in range(4):
        V(lambda e: e.memset(qd[h][:], 0.0), [], [qd[h]])
    kdT = k.sb([128, BT, 4, 128], BF16, "kdT")
    sdec = k.sb([128, 4, 2 * BT], F32, "sdec")
    vtm = k.sb([128, BT, 512], BF16, "vtm")
    Sst = [k.sb([128, 4, 128], F32, f"Sst{d}") for d in range(2)]
    kTs = k.sb([128, N], BF16, "kTs"); kcs = k.sb([128, L], BF16, "kcs")
    vaug = k.sb([128, NT, 2, 66], BF16, "vaug"); vcaug = k.sb([128, L // 128, 2, 66], BF16, "vcaug")
    V(lambda e: e.memset(vaug[:], 1.0), [], [vaug]); V(lambda e: e.memset(vcaug[:], 1.0), [], [vcaug])
    Sbfs = [k.sb([128, 4, 128], BF16, f"Sbf{i}") for i in range(4)]
    sbi = [0]

    def SBF():
        sbi[0] = (sbi[0] + 1) % 4
        return Sbfs[sbi[0]]

    T1 = lambda nm: ring(nm, 1, [128, BLK], F32)

    def hgrn_prep(hT, T, d, dosc=True):
        C = T // 64
        NTl = T // 128
        for h in range(4):
            zf = PB()
            proj_fm(hT, T, "zff" if d == 0 else "zfb", h, zf)
            e1 = T1("hp_e1")
            A(lambda e: e.activation(e1[:, 0:T], zf[:, 0:T], AF.Exp), [zf], [e1])
            rr = T1("hp_rr")
            V(lambda e: e.tensor_scalar(rr[:, 0:T], e1[:, 0:T], 1.0, None, ALU.add), [e1], [rr])
            V(lambda e: e.reciprocal(rr[:, 0:T], rr[:, 0:T]), [rr], [rr])
            g = T1("hp_g")
            A(lambda e: e.activation(g[:, 0:T], rr[:, 0:T], AF.Ln, bias=1.0, scale=noml[:, d, h:h + 1]), [rr, noml], [g])
            G(lambda e: e.tensor_scalar(g[:, 0:T], g[:, 0:T], -4.0, None, ALU.max), [g], [g])
            bcm = T1("hp_bc")
            V(lambda e: e.tensor_tensor_scan(bcm[:, 0:T], r64[:, 0:T], g[:, 0:T], 0.0, ALU.mult, ALU.add), [r64, g], [bcm])
            b3 = bcm[:, 0:T].rearrange("p (c j) -> p c j", j=64)
            A(lambda e: e.activation(sdec[:, h, 0:C], b3[:, :, 63], AF.Exp), [bcm], [sdec])
            dk = T1("hp_dk")
            dk3 = dk[:, 0:T].rearrange("p (c j) -> p c j", j=64)
            if d == 0:
                V(lambda e: e.tensor_tensor(dk3, bc(b3[:, :, 63], 2, [128, C, 64]), b3, ALU.subtract), [bcm], [dk])
            else:
                V(lambda e: e.tensor_tensor(dk[:, 0:T], bcm[:, 0:T], g[:, 0:T], ALU.subtract), [bcm, g], [dk])
            A(lambda e: e.activation(dk[:, 0:T], dk[:, 0:T], AF.Exp), [dk], [dk])
            kd = ring("hp_kd", 1, [128, BLK], BF16)
            V(lambda e: e.scalar_tensor_tensor(kd[:, 0:T], rr[:, 0:T], oml[:, d, h:h + 1], dk[:, 0:T], ALU.mult, ALU.mult), [rr, oml, dk], [kd])
            pk2 = PT()
            k.pe([(lambda e, ti=ti: e.transpose(pk2[:, ti * 128:(ti + 1) * 128], kd[:, ti * 128:(ti + 1) * 128], identB[:])) for ti in range(NTl)],
                 [kd, identB], pk2)
            A(lambda e: e.copy(kdT[:, 0:NTl, h, :], pk2[:, 0:NTl * 128].rearrange("p (t d) -> p t d", d=128)), [pk2], [kdT])
            if not dosc:
                continue
            zq = PB()
            proj_fm(hT, T, "zq", h, zq)
            lcm = T1("hp_lc")
            V(lambda e: e.tensor_tensor_scan(lcm[:, 0:T], r16[:, 0:T], g[:, 0:T], 0.0, ALU.mult, ALU.add), [r16, g], [lcm])
            Pq = bcm
            lq = lcm
            if d == 1:
                u = T1("hp_u")
                u3 = u[:, 0:T].rearrange("p (c j) -> p c j", j=64)
                V(lambda e: e.tensor_tensor(u3, bc(b3[:, :, 63], 2, [128, C, 64]), b3, ALU.subtract), [bcm], [u])
                V(lambda e: e.tensor_tensor(u[:, 0:T], u[:, 0:T], g[:, 0:T], ALU.add), [u, g], [u])
                l4 = lcm[:, 0:T].rearrange("p (c j) -> p c j", j=16)
                ls = T1("hp_ls")
                ls4 = ls[:, 0:T].rearrange("p (c j) -> p c j", j=16)
                V(lambda e: e.tensor_tensor(ls4, bc(l4[:, :, 15], 2, [128, T // 16, 16]), l4, ALU.subtract), [lcm], [ls])
                V(lambda e: e.tensor_tensor(ls[:, 0:T], ls[:, 0:T], g[:, 0:T], ALU.add), [ls, g], [ls])
                Pq = u
                lq = ls
            P3 = Pq[:, 0:T].rearrange("p (c j) -> p c j", j=64)
            if d == 0:
                V(lambda e: e.tensor_copy(Rref[0][:, 0:C, 1:4], P3[:, :, 15:63:16]), [Pq], [Rref[0]])
            else:
                V(lambda e: e.tensor_copy(Rref[1][:, 0:C, 0:3], P3[:, :, 16:64:16]), [Pq], [Rref[1]])
            Rr = Rref[d]
            R4 = Rr[:, 0:C, :].rearrange("p (t c) n -> p t c n", c=2)
            P4 = Pq[:, 0:T].rearrange("p (t c j) -> p t c j", c=2, j=64)
            for n in range(4):
                lo, hi = (0, 16 * (n + 1)) if d == 0 else (16 * n, 64)
                V(lambda e: e.tensor_tensor(Dpad[d][:, 0:NTl, n, :, lo:hi], bc(R4[:, :, :, n], 3, [128, NTl, 2, hi - lo]),
                                            P4[:, :, :, lo:hi], ALU.subtract), [Rr, Pq], [Dpad[d]])
            Ep = ring("hp_Ep", 1, [128, BT, 4, 2, 64], F32)
            A(lambda e: e.activation(Ep[:, 0:NTl].rearrange("p t n c j -> p (t n c j)"),
                                     Dpad[d][:, 0:NTl].rearrange("p t n c j -> p (t n c j)"), AF.Exp), [Dpad[d]], [Ep])
            rr4 = rr[:, 0:T].rearrange("p (t c j) -> p t c j", c=2, j=64)
            for n in range(4):
                V(lambda e: e.scalar_tensor_tensor(khat[h][:, 0:NTl, n, :, :], rr4, oml[:, d, h:h + 1], Ep[:, 0:NTl, n, :, :], ALU.mult, ALU.mult),
                  [rr, oml, Ep], [khat[h]])
            el = T1("hp_el")
            A(lambda e: e.activation(el[:, 0:T], lq[:, 0:T], AF.Exp), [lq], [el])
            V(lambda e: e.tensor_tensor(qhat[h][:, 0:T], zq[:, 0:T], el[:, 0:T], ALU.mult), [zq, el], [qhat[h]])
            eb = T1("hp_eb")
            A(lambda e: e.activation(eb[:, 0:T], Pq[:, 0:T], AF.Exp), [Pq], [eb])
            zq4 = zq[:, 0:T].rearrange("p (t c j) -> p t c j", c=2, j=64)
            eb4 = eb[:, 0:T].rearrange("p (t c j) -> p t c j", c=2, j=64)
            for c in range(2):
                V(lambda e: e.tensor_tensor(qd[h][:, 0:NTl, c, c, :], zq4[:, :, c, :], eb4[:, :, c, :], ALU.mult), [zq, eb], [qd[h]])

    def state_update(d, ti, c, Sbf_next):
        pS = PB()
        lo = c * 64
        k.pe([(lambda e, h=h: e.matmul(pS[:, h * 128:(h + 1) * 128], kdT[lo:lo + 64, ti, h, :], vtm[lo:lo + 64, ti, h * 128:(h + 1) * 128],
                                      start=True, stop=True)) for h in range(4)], [kdT, vtm], pS)
        S = Sst[d]
        ci = ti * 2 + c
        V(lambda e: e.tensor_tensor(S[:], S[:], bc(sdec[:, :, ci], 2, [128, 4, 128]), ALU.mult), [S, sdec], [S])
        V(lambda e: e.tensor_tensor(S[:], S[:], pS[:].rearrange("p (h e) -> p h e", e=128), ALU.add), [S, pS], [S])
        if Sbf_next is not None:
            A(lambda e: e.copy(Sbf_next[:], S[:]), [S], [Sbf_next])

    def hgrn_tile(d, ti, Sbf_in):
        pA = PB()
        fns = []
        for h in range(4):
            for cq in range(2):
                for n in range(4):
                    q0 = ti * 128 + cq * 64 + n * 16
                    c0 = h * 128 + cq * 64 + n * 16
                    fns.append(lambda e, h=h, n=n, q0=q0, c0=c0: e.matmul(
                        pA[:, c0:c0 + 16], khat[h][:, ti, n, :, :].rearrange("p c j -> p (c j)"), qhat[h][:, q0:q0 + 16], start=True, stop=True))
        k.pe(fns, khat + qhat, pA)
        Asb = ring("Asb", 2, [128, 4, 128], BF16)
        mk = maskF if d == 0 else maskB
        V(lambda e: e.tensor_tensor(Asb[:], pA[:].rearrange("p (h t) -> p h t", t=128), bc(mk[:], 1, [128, 4, 128]), ALU.mult), [pA, mk], [Asb])
        order = (0, 1) if d == 0 else (1, 0)
        Smid = SBF()
        state_update(d, ti, order[0], Smid)
        Sout = SBF()
        state_update(d, ti, order[1], Sout)
        Sin = {order[0]: Sbf_in, order[1]: Smid}
        pO = PB()
        fns = []
        for h in range(4):
            o_ap = pO[:, h * 128:(h + 1) * 128]
            fns.append(lambda e, h=h, o_ap=o_ap: e.matmul(o_ap, Asb[:, h, :], vtm[:, ti, h * 128:(h + 1) * 128], start=True, stop=False))
            for ci, c in enumerate((0, 1)):
                fns.append(lambda e, h=h, c=c, o_ap=o_ap, ci=ci: e.matmul(o_ap, qd[h][:, ti, c, :, :].rearrange("p a j -> p (a j)"),
                                                                      Sin[c][:, h, :], start=False, stop=(ci == 1)))
        k.pe(fns, [Asb, vtm, Sbf_in, Smid] + qd, pO)
        return pO, Sout

    def load_block_hT(src3, b, r, t0, T, keep=None):
        hT = ring("hT", 1, [128, 8, BLK], BF16)
        for ti in range(T // 128):
            xt = XT()
            k.dma("sp", xt[:], src3[b, t0 + ti * 128:t0 + (ti + 1) * 128, :], writes=[xt])
            if keep is not None:
                keep.append(xt)
            ln_mod_T(xt, r, 0, 8, hT, ti * 128)
        return hT

    def v_block(hT, T):
        for ti in range(T // 128):
            pz = PB()
            proj_tm(hT, ti, "zi", 512, pz)
            A(lambda e: e.copy(vtm[:, ti, :], pz[:]), [pz], [vtm])

    def rope_to(dst_ap, dst_buf, pa, pb_, t0, T):
        cs = ring("cs", 1, [128, 2, BLK], F32)
        k.dma("act", cs[:, 0, 0:T], cosT[:, t0:t0 + T], writes=[cs])
        k.dma("act", cs[:, 1, 0:T], sinT[:, t0:t0 + T], writes=[cs])
        t1 = T1("rp1"); t2 = T1("rp2")
        V(lambda e: e.tensor_tensor(t1[:, 0:T], pa[:, 0:T], cs[:, 0, 0:T], ALU.mult), [pa, cs], [t1])
        V(lambda e: e.tensor_tensor(t2[:, 0:T], pb_[:, 0:T], cs[:, 1, 0:T], ALU.mult), [pb_, cs], [t2])
        G(lambda e: e.tensor_tensor(dst_ap, t1[:, 0:T], t2[:, 0:T], ALU.add), [t1, t2], [dst_buf])

    for b in range(NB):
        loadw(["zff", "zfb", "ak"], ["zi", "av"])
        hTc = load_block_hT(ctx, b, NB, 0, L)
        v_block(hTc, L)
        pk_ = PB()
        proj_fm(hTc, L, "ak", 0, pk_)
        A(lambda e: e.copy(kcs[:, 0:L], pk_[:, 0:L]), [pk_], [kcs])
        for ti in range(L // 128):
            pv = PB()
            proj_tm(hTc, ti, "av", 128, pv)
            V(lambda e: e.tensor_copy(vcaug[:, ti, :, 0:64], pv[:, 0:128].rearrange("p (g d) -> p g d", d=64)), [pv], [vcaug])
        for d in range(2):
            hgrn_prep(hTc, L, d, dosc=False)
            V(lambda e: e.memset(Sst[d][:], 0.0), [], [Sst[d]])
            tiles = list(range(L // 128)) if d == 0 else list(reversed(range(L // 128)))
            for ti in tiles:
                for c in ((0, 1) if d == 0 else (1, 0)):
                    state_update(d, ti, c, None)
        loadw(["zq", "zfb", "ak", "akp"], ["zi", "av"])
        Sbf = SBF()
        A(lambda e: e.copy(Sbf[:], Sst[1][:]), [Sst[1]], [Sbf])
        for blk in reversed(range(NBLK)):
            t0 = blk * BLK
            background_dmas(-(-len(bg_list) // (2 * NBLK)))
            hT = load_block_hT(x, b, b, t0, BLK)
            v_block(hT, BLK)
            pk_ = PB(); pkp = PB()
            proj_fm(hT, BLK, "ak", 0, pk_)
            proj_fm(hT, BLK, "akp", 0, pkp)
            rope_to(kTs[:, t0:t0 + BLK], kTs, pk_, pkp, t0, BLK)
            for ti in range(BT):
                pv = PB()
                proj_tm(hT, ti, "av", 128, pv)
                V(lambda e: e.tensor_copy(vaug[:, blk * BT + ti, :, 0:64], pv[:, 0:128].rearrange("p (g d) -> p g d", d=64)), [pv], [vaug])
            hgrn_prep(hT, BLK, 1)
            for ti in reversed(range(BT)):
                pO, Sbf = hgrn_tile(1, ti, Sbf)
                obt = ring("obt", 1, [128, 512], F32)
                A(lambda e: e.copy(obt[:], pO[:]), [pO], [obt])
                gt = blk * BT + ti
                k.dma("sp", obs[b, gt * 128:(gt + 1) * 128, :], obt[:], reads=[obt])
        k.barrier()
        loadw(["zq", "zff", "aq", "aqp"], ["zi", "zg"], with_wo=b)
        Sbf = SBF()
        A(lambda e: e.copy(Sbf[:], Sst[0][:]), [Sst[0]], [Sbf])
        for blk in range(NBLK):
            t0 = blk * BLK
            background_dmas(-(-len(bg_list) // (2 * NBLK)))
            xts = []
            hT = load_block_hT(x, b, b, t0, BLK, keep=xts)
            v_block(hT, BLK)
            qT = ring("qT", 1, [128, 4, BLK], BF16)
            for j in range(4):
                pq = PB(); pqp = PB()
                proj_fm(hT, BLK, "aq", j, pq)
                proj_fm(hT, BLK, "aqp", j, pqp)
                rope_to(qT[:, j, :], qT, pq, pqp, t0, BLK)
            hgrn_prep(hT, BLK, 0)
            for ti in range(BT):
                gt = blk * BT + ti
                ylat = ring("ylat", 1, [128, D], BF16)
                obt = ring("obt", 1, [128, 512], F32)
                k.dma("act", obt[:], obs[b, gt * 128:(gt + 1) * 128, :], writes=[obt])
                pO, Sbf = hgrn_tile(0, ti, Sbf)
                o = ring("ro_o", 1, [128, 512], F32)
                V(lambda e: e.tensor_tensor(o[:], pO[:], obt[:], ALU.add), [pO, obt], [o])
                sq = ring("ro_sq", 1, [128, 512], F32)
                G(lambda e: e.tensor_tensor(sq[:], o[:], o[:], ALU.mult), [o], [sq])
                ss = ring("ro_ss", 1, [128, 4], F32)
                V(lambda e: e.reduce_sum(ss[:], sq[:].rearrange("p (h e) -> p h e", e=128), AX.X), [sq], [ss])
                A(lambda e: e.activation(ss[:], ss[:], AF.Ln, bias=1e-6, scale=1.0 / 128.0), [ss], [ss])
                A(lambda e: e.activation(ss[:], ss[:], AF.Exp, scale=-0.5), [ss], [ss])
                V(lambda e: e.tensor_tensor(o[:].rearrange("p (h e) -> p h e", e=128), o[:].rearrange("p (h e) -> p h e", e=128),
                                            bc(ss[:], 2, [128, 4, 128]), ALU.mult), [o, ss], [o])
                G(lambda e: e.tensor_tensor(o[:], o[:], normw[:], ALU.mult), [o, normw], [o])
                pg = PB()
                proj_tm(hT, ti, "zg", 512, pg)
                sg = ring("ro_sg", 1, [128, 512], F32)
                A(lambda e: e.activation(sg[:], pg[:], AF.Exp, scale=-1.0), [pg], [sg])
                V(lambda e: e.tensor_scalar(sg[:], sg[:], 1.0, None, ALU.add), [sg], [sg])
                V(lambda e: e.reciprocal(sg[:], sg[:]), [sg], [sg])
                V(lambda e: e.tensor_tensor(sg[:], sg[:], pg[:], ALU.mult), [sg, pg], [sg])
                V(lambda e: e.tensor_tensor(ylat[:, 0:512], o[:], sg[:], ALU.mult), [o, sg], [ylat])
                for kh in range(2):
                    po = kh * 64
                    kts = [("c", i) for i in range(L // 128)]
                    if gt > 0:
                        kts.append(("p", gt - 1))
                    kts.append(("s", gt))
                    if gt < NT - 1:
                        kts.append(("n", gt + 1))
                    Pl = []
                    for ki, (kind, kt) in enumerate(kts):
                        pS = PB()
                        kap = kcs[po:po + 64, kt * 128:(kt + 1) * 128] if kind == "c" else kTs[po:po + 64, kt * 128:(kt + 1) * 128]
                        qap = qT[po:po + 64, :, ti * 128:(ti + 1) * 128]
                        msk = kind in ("p", "n")
                        pS3 = pS[:].rearrange("p (h q) -> p h q", q=128)
                        fns = [lambda e, kap=kap, qap=qap, msk=msk, pS3=pS3: e.matmul(pS3, kap, qap, start=True, stop=not msk)]
                        if msk:
                            nm = negP if kind == "p" else negN
                            fns.append(lambda e, nm=nm, pS3=pS3: e.matmul(pS3, identB[:], bc(nm[:], 1, [128, 4, 128]), start=False, stop=True))
                        k.pe(fns, [kcs, kTs, qT, identB, negP, negN], pS)
                        Psb = ring("Psb", 5, [128, 512], BF16)
                        A(lambda e: e.activation(Psb[:], pS[:], AF.Exp, scale=0.125), [pS], [Psb])
                        Pl.append((Psb, vcaug if kind == "c" else vaug, kt))
                    pOa = PB()
                    fns = []
                    for hq in range(4):
                        for ki, (Psb, va, kt) in enumerate(Pl):
                            fns.append(lambda e, hq=hq, Psb=Psb, va=va, kt=kt, ki=ki: e.matmul(
                                pOa[:, hq * 80:hq * 80 + 65], Psb[:, hq * 128:(hq + 1) * 128], va[:, kt, kh, 0:65],
                                start=(ki == 0), stop=(ki == len(Pl) - 1)))
                    k.pe(fns, [p[0] for p in Pl] + [vaug, vcaug], pOa)
                    den = ring("at_den", 1, [128, 4], F32)
                    pO4 = pOa[:, 0:320].rearrange("p (h c) -> p h c", c=80)
                    V(lambda e: e.tensor_tensor(den[:], pO4[:, :, 64], esink[:, kh * 4:(kh + 1) * 4], ALU.add), [pOa, esink], [den])
                    V(lambda e: e.reciprocal(den[:], den[:]), [den], [den])
                    V(lambda e: e.tensor_tensor(ylat[:, 512 + kh * 256:512 + (kh + 1) * 256].rearrange("p (h c) -> p h c", c=64), pO4[:, :, 0:64],
                                                bc(den[:], 2, [128, 4, 64]), ALU.mult), [pOa, den], [ylat])
                pt = PT()
                k.pe([(lambda e, kc=kc: e.transpose(pt[:, kc * 128:(kc + 1) * 128], ylat[:, kc * 128:(kc + 1) * 128], identB[:])) for kc in range(8)],
                     [ylat, identB], pt)
                yT = ring("yT", 1, [128, 8, 128], BF16)
                A(lambda e: e.copy(yT[:], pt[:].rearrange("p (c t) -> p c t", t=128)), [pt], [yT])
                xt = xts[ti]
                wo = wmap["wo"]
                for hf in range(2):
                    pp = PB()
                    k.pe([(lambda e, kc=kc: e.matmul(pp[:], yT[:, kc, :], W[:, kc, wo + hf * 512:wo + (hf + 1) * 512], start=(kc == 0), stop=(kc == 7)))
                          for kc in range(8)], [yT, W], pp)
                    V(lambda e: e.scalar_tensor_tensor(u1[:, hf * 512:(hf + 1) * 512], xt[:, hf * 512:(hf + 1) * 512], ALPHA, pp[:], ALU.mult, ALU.add),
                      [xt, pp], [u1])
                rstd, nmr = ln_stats(u1, 1e-5)
                x1 = X1()
                A(lambda e: e.activation(x1[:], u1[:], AF.Identity, bias=nmr[:, 0:1], scale=rstd[:, 0:1]), [u1, rstd, nmr], [x1])
                G(lambda e: e.tensor_tensor(x1[:], x1[:], ln1w[:], ALU.mult), [x1, ln1w], [x1])
                G(lambda e: e.tensor_tensor(x1[:], x1[:], ln1b[:], ALU.add), [x1, ln1b], [x1])
                k.dma("sp", x1s[b, gt * 128:(gt + 1) * 128, :], x1[:], reads=[x1], stream=x1)
        k.barrier()
    k.scope.close()

    background_dmas(len(bg_list))
    for j in range(NTL):
        k.dma("pool", Xs[j * 512:(j + 1) * 512, :], zrows, reads=[ztok], stream=ztok)
    I32 = mybir.dt.int32
    moe_scope = contextlib.ExitStack()
    k.scope = moe_scope
    NEX = NE + 1
    ln2w = const(ln2w_b, [128, D], name="ln2w"); ln2b = const(ln2b_b, [128, D], name="ln2b")
    rowsb = [k.sb([128, D], F32, f"rowsb{i}") for i in range(3)]
    rows_cur = [None]

    def load_rows(b):
        if rows_cur[0] != b:
            for i in range(3):
                k.dma("sp", rowsb[i][:], rws[b, i], writes=[rowsb[i]])
            rows_cur[0] = b

    slall = k.sb([128, TOT, NA], I32, "slall")
    gjall = k.sb([128, TOT, NA], F32, "gjall")
    idxw = k.sb([128, NTL], I32, "idxw")
    sub = contextlib.ExitStack()
    k.scope = sub
    rbias = const(rbias_b, [128, NE], name="rbias")
    ramp = const(ramp_d, [128, 256], name="ramp"); jv = const(jv_d, [128, NA], name="jv"); pcol = const(pcol_d, [128, 1], name="pcol")
    ustr = const(ustrict_d, [128, 128], BF16, name="ustr")
    onesB = k.sb([128, 128], BF16, "onesB")
    V(lambda e: e.memset(onesB[:], 1.0), [], [onesB])
    rw = k.sb([128, 8, NE], F32, "rw")
    k.dma("sp", rw[:], router_w.rearrange("(kc p) n -> p kc n", p=128), writes=[rw])
    emask = k.sb([128, TOT, NEX], F32, "emask")
    gall = k.sb([128, TOT, NEX], F32, "gall")
    V(lambda e: e.memset(emask[:], 1.0), [], [emask])
    V(lambda e: e.memset(gall[:], 1.0), [], [gall])
    cnt = k.sb([128, NEX], F32, "cnt")
    V(lambda e: e.memset(cnt[:], 0.0), [], [cnt])
    RT = lambda nm, w: ring(nm, 1, [128, w], F32)

    for gti in range(TOT):
        b = gti // NT
        ti = gti % NT
        load_rows(b)
        xt = XT()
        k.dma("sp", xt[:], x1s[b, ti * 128:(ti + 1) * 128, :], writes=[xt])
        hf32 = ring("h2f", 1, [128, 8, 128], F32)
        ln_mod_T(xt, b, 24, 32, None, 0, dstf=hf32)
        h2b = ring("h2b", 2, [128, D], BF16)
        V(lambda e: e.tensor_tensor(u1[:], xh[:], rowsb[1][:], ALU.mult), [xh, rowsb[1]], [u1])
        V(lambda e: e.tensor_tensor(h2b[:], u1[:], rowsb[0][:], ALU.add), [u1, rowsb[0]], [h2b])
        k.dma("sp", H2[gti * 128:(gti + 1) * 128, :], h2b[:], reads=[h2b])
        pr = PB()
        k.pe([(lambda e, kc=kc: e.matmul(pr[:, 0:NE], hf32[:, kc, :], rw[:, kc, :], start=(kc == 0), stop=(kc == 7))) for kc in range(8)], [hf32, rw], pr)
        if DEBUG and gti == 0:
            dbg_h = nc.dram_tensor("dbg_h", [128, 8, 128], F32, kind="ExternalOutput").ap()
            dbg_lg = nc.dram_tensor("dbg_lg", [128, NE], F32, kind="ExternalOutput").ap()
            k.dma("sp", dbg_h, hf32[:], reads=[hf32])
            lgs = k.sb([128, NE], F32, "lgs")
            V(lambda e: e.tensor_copy(lgs[:], pr[:, 0:NE]), [pr], [lgs])
            k.dma("sp", dbg_lg, lgs[:], reads=[lgs])
        sc = RT("rt_sc", NE)
        A(lambda e: e.activation(sc[:], pr[:, 0:NE], AF.Exp, scale=-1.0), [pr], [sc])
        V(lambda e: e.tensor_scalar(sc[:], sc[:], 1.0, None, ALU.add), [sc], [sc])
        V(lambda e: e.reciprocal(sc[:], sc[:]), [sc], [sc])
        sel = RT("rt_sel", NE)
        V(lambda e: e.tensor_tensor(sel[:], sc[:], rbias[:], ALU.add), [sc, rbias], [sel])
        sel3 = sel[:].rearrange("p (g i) -> p g i", i=8)
        m1 = RT("rt_m1", 8); m2 = RT("rt_m2", 8)
        V(lambda e: e.reduce_max(m1[:], sel3, AX.X), [sel], [m1])
        eq = RT("rt_eq", NE)
        eq3 = eq[:].rearrange("p (g i) -> p g i", i=8)
        V(lambda e: e.tensor_tensor(eq3, sel3, bc(m1[:], 2, [128, 8, 8]), ALU.is_ge), [sel, m1], [eq])
        V(lambda e: e.scalar_tensor_tensor(eq[:], eq[:], -1e9, sel[:], ALU.mult, ALU.add), [eq, sel], [eq])
        V(lambda e: e.reduce_max(m2[:], eq3, AX.X), [eq], [m2])
        V(lambda e: e.tensor_tensor(m1[:], m1[:], m2[:], ALU.add), [m1, m2], [m1])
        top8 = RT("rt_t8", 8)
        V(lambda e: e.max(top8[:], m1[:]), [m1], [top8])
        gm = RT("rt_gm", 8); gneg = RT("rt_gn", 8)
        V(lambda e: e.tensor_scalar(gm[:], m1[:], top8[:, 3:4], None, ALU.is_ge), [m1, top8], [gm])
        V(lambda e: e.tensor_scalar(gneg[:], gm[:], 1e9, -1e9, ALU.mult, ALU.add), [gm], [gneg])
        V(lambda e: e.tensor_tensor(eq3, sel3, bc(gm[:], 2, [128, 8, 8]), ALU.mult), [sel, gm], [eq])
        V(lambda e: e.tensor_tensor(eq3, eq3, bc(gneg[:], 2, [128, 8, 8]), ALU.add), [eq, gneg], [eq])
        t8b = RT("rt_t8b", 8)
        V(lambda e: e.max(t8b[:], eq[:]), [eq], [t8b])
        V(lambda e: e.tensor_scalar(emask[:, gti, 0:NE], eq[:], t8b[:, 7:8], None, ALU.is_ge), [eq, t8b], [emask])
        V(lambda e: e.tensor_tensor(eq[:], emask[:, gti, 0:NE], sc[:], ALU.mult), [emask, sc], [eq])
        ws = RT("rt_ws", 1)
        V(lambda e: e.reduce_sum(ws[:], eq[:], AX.X), [eq], [ws])
        V(lambda e: e.reciprocal(ws[:], ws[:]), [ws], [ws])
        V(lambda e: e.tensor_scalar(gall[:, gti, 0:NE], eq[:], ws[:, 0:1], 2.5, ALU.mult, ALU.mult), [eq, ws], [gall])
        emb = ring("emb", 2, [128, NEX], BF16)
        V(lambda e: e.tensor_copy(emb[:], emask[:, gti, :]), [emask], [emb])
        pc = PB()
        k.pe([lambda e: e.matmul(pc[:, 0:NEX], onesB[:], emb[:], start=True, stop=True)], [onesB, emb], pc)
        V(lambda e: e.tensor_tensor(cnt[:], cnt[:], pc[:, 0:NEX], ALU.add), [cnt, pc], [cnt])

    MT = (TOT * 128) // 512 + 1
    assert MT <= 256 and NTL <= 256
    cmpb = k.sb([128, NEX, MT], F32, "cmpb")
    V(lambda e: e.tensor_tensor(cmpb[:], bc(cnt[:], 2, [128, NEX, MT]), bc(ramp[:, 0:MT], 1, [128, NEX, MT]), ALU.is_gt), [cnt, ramp], [cmpb])
    pcn = k.sb([128, NEX], F32, "pcn")
    V(lambda e: e.reduce_sum(pcn[:], cmpb[:], AX.X), [cmpb], [pcn])
    V(lambda e: e.tensor_scalar(pcn[:], pcn[:], 512.0, None, ALU.mult), [pcn], [pcn])
    ends = k.sb([128, NEX], F32, "ends"); starts = k.sb([128, NEX], F32, "starts")
    onesf = k.sb([128, NEX], F32, "onesf")
    V(lambda e: e.memset(onesf[:], 1.0), [], [onesf])
    V(lambda e: e.tensor_tensor_scan(ends[:], onesf[:], pcn[:], 0.0, ALU.mult, ALU.add), [onesf, pcn], [ends])
    V(lambda e: e.tensor_tensor(starts[:], ends[:], pcn[:], ALU.subtract), [ends, pcn], [starts])
    ej = k.sb([128, NTL], F32, "ej")
    JC = 16
    cmp2 = k.sb([128, JC, NEX], F32, "cmp2")
    for j0 in range(0, NTL, JC):
        jn = min(JC, NTL - j0)
        V(lambda e: e.tensor_tensor(cmp2[:, 0:jn, :], bc(ends[:], 1, [128, jn, NEX]), bc(ramp[:, j0:j0 + jn], 2, [128, jn, NEX]), ALU.is_le),
          [ends, ramp], [cmp2])
        V(lambda e: e.reduce_sum(ej[:, j0:j0 + jn], cmp2[:, 0:jn, :], AX.X), [cmp2], [ej])
    V(lambda e: e.tensor_scalar(ej[:], ej[:], float(NE), 128.0, ALU.min, ALU.mult), [ej], [ej])
    V(lambda e: e.tensor_scalar(ej[:], ej[:], pcol[:, 0:1], None, ALU.add), [ej, pcol], [ej])
    V(lambda e: e.tensor_copy(idxw[:], ej[:]), [ej], [idxw])

    k.barrier()
    offs = k.sb([128, NEX], F32, "offs")
    V(lambda e: e.tensor_copy(offs[:], starts[:]), [starts], [offs])
    sct = k.sb([1, 2], F32, "sct")
    for gti in range(TOT):
        emb = ring("emb", 2, [128, NEX], BF16)
        V(lambda e: e.tensor_copy(emb[:], emask[:, gti, :]), [emask], [emb])
        pc = PB()
        k.pe([lambda e: e.matmul(pc[:, 0:NEX], ustr[:], emb[:], start=True, stop=True),
              lambda e: e.matmul(pc[:, 128:128 + NEX], onesB[:], emb[:], start=True, stop=True)], [ustr, onesB, emb], pc)
        slot = RT("p2_slot", NEX)
        V(lambda e: e.tensor_tensor(slot[:], pc[:, 0:NEX], offs[:], ALU.add), [pc, offs], [slot])
        V(lambda e: e.tensor_tensor(offs[:], offs[:], pc[:, 128:128 + NEX], ALU.add), [offs, pc], [offs])
        ks = RT("p2_ks", NEX)
        V(lambda e: e.tensor_tensor_scan(ks[:], onesf[:], emask[:, gti, :], 0.0, ALU.mult, ALU.add), [onesf, emask], [ks])
        s3 = ring("p2_s3", 1, [128, NA, NEX], F32)
        V(lambda e: e.tensor_tensor(s3[:], bc(ks[:], 1, [128, NA, NEX]), bc(jv[:], 2, [128, NA, NEX]), ALU.is_equal), [ks, jv], [s3])
        V(lambda e: e.tensor_tensor(s3[:], s3[:], bc(emask[:, gti, :], 1, [128, NA, NEX]), ALU.mult), [s3, emask], [s3])
        t3 = ring("p2_t3", 1, [128, NA, NEX], F32)
        V(lambda e: e.tensor_tensor(t3[:], s3[:], bc(slot[:], 1, [128, NA, NEX]), ALU.mult), [s3, slot], [t3])
        slf = RT("p2_slf", NA)
        V(lambda e: e.reduce_sum(slf[:], t3[:], AX.X), [t3], [slf])
        V(lambda e: e.tensor_copy(slall[:, gti, :], slf[:]), [slf], [slall])
        V(lambda e: e.tensor_tensor(t3[:], s3[:], bc(gall[:, gti, :], 1, [128, NA, NEX]), ALU.mult), [s3, gall], [t3])
        V(lambda e: e.reduce_sum(gjall[:, gti, :], t3[:], AX.X), [t3], [gjall])
        h2s = ring("h2s", 2, [128, D], BF16)
        k.dma("sp", h2s[:], H2[gti * 128:(gti + 1) * 128, :], writes=[h2s])
        for j in range(NA):
            k.idma(Xs[:, :], bass.IndirectOffsetOnAxis(ap=slall[:, gti, j:j + 1], axis=0), h2s[:, :], None, reads=[h2s, slall], stream=sct)
    k.barrier()
    sub.close()
    k.scope = moe_scope

    if DEBUG:
        dbg_sl = nc.dram_tensor("dbg_sl", [128, TOT, NA], I32, kind="ExternalOutput").ap()
        dbg_gj = nc.dram_tensor("dbg_gj", [128, TOT, NA], F32, kind="ExternalOutput").ap()
        dbg_iw = nc.dram_tensor("dbg_iw", [128, NTL], I32, kind="ExternalOutput").ap()
        k.dma("sp", dbg_sl, slall[:], reads=[slall]); k.dma("sp", dbg_gj, gjall[:], reads=[gjall]); k.dma("sp", dbg_iw, idxw[:], reads=[idxw])
    wbfs = [k.sb([128, 6144], BF16, f"wbf{i}") for i in range(3)]
    NSUB = NTL * 4
    stt = {}
    wcur = {}

    def tile_prologue(j):
        wbf = wbfs[j % 3]
        k.idma(wbf[:, :], None, ewb[:, :], bass.IndirectOffsetOnAxis(ap=idxw[:, j:j + 1], axis=0), reads=[idxw], writes=[wbf], stream=wbf)
        xst = ring("xst", 3, [128, 4, D], BF16)
        k.dma("sp", xst[:], Xs[j * 512:(j + 1) * 512, :].rearrange("(s p) d -> p s d", p=128), writes=[xst])
        wcur[j] = (wbf, xst)

    def stA0(i):
        j, s_ = divmod(i, 4)
        if s_ == 0 and j == 0:
            tile_prologue(0)
            if NTL > 1:
                tile_prologue(1)
        if s_ == 3 and j + 2 < NTL:
            tile_prologue(j + 2)
        wbf, xst = wcur[j]
        pt = PT()
        k.pe([(lambda e, kc=kc: e.transpose(pt[:, kc * 128:(kc + 1) * 128], xst[:, s_, kc * 128:(kc + 1) * 128], identB[:])) for kc in range(8)], [xst, identB], pt)
        hsT = ring("hsT", 4, [128, 8, 128], BF16)
        A(lambda e: e.copy(hsT[:], pt[:].rearrange("p (c t) -> p c t", t=128)), [pt], [hsT])
        stt[i] = [None, None, hsT]

    def stA1(i):
        j, s_ = divmod(i, 4)
        wbf, xst = wcur[j]
        hsT = stt[i][2]
        p1 = PB()
        k.pe([(lambda e, kc=kc: e.matmul(p1[:], hsT[:, kc, :], wbf[:, kc * 512:(kc + 1) * 512], start=(kc == 0), stop=(kc == 7))) for kc in range(8)],
             [hsT, wbf], p1)
        sg = ring("ex_sg", 4, [128, 256], F32)
        A(lambda e: e.activation(sg[:], p1[:, 0:256], AF.Silu), [p1], [sg])
        h1 = ring("ex_h1", 4, [128, 256], BF16)
        V(lambda e: e.tensor_tensor(h1[:], p1[:, 256:512], sg[:], ALU.mult), [p1, sg], [h1])
        stt[i][0] = h1

    def stB(i):
        h1 = stt[i][0]
        pt = PT()
        k.pe([(lambda e, fc=fc: e.transpose(pt[:, fc * 128:(fc + 1) * 128], h1[:, fc * 128:(fc + 1) * 128], identB[:])) for fc in range(2)], [h1, identB], pt)
        h1T = ring("ex_h1T", 4, [128, 2, 128], BF16)
        A(lambda e: e.copy(h1T[:], pt[:, 0:256].rearrange("p (c t) -> p c t", t=128)), [pt], [h1T])
        stt[i][1] = h1T

    def stC(i):
        j, s_ = divmod(i, 4)
        wbf, xst = wcur[j]
        h1T = stt[i][1]
        ysb = ring("ysb", 3, [128, D], BF16)
        for hf in range(2):
            p2 = PB()
            k.pe([(lambda e, fc=fc: e.matmul(p2[:], h1T[:, fc, :], wbf[:, 4096 + fc * 1024 + hf * 512:4096 + fc * 1024 + (hf + 1) * 512],
                                            start=(fc == 0), stop=(fc == 1))) for fc in range(2)], [h1T, wbf], p2)
            V(lambda e: e.tensor_copy(ysb[:, hf * 512:(hf + 1) * 512], p2[:]), [p2], [ysb])
        k.dma("sp", Ys[i * 128:(i + 1) * 128, :], ysb[:], reads=[ysb])
        del stt[i]

    for i in range(NSUB + 3):
        if i < NSUB:
            stA0(i)
        if 0 <= i - 1 < NSUB:
            stA1(i - 1)
        if 0 <= i - 2 < NSUB:
            stB(i - 2)
        if 0 <= i - 3 < NSUB:
            stC(i - 3)
    k.barrier()

    ybs = {}

    def gath(g_):
        yb_ = ring("ybuf", 2, [128, NA, D], BF16)
        for j in range(NA):
            k.idma(yb_[:, j, :], None, Ys[:, :], bass.IndirectOffsetOnAxis(ap=slall[:, g_, j:j + 1], axis=0), reads=[slall], writes=[yb_], stream=yb_,
                   nowaw=(j > 0))
        ybs[g_] = yb_

    gath(0)
    for gti in range(TOT):
        b = gti // NT
        ti = gti % NT
        load_rows(b)
        if gti + 1 < TOT:
            gath(gti + 1)
        yb = ybs.pop(gti)
        xt = XT()
        k.dma("sp", xt[:], x1s[b, ti * 128:(ti + 1) * 128, :], writes=[xt])
        A(lambda e: e.activation(xh[:], yb[:, 0, :], AF.Copy, scale=gjall[:, gti, 0:1]), [yb, gjall], [xh])
        for j in range(1, NA):
            V(lambda e: e.scalar_tensor_tensor(xh[:], yb[:, j, :], gjall[:, gti, j:j + 1], xh[:], ALU.mult, ALU.add), [yb, gjall, xh], [xh])
        V(lambda e: e.tensor_tensor(u1[:], xh[:], rowsb[2][:], ALU.mult), [xh, rowsb[2]], [u1])
        V(lambda e: e.scalar_tensor_tensor(u1[:], xt[:], ALPHA, u1[:], ALU.mult, ALU.add), [xt, u1], [u1])
        rstd, nmr = ln_stats(u1, 1e-5)
        x2 = X1()
        A(lambda e: e.activation(x2[:], u1[:], AF.Identity, bias=nmr[:, 0:1], scale=rstd[:, 0:1]), [u1, rstd, nmr], [x2])
        G(lambda e: e.tensor_tensor(x2[:], x2[:], ln2w[:], ALU.mult), [x2, ln2w], [x2])
        V(lambda e: e.tensor_tensor(x2[:], x2[:], ln2b[:], ALU.add), [x2, ln2b], [x2])
        k.dma("sp", out[b, ti * 128:(ti + 1) * 128, :], x2[:], reads=[x2], stream=x2)
    k.barrier()
    moe_scope.close()
    build.ninstr = k.ninstr
    return nc


def host_constants(N, R):
    GRID_W = 64
    rows = N // GRID_W
    row = np.repeat(np.arange(rows), GRID_W).astype(np.float32)
    col = np.tile(np.arange(GRID_W), rows).astype(np.float32)
    freqs = (10000.0 ** (-np.arange(16, dtype=np.float32) / 16)).astype(np.float32)
    ar = row[None, :] * freqs[:, None]
    ac = col[None, :] * freqs[:, None]
    ang = np.concatenate([ar, ar, ac, ac], axis=0)
    sign = np.concatenate([-np.ones(16), np.ones(16), -np.ones(16), np.ones(16)]).astype(np.float32)[:, None]
    cos64 = np.cos(ang).astype(np.float32)
    sin64 = (np.sin(ang) * sign).astype(np.float32)
    cosT = np.concatenate([cos64, cos64], 0)
    sinT = np.concatenate([sin64, sin64], 0)
    j = np.arange(128)[:, None]
    t = np.arange(128)[None, :]
    same = (j // 64) == (t // 64)
    mask_f = (same & (j <= t)).astype(np.float32)
    mask_b = (same & (j >= t)).astype(np.float32)
    negm_prev = np.where(j >= t, 0.0, NEG).astype(np.float32)
    negm_next = np.where(j <= t, 0.0, NEG).astype(np.float32)
    rst64 = np.ones((128, 512), np.float32); rst64[:, ::64] = 0
    rst16 = np.ones((128, 512), np.float32); rst16[:, ::16] = 0
    sel_rows = np.zeros((R, R, 128), np.float32)
    for r in range(R):
        sel_rows[r, r, :] = 1.0
    import ml_dtypes
    extra = dict(zrows=np.zeros((512, D), ml_dtypes.bfloat16),
                 ramp=np.ascontiguousarray(np.broadcast_to((512.0 * np.arange(256, dtype=np.float32))[None, :], (128, 256))),
                 jv=np.ascontiguousarray(np.broadcast_to(np.arange(1, 10, dtype=np.float32)[None, :], (128, 9))),
                 pcol=np.arange(128, dtype=np.float32).reshape(128, 1),
                 ustrict=(np.arange(128)[:, None] < np.arange(128)[None, :]).astype(np.float32))
    return dict(**extra, ident_f=np.eye(128, dtype=np.float32), cosT=np.ascontiguousarray(cosT), sinT=np.ascontiguousarray(sinT),
                mask_f=mask_f, mask_b=mask_b, negm_prev=negm_prev, negm_next=negm_next, rst64=rst64, rst16=rst16, sel_rows=sel_rows)


def rope_perm():
    p = np.arange(64)
    p[0:16] += 16
    p[16:32] -= 16
    p[32:48] += 16
    p[48:64] -= 16
    return p


def expert_weight_rows(inp):
    g = np.concatenate([np.asarray(inp["exp_w_gate"][0]), np.asarray(inp["shared_w_gate"])], 0)
    u = np.concatenate([np.asarray(inp["exp_w_up"][0]), np.asarray(inp["shared_w_up"])], 0)
    d = np.concatenate([np.asarray(inp["exp_w_down"][0]), np.asarray(inp["shared_w_down"])], 0)
    ne = g.shape[0]
    g4 = g.reshape(ne, 8, 128, 256).transpose(0, 2, 1, 3)
    u4 = u.reshape(ne, 8, 128, 256).transpose(0, 2, 1, 3)
    gu = np.concatenate([g4, u4], axis=3).reshape(ne, 128, 4096)
    d3 = d.reshape(ne, 2, 128, 1024).transpose(0, 2, 1, 3).reshape(ne, 128, 2048)
    return np.ascontiguousarray(np.concatenate([gu, d3], axis=2).reshape(ne * 128, 6144))


def shared_inputs(inp):
    w_in = np.asarray(inp["w_in"][0])
    zq, zff, zfb, zi, zg = [w_in[:, i * 512:(i + 1) * 512] for i in range(5)]
    aq = w_in[:, 2560:3072]; ak = w_in[:, 3072:3200]; av = w_in[:, 3200:3328]
    perm = rope_perm()
    hord = [0, 4, 1, 5, 2, 6, 3, 7]
    aq8 = aq.reshape(D, 8, 64)
    aqo = aq8[:, hord, :].reshape(D, 512)
    aqp = aq8[:, :, perm][:, hord, :].reshape(D, 512)
    akp = ak.reshape(D, 2, 64)[:, :, perm].reshape(D, 128)
    w_fm = np.ascontiguousarray(np.concatenate([zq, zff, zfb, aqo, aqp, ak, akp], axis=1))
    w_tm = np.ascontiguousarray(np.concatenate([zi, zg, av], axis=1))
    rep = lambda v: np.ascontiguousarray(np.broadcast_to(np.asarray(v).reshape(1, -1), (128, np.asarray(v).size)))
    lb = np.stack([np.asarray(inp["hg_lb_fwd"]), np.asarray(inp["hg_lb_bwd"])], 0)
    lbT = np.ascontiguousarray(lb.reshape(2, 2, 4, 128).transpose(3, 0, 1, 2))
    b_ada = np.asarray(inp["b_ada"][0])
    sh = dict(
        w_ada=np.asarray(inp["w_ada"][0]), b_adaT=np.ascontiguousarray(b_ada.reshape(48, 128).T),
        b_ada_g=np.ascontiguousarray(np.stack([b_ada[2048:3072], b_ada[3072:4096], b_ada[4096:5120], b_ada[5120:6144]], 0)),
        w_fm=w_fm, w_tm=w_tm, lbT=lbT, normw_b=rep(inp["hg_norm_w"][0]), sink_b=rep(inp["attn_sink"][0]),
        w_out=np.asarray(inp["w_out"][0]),
        ln1w_b=rep(inp["ln1_w"][0]), ln1b_b=rep(inp["ln1_b"][0]), ln2w_b=rep(inp["ln2_w"][0]), ln2b_b=rep(inp["ln2_b"][0]),
        router_w=np.asarray(inp["router_w"][0]), rbias_b=rep(inp["router_bias"][0]),
        ew_all=expert_weight_rows(inp),
    )
    return sh


def core_inputs(inp, b0, NB):
    x = np.ascontiguousarray(np.asarray(inp["x"][b0:b0 + NB]))
    ctx = np.ascontiguousarray(np.asarray(inp["ctx"][b0:b0 + NB]))
    c3 = np.concatenate([np.asarray(inp["c"][b0:b0 + NB]), np.asarray(inp["c_ctx"])[None, :]], 0)
    return dict(x=x, ctx=ctx, c3T=np.ascontiguousarray(c3.T))


def kernel(**inputs):
    B, N, _ = inputs["x"].shape
    L = inputs["ctx"].shape[1]
    ncores = 8
    NB = B // ncores
    nc = build(NB, N, L, TBT=16)
    sh = shared_inputs(inputs)
    sh.update(host_constants(N, NB + 1))
    in_maps = []
    for c in range(ncores):
        m = dict(sh)
        m.update(core_inputs(inputs, c * NB, NB))
        in_maps.append(m)
    res = run_bass_kernel_spmd(nc, in_maps, core_ids=list(range(ncores)))
    return np.concatenate([np.asarray(r["out"]) for r in res.results], axis=0).astype(np.float32)
```
